# Optimizing a Trainium2 kernel written in Bass

```python
import math
import jax, jax.numpy as jnp
from jax import lax
import numpy as np

D_MODEL = 1024
BATCH = 8
SEQ = 4096
DEPTH = 4

HEAD_DIM = 64
D_MIX = D_MODEL
RWKV_HEADS = (3 * D_MIX) // (8 * HEAD_DIM)
D_RWKV = RWKV_HEADS * HEAD_DIM
ATTN_HEADS = (3 * D_MIX) // (8 * HEAD_DIM)
D_ATTN = ATTN_HEADS * HEAD_DIM
D_CONV = D_MIX - D_RWKV - D_ATTN
DECAY_LORA = 32
ICLR_LORA = 32
GATE_LORA = 64
CONV_WIDTH = 31
DILATED_PATTERNS = ((128, 1), (512, 4), (2048, 16))
ATTN_BLOCK = 128
D_FF = 2816
NORM_EPS = 1e-6
RWKV_GN_EPS = 64e-5
CONV_LN_EPS = 1e-5
D_SHIFT = 3 * D_RWKV + DECAY_LORA + ICLR_LORA + GATE_LORA
D_IN = D_SHIFT + 3 * D_ATTN + 2 * D_CONV

kernel_name = 'hybrid_rwkv7_dilattn_conformer_macaron'


def _alibi_slopes(n):
    def pow2(m):
        start = 2.0 ** (-8.0 / m)
        return [start ** (i + 1) for i in range(m)]
    if math.log2(n).is_integer():
        return pow2(n)
    c = 2 ** int(math.floor(math.log2(n)))
    return pow2(c) + pow2(2 * c)[0::2][: n - c]


def _rms_norm(x, g):
    xf = x.astype(jnp.float32)
    y = xf * lax.rsqrt(jnp.mean(xf * xf, axis=-1, keepdims=True) + NORM_EPS)
    return y.astype(x.dtype) * g


def _head_rms(t, g):
    return t * lax.rsqrt(jnp.mean(t * t, axis=-1, keepdims=True) + NORM_EPS) * g.astype(jnp.float32)


def _swiglu(x, w_gate, w_up, w_down):
    return (jax.nn.silu(x @ w_gate) * (x @ w_up)) @ w_down


def _token_shift(p, mu):
    prev = jnp.pad(p, ((0, 0), (1, 0), (0, 0)))[:, :-1]
    return p + (prev - p) * mu.astype(p.dtype)


def _rwkv7_scan(r, w, k, v, kk, a):
    B, S, H, N = r.shape
    xs = tuple(jnp.moveaxis(t, 1, 0) for t in (r, w, k, v, -kk, kk * a))

    def step(state, inp):
        r_t, w_t, k_t, v_t, a_t, b_t = inp
        sa = jnp.einsum('bhij,bhj->bhi', state, a_t)
        state = (state * w_t[:, :, None, :]
                 + sa[..., None] * b_t[:, :, None, :]
                 + v_t[..., None] * k_t[:, :, None, :])
        y_t = jnp.einsum('bhij,bhj->bhi', state, r_t)
        return state, y_t

    state0 = jnp.zeros((B, H, N, N), jnp.float32)
    _, y = lax.scan(step, state0, xs)
    return jnp.moveaxis(y, 0, 1)


def _rwkv7_mixer(p, w0, w2, a0, a2, g2, k_k, k_a, r_k, ln_w, ln_b):
    B, S, _ = p.shape
    o = 0
    r = p[..., o:o + D_RWKV]; o += D_RWKV
    k = p[..., o:o + D_RWKV]; o += D_RWKV
    v = p[..., o:o + D_RWKV]; o += D_RWKV
    xw = p[..., o:o + DECAY_LORA]; o += DECAY_LORA
    xa = p[..., o:o + ICLR_LORA]; o += ICLR_LORA
    xg = p[..., o:o + GATE_LORA]
    f32 = jnp.float32
    w_log = -jax.nn.softplus(-(w0.astype(f32) + jnp.tanh(xw) @ w2.astype(f32))) - 0.5
    decay = jnp.exp(-jnp.exp(w_log))
    a = jax.nn.sigmoid(a0.astype(f32) + xa @ a2.astype(f32))
    g = jax.nn.sigmoid(xg) @ g2.astype(f32)
    heads = lambda t: t.reshape(B, S, RWKV_HEADS, HEAD_DIM)
    kk = heads(k * k_k.astype(f32))
    kk = kk * lax.rsqrt(jnp.sum(kk * kk, axis=-1, keepdims=True) + 1e-12)
    k = k * (1.0 + (a - 1.0) * k_a.astype(f32))
    rh, kh, vh = heads(r), heads(k), heads(v)
    y = _rwkv7_scan(rh, heads(decay), kh, vh, kk, heads(a))
    mu = jnp.mean(y, axis=-1, keepdims=True)
    var = jnp.mean(jnp.square(y - mu), axis=-1, keepdims=True)
    y = ((y - mu) * lax.rsqrt(var + RWKV_GN_EPS)).reshape(B, S, D_RWKV)
    y = y * ln_w.astype(f32) + ln_b.astype(f32)
    bonus = jnp.sum(rh * kh * r_k.astype(f32), axis=-1, keepdims=True) * vh
    return (y + bonus.reshape(B, S, D_RWKV)) * g


def _dilated_attention(q, k, v, slopes, window, dilation):
    B, S, H, N = q.shape
    n_keys = window // dilation
    L = S // dilation
    nb = -(-L // ATTN_BLOCK)
    Lp = nb * ATTN_BLOCK

    def blocks(t):
        t = t.reshape(B, L, dilation, H, N)
        t = jnp.pad(t, ((0, 0), (0, Lp - L), (0, 0), (0, 0), (0, 0)))
        return t.reshape(B, nb, ATTN_BLOCK, dilation, H, N)

    def band(t):
        prev = jnp.pad(t, ((0, 0), (1, 0), (0, 0), (0, 0), (0, 0), (0, 0)))[:, :-1]
        return jnp.concatenate([prev, t], axis=2)

    qb = blocks(q)
    kb = band(blocks(k))
    vb = band(blocks(v))
    s = jnp.einsum('bnqchd,bnkchd->bnqchk', qb, kb)
    qi = jnp.arange(ATTN_BLOCK)
    ki = jnp.arange(2 * ATTN_BLOCK)
    dist = qi[:, None] + ATTN_BLOCK - ki[None, :]
    key_idx = jnp.arange(nb)[:, None] * ATTN_BLOCK + ki[None, :] - ATTN_BLOCK
    valid = ((dist >= 0) & (dist <= n_keys))[None] & (key_idx >= 0)[:, None, :]
    bias = -slopes[:, None] * (dist * dilation).astype(jnp.float32)[:, None, None, :]
    s = jnp.where(valid[None, :, :, None, None, :], s + bias, -jnp.inf)
    lse = jax.nn.logsumexp(s, axis=-1)
    p = jnp.exp(s - lse[..., None])
    o = jnp.einsum('bnqchk,bnkchd->bnqchd', p, vb)
    o = o.reshape(B, Lp, dilation, H, N)[:, :L].reshape(B, S, H, N)
    lse = lse.reshape(B, Lp, dilation, H)[:, :L].reshape(B, S, H)
    return o, lse


def _attention_mixer(qkv, q_norm, k_norm):
    B, S, _ = qkv.shape
    q = qkv[..., :D_ATTN].reshape(B, S, ATTN_HEADS, HEAD_DIM)
    k = qkv[..., D_ATTN:2 * D_ATTN].reshape(B, S, ATTN_HEADS, HEAD_DIM)
    v = qkv[..., 2 * D_ATTN:].reshape(B, S, ATTN_HEADS, HEAD_DIM)
    q = _head_rms(q, q_norm) * (HEAD_DIM ** -0.5)
    k = _head_rms(k, k_norm)
    slopes = jnp.asarray(_alibi_slopes(ATTN_HEADS), jnp.float32)
    res = [_dilated_attention(q, k, v, slopes, w, d) for (w, d) in DILATED_PATTERNS]
    outs = jnp.stack([r_[0] for r_ in res], axis=0)
    lses = jnp.stack([r_[1] for r_ in res], axis=0)
    alpha = jax.nn.softmax(lses, axis=0)
    out = jnp.sum(alpha[..., None] * outs, axis=0)
    return out.reshape(B, S, D_ATTN)


def _conv_mixer(u, dw_w, dw_b, ln_w, ln_b):
    f32 = jnp.float32
    z = u[..., :D_CONV] * jax.nn.sigmoid(u[..., D_CONV:])
    z = lax.conv_general_dilated(z, dw_w.astype(f32)[:, None, :], window_strides=(1,),
                                 padding=[(CONV_WIDTH - 1, 0)],
                                 dimension_numbers=('NWC', 'WIO', 'NWC'),
                                 feature_group_count=D_CONV) + dw_b.astype(f32)
    mu = jnp.mean(z, axis=-1, keepdims=True)
    var = jnp.mean(jnp.square(z - mu), axis=-1, keepdims=True)
    z = (z - mu) * lax.rsqrt(var + CONV_LN_EPS) * ln_w.astype(f32) + ln_b.astype(f32)
    return jax.nn.silu(z)


def setup_inputs(seed: int = 0) -> dict:
    key = jax.random.key(seed)
    ks = iter(jax.random.split(key, 40))
    f32 = jnp.float32
    L = DEPTH

    def normal(shape, scale):
        return jax.random.normal(next(ks), shape, f32) * scale

    def gain(shape):
        return 1.0 + 0.05 * jax.random.normal(next(ks), shape, f32)

    def unif(shape, lo, hi):
        return jax.random.uniform(next(ks), shape, f32, lo, hi)

    return {
        'x': normal((BATCH, SEQ, D_MODEL), 1.0),
        'norm_ffn1': gain((L, D_MODEL)),
        'ffn1_w_gate': normal((L, D_MODEL, D_FF), D_MODEL ** -0.5),
        'ffn1_w_up': normal((L, D_MODEL, D_FF), D_MODEL ** -0.5),
        'ffn1_w_down': normal((L, D_FF, D_MODEL), D_FF ** -0.5),
        'norm_mix': gain((L, D_MODEL)),
        'w_in': normal((L, D_MODEL, D_IN), D_MODEL ** -0.5),
        'shift_mu': unif((L, D_SHIFT), 0.0, 1.0),
        'rwkv_w0': unif((L, D_RWKV), -6.0, 1.0),
        'rwkv_w2': normal((L, DECAY_LORA, D_RWKV), 0.1),
        'rwkv_a0': normal((L, D_RWKV), 0.5),
        'rwkv_a2': normal((L, ICLR_LORA, D_RWKV), 0.1),
        'rwkv_g2': normal((L, GATE_LORA, D_RWKV), GATE_LORA ** -0.5),
        'rwkv_k_k': 0.85 + normal((L, D_RWKV), 0.05),
        'rwkv_k_a': gain((L, D_RWKV)),
        'rwkv_r_k': 0.5 + normal((L, RWKV_HEADS, HEAD_DIM), 0.1),
        'rwkv_ln_w': gain((L, D_RWKV)),
        'rwkv_ln_b': normal((L, D_RWKV), 0.02),
        'attn_q_norm': gain((L, HEAD_DIM)),
        'attn_k_norm': gain((L, HEAD_DIM)),
        'conv_dw_w': normal((L, CONV_WIDTH, D_CONV), CONV_WIDTH ** -0.5),
        'conv_dw_b': normal((L, D_CONV), 0.02),
        'conv_ln_w': gain((L, D_CONV)),
        'conv_ln_b': normal((L, D_CONV), 0.02),
        'w_out': normal((L, D_MIX, D_MODEL), D_MIX ** -0.5),
        'norm_ffn2': gain((L, D_MODEL)),
        'ffn2_w_gate': normal((L, D_MODEL, D_FF), D_MODEL ** -0.5),
        'ffn2_w_up': normal((L, D_MODEL, D_FF), D_MODEL ** -0.5),
        'ffn2_w_down': normal((L, D_FF, D_MODEL), D_FF ** -0.5),
    }


def reference(x, norm_ffn1, ffn1_w_gate, ffn1_w_up, ffn1_w_down, norm_mix, w_in, shift_mu,
              rwkv_w0, rwkv_w2, rwkv_a0, rwkv_a2, rwkv_g2, rwkv_k_k, rwkv_k_a, rwkv_r_k,
              rwkv_ln_w, rwkv_ln_b, attn_q_norm, attn_k_norm, conv_dw_w, conv_dw_b,
              conv_ln_w, conv_ln_b, w_out, norm_ffn2, ffn2_w_gate, ffn2_w_up, ffn2_w_down):
    for l in range(DEPTH):
        x = x + 0.5 * _swiglu(_rms_norm(x, norm_ffn1[l]), ffn1_w_gate[l], ffn1_w_up[l], ffn1_w_down[l])
        h = _rms_norm(x, norm_mix[l])
        proj = (h @ w_in[l]).astype(jnp.float32)
        p_rwkv = _token_shift(proj[..., :D_SHIFT], shift_mu[l])
        y_rwkv = _rwkv7_mixer(p_rwkv, rwkv_w0[l], rwkv_w2[l], rwkv_a0[l], rwkv_a2[l], rwkv_g2[l],
                              rwkv_k_k[l], rwkv_k_a[l], rwkv_r_k[l], rwkv_ln_w[l], rwkv_ln_b[l])
        y_attn = _attention_mixer(proj[..., D_SHIFT:D_SHIFT + 3 * D_ATTN], attn_q_norm[l], attn_k_norm[l])
        y_conv = _conv_mixer(proj[..., D_SHIFT + 3 * D_ATTN:], conv_dw_w[l], conv_dw_b[l],
                             conv_ln_w[l], conv_ln_b[l])
        mix = jnp.concatenate([y_rwkv, y_attn, y_conv], axis=-1).astype(x.dtype)
        x = x + mix @ w_out[l]
        x = x + 0.5 * _swiglu(_rms_norm(x, norm_ffn2[l]), ffn2_w_gate[l], ffn2_w_up[l], ffn2_w_down[l])
    return x
```

```python
import contextlib
import math
import os
import numpy as np
import concourse.bass as bass
import concourse.mybir as mybir
from concourse.bass_utils import run_bass_kernel_spmd

F32 = mybir.dt.float32
ALU = mybir.AluOpType
AF = mybir.ActivationFunctionType

S = 4096
D = 1024
DFF = 2816
NFC = 22
NOC = 23
TT = 512
NT = S // TT
NP = 128
C_ID, C_ONES, C_BD, C_MSU, C_MSL, C_MU, C_RM = 0, 128, 256, 384, 512, 640, 768
NCST = 768 + 512
PATTERNS = ((128, 1), (512, 4), (2048, 16))
EXPM05 = math.exp(-0.5)

SEM_GEN = 8000
KF = os.environ.get('KF', 'ofi')
DMA_RING = 6


class Op:
    __slots__ = ('eng', 'fn', 'deps', 'is_dma', 'needs_inc', 'tok', 'ring_wait')

    def __init__(self, eng, fn, is_dma):
        self.eng = eng
        self.fn = fn
        self.deps = []
        self.is_dma = is_dma
        self.needs_inc = is_dma
        self.tok = None
        self.ring_wait = None


class Prog:
    def __init__(self, nc):
        self.nc = nc
        self.ops = {e: [] for e in ('pe', 'act', 'dve', 'pool', 'sp')}
        self.lastw = {}
        self.readers = {}
        self.fine = {}
        self.pending = None
        self.applied = set()

    def keys(self, ap):
        name = ap.tensor.name
        g = self.fine.get(name)
        if g is None:
            return [name]
        gran, row = g
        off = ap.offset % row
        span = 1
        for st, cnt in list(ap.ap)[1:]:
            span += (cnt - 1) * st
        return [(name, i) for i in range(off // gran, (off + span - 1) // gran + 1)]

    def _add(self, eng, fn, reads, writes, is_dma):
        o = Op(eng, fn, is_dma)
        lst = self.ops[eng]
        me = (eng, len(lst))
        lst.append(o)
        reads = list(dict.fromkeys(reads))
        writes = list(dict.fromkeys(writes))
        deps = set()
        for k in reads:
            w = self.lastw.get(k)
            if w is not None:
                if self.ops[w[0]][w[1]].is_dma or is_dma or w[0] != eng or eng != 'pe':
                    deps.add(w)
            if isinstance(k, str) and k.startswith('pb'):
                for r in self.readers.get(k, ()):
                    if r[0] != eng:
                        deps.add(r)
        for k in writes:
            w = self.lastw.get(k)
            if w is not None:
                if self.ops[w[0]][w[1]].is_dma or w[0] != eng or is_dma or eng != 'pe':
                    deps.add(w)
            for r in self.readers.get(k, ()):
                if self.ops[r[0]][r[1]].is_dma or r[0] != eng or is_dma or eng != 'pe':
                    deps.add(r)
        if self.pending is not None and eng not in self.applied:
            deps.update(self.pending)
            self.applied.add(eng)
        deps.discard(me)
        o.deps = list(deps)
        for d in o.deps:
            self.ops[d[0]][d[1]].needs_inc = True
        for k in reads:
            rl = self.readers.setdefault(k, [])
            if not is_dma:
                rl[:] = [r for r in rl if r[0] != eng or self.ops[r[0]][r[1]].is_dma]
            rl.append(me)
        for k in writes:
            self.lastw[k] = me
            self.readers[k] = []
        return o

    def barrier(self):
        deps = []
        for eng, lst in self.ops.items():
            for i in range(len(lst) - 1, -1, -1):
                if not lst[i].is_dma:
                    deps.append((eng, i))
                    break
            c = 0
            for i in range(len(lst) - 1, -1, -1):
                if lst[i].is_dma:
                    deps.append((eng, i))
                    c += 1
                    if c >= DMA_RING:
                        break
        self.pending = deps
        self.applied = set()
        self.lastw.clear()
        self.readers.clear()

    def _k(self, aps):
        ks = []
        for a in aps:
            if a is not None and not isinstance(a, (int, float)) and a.tensor.name in self.track:
                ks += self.keys(a)
        return ks

    def mm(self, out, lhsT, rhs, start=True, stop=True):
        return self._add('pe', lambda e: e.matmul(out, lhsT=lhsT, rhs=rhs, start=start, stop=stop),
                         self._k([lhsT, rhs]), self._k([out]), False)

    def tr(self, out, in_, ident):
        return self._add('pe', lambda e: e.transpose(out, in_, ident), self._k([in_, ident]), self._k([out]), False)

    def act(self, out, in_, func, bias=0.0, scale=1.0):
        return self._add('act', lambda e: e.activation(out=out, in_=in_, func=func, bias=bias, scale=scale),
                         self._k([in_, bias, scale]), self._k([out]), False)

    def tt(self, eng, out, in0, in1, op):
        return self._add(eng, lambda e: e.tensor_tensor(out=out, in0=in0, in1=in1, op=op),
                         self._k([in0, in1]), self._k([out]), False)

    def ts(self, eng, out, in0, s1, op0, s2=None, op1=None):
        if op1 is None:
            fn = lambda e: e.tensor_scalar(out=out, in0=in0, scalar1=s1, scalar2=None, op0=op0)
        else:
            fn = lambda e: e.tensor_scalar(out=out, in0=in0, scalar1=s1, scalar2=s2, op0=op0, op1=op1)
        return self._add(eng, fn, self._k([in0, s1, s2]), self._k([out]), False)

    def stt(self, eng, out, in0, scalar, in1, op0, op1):
        return self._add(eng, lambda e: e.scalar_tensor_tensor(out=out, in0=in0, scalar=scalar, in1=in1, op0=op0, op1=op1),
                         self._k([in0, scalar, in1]), self._k([out]), False)

    def scan(self, out, d0, d1, init, op0, op1):
        return self._add('dve', lambda e: e.tensor_tensor_scan(out=out, data0=d0, data1=d1, initial=init, op0=op0, op1=op1),
                         self._k([d0, d1]), self._k([out]), False)

    def copy(self, eng, out, in_):
        if eng == 'act':
            return self.act(out, in_, AF.Copy)
        return self._add(eng, lambda e: e.tensor_copy(out=out, in_=in_), self._k([in_]), self._k([out]), False)

    def recip(self, out, in_):
        return self._add('dve', lambda e: e.reciprocal(out=out, in_=in_), self._k([in_]), self._k([out]), False)

    def memset(self, eng, ap, val):
        return self._add(eng, lambda e: e.memset(ap, val), [], self._k([ap]), False)

    def dma(self, eng, out, in_):
        return self._add(eng, lambda e: e.dma_start(out=out, in_=in_), self._k([in_]), self._k([out]), True)

    def emit(self, final_ops):
        nc = self.nc
        with contextlib.ExitStack() as es:
            sems = {}
            tail = []
            for eng, lst in self.ops.items():
                for i in range(len(lst) - 1, -1, -1):
                    if not lst[i].is_dma:
                        lst[i].needs_inc = True
                        tail.append(lst[i])
                        break
                c_ = 0
                for i in range(len(lst) - 1, -1, -1):
                    if lst[i].is_dma:
                        tail.append(lst[i])
                        c_ += 1
                        if c_ >= DMA_RING:
                            break
            for eng, lst in self.ops.items():
                cnt = 0
                nd = 0
                for o in lst:
                    if o.is_dma:
                        slot, rnd = nd % DMA_RING, nd // DMA_RING
                        o.tok = (f"d_{eng}_{slot}", 16 * (rnd + 1))
                        if rnd > 0:
                            o.ring_wait = (f"d_{eng}_{slot}", 16 * rnd)
                        nd += 1
                    elif o.needs_inc:
                        g = cnt // SEM_GEN
                        o.tok = (f"c_{eng}_{g}", cnt - g * SEM_GEN + 1)
                        cnt += 1
            self.maxtok = {}
            for lst in self.ops.values():
                for o in lst:
                    if o.tok:
                        self.maxtok[o.tok[0]] = max(self.maxtok.get(o.tok[0], 0), o.tok[1])
            if os.environ.get('KSIM'):
                print('MAXTOK', self.maxtok)
            for lst in self.ops.values():
                for o in lst:
                    if o.tok and o.tok[0] not in sems:
                        sems[o.tok[0]] = es.enter_context(nc.semaphore(o.tok[0]))
            engmap = {'pe': 'tensor', 'act': 'scalar', 'dve': 'vector', 'pool': 'gpsimd', 'sp': 'sync'}
            plan = {}
            stats = {}
            for eng, lst in self.ops.items():
                known = {}
                nw = 0
                pl = []
                for o in lst:
                    need = {}
                    for d in o.deps:
                        t = self.ops[d[0]][d[1]].tok
                        if need.get(t[0], 0) < t[1]:
                            need[t[0]] = t[1]
                    if o.ring_wait:
                        t = o.ring_wait
                        if need.get(t[0], 0) < t[1]:
                            need[t[0]] = t[1]
                    ws = []
                    for s_, v in need.items():
                        if known.get(s_, 0) < v:
                            ws.append((s_, v))
                            known[s_] = v
                            nw += 1
                    pl.append(ws)
                plan[eng] = pl
                stats[eng] = (len(lst), nw)
            if os.environ.get('KSIM'):
                val = {k_: 0 for k_ in sems}
                pos = {e_: 0 for e_ in self.ops}
                prog = True
                while prog:
                    prog = False
                    for e_, lst in self.ops.items():
                        while pos[e_] < len(lst):
                            i_ = pos[e_]
                            if all(val[s_] >= v for s_, v in plan[e_][i_]):
                                o = lst[i_]
                                if o.tok is not None:
                                    val[o.tok[0]] += 16 if o.is_dma else 1
                                    assert val[o.tok[0]] == o.tok[1], (e_, i_, o.tok, val[o.tok[0]])
                                pos[e_] += 1
                                prog = True
                            else:
                                break
                for e_, lst in self.ops.items():
                    if pos[e_] < len(lst):
                        print("SIM DEADLOCK", e_, pos[e_], len(lst), plan[e_][pos[e_]], {s_: val[s_] for s_, v in plan[e_][pos[e_]]})
                print("SIM done", {e_: pos[e_] for e_ in pos})
            block = es.enter_context(nc.Block())
            for eng, lst in self.ops.items():
                def body(e, eng=eng, lst=lst):
                    for o, ws in zip(lst, plan[eng]):
                        for s_, v in ws:
                            e.wait_ge(sems[s_], v)
                        ins = o.fn(e)
                        if o.tok is not None:
                            ins.then_inc(sems[o.tok[0]], 16 if o.is_dma else 1)
                    for fo in list(final_ops) + tail:
                        e.wait_ge(sems[fo.tok[0]], fo.tok[1])
                getattr(block, engmap[eng])(body)
            self.stats = stats
        return stats


def build(L, dbg=False, phases=('pass', 'conv', 'attn', 'rwkv')):
    nc = bass.Bass("TRN2", target_bir_lowering=False)

    def din(name, shape):
        return nc.dram_tensor(name, shape, F32, kind="ExternalInput").ap()

    x_in = din("x", [S, D])
    wgu = [din(f"wgu{i}", [L, NFC, 128, 2048]) for i in (1, 2)]
    wd = [din(f"wd{i}", [L, 8, 128, NFC * 128]) for i in (1, 2)]
    win = din("win", [L, NOC, 128, 1024])
    wout = din("wout", [L, 8, 128, 1024])
    lora_d = din("lora", [L, 128, 384])
    prm_d = din("prm", [128, L * NP])
    cst_d = din("cst", [128, NCST])
    abias_d = din("abias", [128, 18 * 256])
    out = nc.dram_tensor("out", [S, D], F32, kind="ExternalOutput").ap()
    skind = "ExternalOutput" if dbg else "Internal"
    xT = nc.dram_tensor("xT", [8, 128, S], F32, kind=skind).ap()
    projT = nc.dram_tensor("projT", [NOC, 128, S], F32, kind=skind).ap()
    mixT = nc.dram_tensor("mixT", [8, 128, S], F32, kind=skind).ap()

    P = Prog(nc)
    P.track = set()
    final_ops = []

    with contextlib.ExitStack() as gs:
        uid = [0]

        def sb(es, name, shape, fine=None):
            uid[0] += 1
            name = f"{name}_u{uid[0]}"
            t = es.enter_context(nc.sbuf_tensor(name, shape, F32))
            P.track.add(name)
            if fine:
                row = 1
                for s_ in shape[1:]:
                    row *= s_
                P.fine[name] = (fine, row)
            return t

        cst = sb(gs, "cst_s", [128, NCST], fine=128)
        prm = sb(gs, "prm_s", [128, L * NP])
        pbs = []
        for i in range(8):
            t = gs.enter_context(nc.psum_tensor(f"pb{i}", [128, 512], F32))
            P.track.add(f"pb{i}")
            pbs.append(t)
        P.dma('sp', cst[:], cst_d)
        P.dma('sp', prm[:], prm_d)
        ident = cst[:, C_ID:C_ID + 128]
        ones = cst[:, C_ONES:C_ONES + 128]
        bd64 = cst[:, C_BD:C_BD + 128]
        msu = cst[:, C_MSU:C_MSU + 128]
        msl = cst[:, C_MSL:C_MSL + 128]
        mu_ = cst[:, C_MU:C_MU + 128]
        rmask = cst[:, C_RM:C_RM + 512]

        def pcol(l, c, n=1):
            return prm[:, l * NP + c: l * NP + c + n]

        def run_pass(l):
            with contextlib.ExitStack() as es:
                xt = sb(es, "xt", [128, 8, TT], fine=TT)
                hT = sb(es, "hT", [128, 8, TT])
                actT = sb(es, "actT", [128, NFC, TT], fine=TT)
                sq = sb(es, "sq", [128, 8 * TT])
                rstd = sb(es, "rstd", [128, TT])
                sg = [sb(es, f"sg{i}", [128, TT]) for i in range(2)]
                wb = [sb(es, f"wb{i}", [128, 2048]) for i in range(3)]
                wdb = [sb(es, f"wdb{i}", [128, NFC * 128]) for i in range(2)]
                pj = [sb(es, f"pj{i}", [128, TT]) for i in range(3)]
                xtok = [sb(es, f"xtok{i}", [128, D]) for i in range(2)]
                cnt = {'wb': 0, 'wd': 0, 'pj': 0, 'g': 0, 'o': 0}

                def norm_h(gcol0):
                    P.act(sq[:], xt[:].rearrange("p c t -> p (c t)"), AF.Square)
                    for dc in range(8):
                        P.mm(pbs[0][:], ones, sq[:, dc * TT:(dc + 1) * TT], start=(dc == 0), stop=(dc == 7))
                    P.act(rstd[:], pbs[0][:], AF.Sqrt, bias=eps6[:, 0:1], scale=1.0 / D)
                    P.recip(rstd[:], rstd[:])
                    for dc in range(8):
                        P.stt('dve', hT[:, dc, :], xt[:, dc, :], pcol(l_cur[0], gcol0 + dc), rstd[:], ALU.mult, ALU.mult)

                def ffn(ll, which):
                    l_cur[0] = ll
                    norm_h(0 if which == 0 else 16)
                    for fc in range(NFC):
                        w = wb[cnt['wb'] % 3]
                        cnt['wb'] += 1
                        P.dma('sp', w[:], wgu[which][ll, fc])
                        pg = pbs[1 + cnt['g'] % 2]
                        pu = pbs[3 + cnt['g'] % 2]
                        sgt = sg[cnt['g'] % 2]
                        cnt['g'] += 1
                        for dc in range(8):
                            P.mm(pg[:], w[:, dc * 128:(dc + 1) * 128], hT[:, dc, :], start=(dc == 0), stop=(dc == 7))
                        for dc in range(8):
                            P.mm(pu[:], w[:, 1024 + dc * 128:1024 + (dc + 1) * 128], hT[:, dc, :], start=(dc == 0), stop=(dc == 7))
                        P.act(sgt[:], pg[:], AF.Silu)
                        P.tt('dve', actT[:, fc, :], sgt[:], pu[:], ALU.mult)
                    for dc in range(8):
                        w = wdb[cnt['wd'] % 2]
                        cnt['wd'] += 1
                        P.dma('sp', w[:], wd[which][ll, dc])
                        po = pbs[5 + cnt['o'] % 2]
                        cnt['o'] += 1
                        for fc in range(NFC):
                            P.mm(po[:], w[:, fc * 128:(fc + 1) * 128], actT[:, fc, :], start=(fc == 0), stop=(fc == NFC - 1))
                        P.stt('dve', xt[:, dc, :], po[:], 0.5, xt[:, dc, :], ALU.mult, ALU.add)

                def inproj(ll, tok0):
                    l_cur[0] = ll
                    norm_h(8)
                    for oc in range(NOC):
                        w = wb[cnt['wb'] % 3]
                        cnt['wb'] += 1
                        P.dma('sp', w[:, 0:1024], win[ll, oc])
                        po = pbs[5 + cnt['o'] % 2]
                        cnt['o'] += 1
                        for dc in range(8):
                            P.mm(po[:], w[:, dc * 128:(dc + 1) * 128], hT[:, dc, :], start=(dc == 0), stop=(dc == 7))
                        pt = pj[cnt['pj'] % 3]
                        cnt['pj'] += 1
                        P.copy('act' if oc % 2 else 'dve', pt[:], po[:])
                        P.dma('pool', projT[oc, :, tok0:tok0 + TT], pt[:])

                def outproj(ll, tok0):
                    mt = actT
                    P.dma('pool', mt[:, 0:8, :], mixT[:, :, tok0:tok0 + TT].rearrange("c p t -> p c t"))
                    for dc in range(8):
                        w = wb[cnt['wb'] % 3]
                        cnt['wb'] += 1
                        P.dma('sp', w[:, 0:1024], wout[ll, dc])
                        po = pbs[5 + cnt['o'] % 2]
                        cnt['o'] += 1
                        for kc in range(8):
                            P.mm(po[:], w[:, kc * 128:(kc + 1) * 128], mt[:, kc, :], start=(kc == 0), stop=(kc == 7))
                        P.tt('dve', xt[:, dc, :], po[:], xt[:, dc, :], ALU.add)

                l_cur = [0]
                eps6 = sb(es, "eps6", [128, 1])
                P.memset('dve', eps6[:], 1e-6)
                for tt_ in range(int(os.environ.get('KNT', NT))):
                    tok0 = tt_ * TT
                    if l == 0:
                        for s4 in range(4):
                            xk = xtok[s4 % 2]
                            P.dma('pool', xk[:], x_in[tok0 + s4 * 128: tok0 + (s4 + 1) * 128, :])
                            for half in range(2):
                                pb = pbs[5 + half]
                                for q in range(4):
                                    dc = half * 4 + q
                                    P.tr(pb[:, q * 128:(q + 1) * 128], xk[:, dc * 128:(dc + 1) * 128], ident)
                                P.copy('act' if half else 'dve', xt[:, half * 4:half * 4 + 4, s4 * 128:(s4 + 1) * 128],
                                       pb[:].rearrange("p (c t) -> p c t", t=128))
                    else:
                        P.dma('pool', xt[:], xT[:, :, tok0:tok0 + TT].rearrange("c p t -> p c t"))
                    if l > 0:
                        if 'o' in KF:
                            outproj(l - 1, tok0)
                        if 'f' in KF:
                            ffn(l - 1, 1)
                    if l < L:
                        if 'f' in KF:
                            ffn(l, 0)
                        if 'i' in KF:
                            inproj(l, tok0)
                        P.dma('pool', xT[:, :, tok0:tok0 + TT].rearrange("c p t -> p c t"), xt[:])
                    else:
                        for s4 in range(4):
                            xk = xtok[s4 % 2]
                            for half in range(2):
                                pb = pbs[5 + half]
                                for q in range(4):
                                    dc = half * 4 + q
                                    P.tr(pb[:, q * 128:(q + 1) * 128], xt[:, dc, s4 * 128:(s4 + 1) * 128], ident)
                                P.copy('act' if half else 'dve', xk[:, half * 512:(half + 1) * 512], pb[:])
                            final_ops.append(P.dma('pool', out[tok0 + s4 * 128: tok0 + (s4 + 1) * 128, :], xk[:]))
            P.barrier()

        def run_conv(l):
            with contextlib.ExitStack() as es:
                a_t = sb(es, "cv_a", [128, S])
                b_t = sb(es, "cv_b", [128, S])
                zp = sb(es, "cv_zp", [128, 30 + S])
                zc = [sb(es, f"cv_zc{i}", [128, S]) for i in range(2)]
                sq = sb(es, "cv_sq", [128, TT])
                mean = sb(es, "cv_mean", [128, TT])
                var = sb(es, "cv_var", [128, TT])
                yt = [sb(es, f"cv_y{i}", [128, TT]) for i in range(2)]
                eps5 = sb(es, "eps5", [128, 1])
                P.memset('dve', eps5[:], 1e-5)
                P.memset('dve', zp[:, 0:30], 0.0)
                for ch in range(2):
                    P.dma('pool', a_t[:], projT[19 + ch, :, :])
                    P.dma('pool', b_t[:], projT[21 + ch, :, :])
                    for h in range(4):
                        sl = slice(h * 1024, (h + 1) * 1024)
                        P.act(b_t[:, sl], b_t[:, sl], AF.Sigmoid)
                    P.tt(os.environ.get('KCE', 'pool'), zp[:, 30:30 + S], a_t[:], b_t[:], ALU.mult)
                    if 'b' not in os.environ.get('KC', 'abc'):
                        continue
                    wc = 63 + ch * 31
                    acc = zc[ch]
                    P.ts('dve', acc[:], zp[:, 0:S], pcol(l, wc), ALU.mult, pcol(l, 57 + ch), ALU.add)
                    for j in range(1, 31):
                        P.stt('dve', acc[:], zp[:, j:j + S], pcol(l, wc + j), acc[:], ALU.mult, ALU.add)
                for tt_ in range(NT if 'c' in os.environ.get('KC', 'abc') else 0):
                    sl = slice(tt_ * TT, (tt_ + 1) * TT)
                    for ch in range(2):
                        P.mm(pbs[0][:], ones, zc[ch][:, sl], start=(ch == 0), stop=(ch == 1))
                    for ch in range(2):
                        P.act(sq[:], zc[ch][:, sl], AF.Square)
                        P.mm(pbs[1][:], ones, sq[:], start=(ch == 0), stop=(ch == 1))
                    P.ts('dve', mean[:], pbs[0][:], 1.0 / 256, ALU.mult)
                    P.tt('dve', var[:], mean[:], mean[:], ALU.mult)
                    P.stt('dve', var[:], pbs[1][:], 1.0 / 256, var[:], ALU.mult, ALU.subtract)
                    P.act(var[:], var[:], AF.Sqrt, bias=eps5[:, 0:1], scale=1.0)
                    P.recip(var[:], var[:])
                    for ch in range(2):
                        y = yt[ch]
                        P.tt('dve', y[:], zc[ch][:, sl], mean[:], ALU.subtract)
                        P.tt('dve', y[:], y[:], var[:], ALU.mult)
                        P.act(y[:], y[:], AF.Silu, bias=pcol(l, 61 + ch), scale=pcol(l, 59 + ch))
                        P.dma('pool', mixT[6 + ch, :, sl], y[:])
            P.barrier()

        def run_attn(l):
            with contextlib.ExitStack() as es:
                q = sb(es, "at_q", [128, S])
                k = sb(es, "at_k", [128, S])
                v = sb(es, "at_v", [128, S])
                acc = [sb(es, f"at_acc{i}", [128, S]) for i in range(2)]
                vaug = [sb(es, f"at_va{i}", [128, 32, 128], fine=128) for i in range(2)]
                ab = sb(es, "at_bias", [128, 18 * 256], fine=256)
                et = [sb(es, f"at_e{i}", [128, 256]) for i in range(3)]
                sq = sb(es, "at_sq", [128, TT])
                rs = sb(es, "at_rs", [128, TT])
                gq = sb(es, "at_gq", [128, 1])
                eps6 = sb(es, "at_eps", [128, 1])
                P.memset('dve', eps6[:], 1e-6)
                P.dma('sp', ab[:], abias_d)
                P.ts('dve', gq[:], pcol(l, 55), 0.125, ALU.mult)
                P.memset('pool', vaug[0][:, :, 64:128], 1.0)
                P.memset('pool', vaug[1][:, :, 0:64], 1.0)
                ne = 0
                for c in range(int(os.environ.get('KACH', 3))):
                    P.dma('pool', q[:], projT[10 + c, :, :])
                    P.dma('pool', k[:], projT[13 + c, :, :])
                    P.dma('pool', v[:], projT[16 + c, :, :])
                    for (t_, gcol) in ((q, gq[:, 0:1]), (k, pcol(l, 56))):
                        for tt_ in range(NT):
                            sl = slice(tt_ * TT, (tt_ + 1) * TT)
                            P.act(sq[:], t_[:, sl], AF.Square)
                            P.mm(pbs[0][:], bd64, sq[:])
                            P.act(rs[:], pbs[0][:], AF.Sqrt, bias=eps6[:, 0:1], scale=1.0 / 64)
                            P.recip(rs[:], rs[:])
                            P.stt('dve', t_[:, sl], t_[:, sl], gcol, rs[:], ALU.mult, ALU.mult)
                    P.memset('pool', acc[0][:], 0.0)
                    P.memset('pool', acc[1][:], 0.0)
                    for pi in [int(ch_) for ch_ in os.environ.get('KAP', '012')]:
                        win_, d = PATTERNS[pi]
                        nb = 32 // d
                        Lsub = S // d
                        if os.environ.get('KAB'):
                            P.barrier()
                        for cls in range(d):
                            for kb in range(nb):
                                b = cls * nb + kb
                                st = cls + d * kb * 128
                                pb = pbs[1 + b % 2]
                                P.tr(pb[:, 0:128], v[:, st: st + d * 127 + 1: d] if d > 1 else v[:, st:st + 128], ident)
                                ce = 'act' if b % 2 else 'dve'
                                P.copy(ce, vaug[0][:, b, 0:64], pb[:, 0:64])
                                P.copy(ce, vaug[1][:, b, 64:128], pb[:, 64:128])
                        for hh in range(2):
                            h = 2 * c + hh
                            hp = slice(hh * 64, hh * 64 + 64)
                            for cls in range(d):
                                for kb in range(nb):
                                    b = cls * nb + kb
                                    nq = min(256, Lsub - kb * 128)
                                    ks = cls + d * kb * 128
                                    kap = k[hp, ks: ks + d * 127 + 1: d] if d > 1 else k[hp, ks:ks + 128]
                                    qap = q[hp, ks: ks + d * (nq - 1) + 1: d] if d > 1 else q[hp, ks:ks + nq]
                                    ps_ = pbs[3 + ne % 2]
                                    po_ = pbs[5 + ne % 2]
                                    e_ = et[ne % 3]
                                    ne += 1
                                    P.mm(ps_[:, 0:nq], kap, qap, start=True, stop=False)
                                    bi = (h * 3 + pi) * 256
                                    P.mm(ps_[:, 0:nq], ident, ab[:, bi:bi + nq], start=False, stop=True)
                                    P.act(e_[:, 0:nq], ps_[:, 0:nq], AF.Exp)
                                    P.mm(po_[:, 0:nq], vaug[hh][:, b, :], e_[:, 0:nq])
                                    aap = acc[hh][:, ks: ks + d * (nq - 1) + 1: d] if d > 1 else acc[hh][:, ks:ks + nq]
                                    P.tt('dve', aap, aap, po_[:, 0:nq], ALU.add)
                    for h4 in range(4):
                        sl = slice(h4 * 1024, (h4 + 1) * 1024)
                        P.copy('dve', q[0:64, sl], acc[0][64:128, sl])
                        P.recip(q[0:64, sl], q[0:64, sl])
                        P.tt('dve', q[0:64, sl], acc[0][0:64, sl], q[0:64, sl], ALU.mult)
                        P.copy('act', q[64:128, sl], acc[1][0:64, sl])
                        P.recip(q[64:128, sl], q[64:128, sl])
                        P.tt('dve', q[64:128, sl], acc[1][64:128, sl], q[64:128, sl], ALU.mult)
                    P.dma('pool', mixT[3 + c, :, :], q[:])
            P.barrier()

        def run_rwkv(l):
            SEG = 512
            NSEG = S // SEG
            NCH = SEG // 64
            with contextlib.ExitStack() as es:
                lora = sb(es, "rw_lora", [128, 384])
                P.dma('sp', lora[:], lora_d[l])
                eps12 = sb(es, "rw_e12", [128, 1])
                P.memset('dve', eps12[:], 1e-12)
                epsgn = sb(es, "rw_egn", [128, 1])
                P.memset('dve', epsgn[:], 64e-5)
                praw = sb(es, "rw_praw", [128, 1 + SEG])
                x9 = sb(es, "rw_x9", [128, SEG])
                tmp = sb(es, "rw_tmp", [128, SEG])
                tmp2 = sb(es, "rw_tmp2", [128, SEG])
                pr = []
                for c in range(3):
                    d_ = {}
                    for nm in ('r', 'k', 'v', 'a', 'kk', 'lw', 'cum', 'g', 'bonus', 'y', 'wc'):
                        d_[nm] = sb(es, f"rw_{nm}{c}", [128, SEG])
                    for nm in ('at', 'rt', 'bt', 'kt', 'vb'):
                        d_[nm] = sb(es, f"rw_{nm}bd{c}", [128, NCH, 128], fine=128)
                        P.memset('pool', d_[nm][:], 0.0)
                    d_['ST'] = [sb(es, f"rw_ST{c}_{i}", [128, 128]) for i in range(2)]
                    P.memset('dve', d_['ST'][0][:], 0.0)
                    for nm in ('N', 'NT', 'N2', 'N2T', 'Pm', 'Pm2', 'Aak', 'Arb', 'Ark', 'bT', 'kT', 'Vb', 'XT', 'Ub'):
                        d_[nm] = sb(es, f"rw_{nm}{c}", [128, 128])
                    d_['wend'] = sb(es, f"rw_wend{c}", [128, NCH])
                    pr.append(d_)
                stp = [0, 0, 0]

                def shift_load(dst, ch, tok0, mucol):
                    if tok0 == 0:
                        P.memset('dve', praw[:, 0:1], 0.0)
                        P.dma('pool', praw[:, 1:1 + SEG], projT[ch, :, 0:SEG])
                    else:
                        P.dma('pool', praw[:], projT[ch, :, tok0 - 1:tok0 + SEG])
                    P.tt('dve', tmp[:], praw[:, 0:SEG], praw[:, 1:1 + SEG], ALU.subtract)
                    P.stt('dve', dst, tmp[:], mucol, praw[:, 1:1 + SEG], ALU.mult, ALU.add)

                for seg in range(int(os.environ.get('KSEG', NSEG))):
                    tok0 = seg * SEG
                    shift_load(x9[:], 9, tok0, pcol(l, 24 + 9))
                    P.act(x9[0:32, :], x9[0:32, :], AF.Tanh)
                    P.act(x9[64:128, :], x9[64:128, :], AF.Sigmoid)
                    for c in range(3 if 'b' in os.environ.get('KRW', 'abcd') else 0):
                        d_ = pr[c]
                        cs = slice(c * 128, (c + 1) * 128)
                        shift_load(d_['r'][:], c, tok0, pcol(l, 24 + c))
                        shift_load(d_['k'][:], 3 + c, tok0, pcol(l, 24 + 3 + c))
                        shift_load(d_['v'][:], 6 + c, tok0, pcol(l, 24 + 6 + c))
                        P.mm(pbs[0][:], lora[0:32, cs], x9[0:32, :])
                        P.act(d_['lw'][:], pbs[0][:], AF.Sigmoid, bias=pcol(l, 34 + c), scale=1.0)
                        P.mm(pbs[1][:], lora[32:64, cs], x9[32:64, :])
                        P.act(d_['a'][:], pbs[1][:], AF.Sigmoid, bias=pcol(l, 37 + c), scale=1.0)
                        P.mm(pbs[2][:], lora[64:128, cs], x9[64:128, :])
                        P.copy('act', d_['g'][:], pbs[2][:])
                        P.ts('dve', d_['kk'][:], d_['k'][:], pcol(l, 40 + c), ALU.mult)
                        P.act(tmp2[:], d_['kk'][:], AF.Square)
                        P.mm(pbs[0][:], bd64, tmp2[:])
                        P.act(tmp2[:], pbs[0][:], AF.Sqrt, bias=eps12[:, 0:1], scale=1.0)
                        P.recip(tmp2[:], tmp2[:])
                        P.tt('dve', d_['kk'][:], d_['kk'][:], tmp2[:], ALU.mult)
                        P.ts('dve', tmp2[:], d_['a'][:], -1.0, ALU.add, pcol(l, 43 + c), ALU.mult)
                        P.stt('dve', d_['k'][:], tmp2[:], 1.0, d_['k'][:], ALU.add, ALU.mult)
                        P.tt('pool', tmp2[:], d_['r'][:], d_['k'][:], ALU.mult)
                        P.ts('pool', tmp2[:], tmp2[:], pcol(l, 46 + c), ALU.mult)
                        P.mm(pbs[1][:], bd64, tmp2[:])
                        P.tt('dve', d_['bonus'][:], pbs[1][:], d_['v'][:], ALU.mult)
                        P.ts('dve', d_['lw'][:], d_['lw'][:], -EXPM05, ALU.mult)
                        P.scan(d_['cum'][:], rmask, d_['lw'][:], 0.0, ALU.mult, ALU.add)
                        P.act(d_['wc'][:], d_['cum'][:], AF.Exp)
                        P.copy('dve', d_['wend'][:], d_['wc'][:].rearrange("p (c t) -> p c t", t=64)[:, :, 63])
                        P.tt('dve', tmp2[:], d_['cum'][:], d_['lw'][:], ALU.subtract)
                        P.act(tmp2[:], tmp2[:], AF.Exp)
                        P.tt('dve', tmp2[:], tmp2[:], d_['kk'][:], ALU.mult)
                        P.act(tmp[:], d_['cum'][:], AF.Exp, scale=-1.0)
                        P.tt('pool', d_['y'][:], d_['kk'][:], d_['a'][:], ALU.mult)
                        for hh in range(2):
                            hp = slice(hh * 64, hh * 64 + 64)
                            cp = slice(hh * 64, hh * 64 + 64)

                            def v3(t):
                                return t[hp, :].rearrange("p (c t) -> p c t", t=64)
                            P.ts('dve', d_['at'][hp, :, cp], v3(tmp2), -1.0, ALU.mult)
                            P.tt('dve', d_['rt'][hp, :, cp], v3(d_['r']), v3(d_['wc']), ALU.mult)
                            P.tt('dve', d_['bt'][hp, :, cp], v3(d_['y']), v3(tmp), ALU.mult)
                            P.tt('dve', d_['kt'][hp, :, cp], v3(d_['k']), v3(tmp), ALU.mult)
                            P.copy('pool', d_['vb'][hp, :, cp], v3(d_['v']))
                    def chunk_gen(n, c):
                        d_ = pr[c]
                        at, rt, bt, kt, vb = (d_[x][:, n, :] for x in ('at', 'rt', 'bt', 'kt', 'vb'))
                        pq = [pbs[c * 2][:, 0:128], pbs[c * 2][:, 128:256], pbs[c * 2][:, 256:384], pbs[c * 2][:, 384:512],
                              pbs[c * 2 + 1][:, 0:128], pbs[c * 2 + 1][:, 128:256], pbs[c * 2 + 1][:, 256:384], pbs[c * 2 + 1][:, 384:512]]
                        P.mm(pq[0], bt, at)
                        P.mm(pq[1], at, bt)
                        P.mm(pq[2], kt, at)
                        P.mm(pq[3], bt, rt)
                        P.mm(pq[4], kt, rt)
                        P.tr(pq[5], bt, ident)
                        P.tr(pq[6], kt, ident)
                        P.tr(pq[7], vb, ident)
                        yield
                        P.tt('dve', d_['N'][:], pq[0], msu, ALU.mult)
                        P.tt('dve', d_['NT'][:], pq[1], msl, ALU.mult)
                        P.tt('dve', d_['Aak'][:], pq[2], msu, ALU.mult)
                        P.tt('dve', d_['Arb'][:], pq[3], mu_, ALU.mult)
                        P.tt('dve', d_['Ark'][:], pq[4], mu_, ALU.mult)
                        P.copy('act', d_['bT'][:], pq[5])
                        P.copy('act', d_['kT'][:], pq[6])
                        P.copy('act', d_['Vb'][:], pq[7])
                        P.tt('dve', d_['Pm'][:], d_['N'][:], ident, ALU.add)
                        yield
                        cur, curT, nxt, nxtT = d_['N'], d_['NT'], d_['N2'], d_['N2T']
                        Pc, Pn = d_['Pm'], d_['Pm2']
                        for lev in range(5):
                            P.mm(pq[0], cur[:], curT[:])
                            if lev < 4:
                                P.mm(pq[4], curT[:], cur[:])
                            yield
                            P.copy('act', nxtT[:], pq[0])
                            if lev < 4:
                                P.copy('dve', nxt[:], pq[4])
                            yield
                            P.mm(pq[5], nxtT[:], Pc[:])
                            yield
                            P.tt('dve', Pn[:], pq[5], Pc[:], ALU.add)
                            yield
                            cur, curT, nxt, nxtT = nxt, nxtT, cur, curT
                            Pc, Pn = Pn, Pc
                        Minv = Pc
                        ST0 = d_['ST'][stp[c] % 2]
                        ST1 = d_['ST'][(stp[c] + 1) % 2]
                        stp[c] += 1
                        P.mm(pq[3], at, ST0[:], start=True, stop=False)
                        P.mm(pq[3], d_['Aak'][:], d_['Vb'][:], start=False, stop=True)
                        yield
                        P.copy('act', d_['XT'][:], pq[3])
                        yield
                        P.mm(pq[4], Minv[:], d_['XT'][:])
                        yield
                        P.copy('dve', d_['Ub'][:], pq[4])
                        yield
                        P.mm(pq[6], ident, ST0[:], start=True, stop=False)
                        P.mm(pq[6], d_['bT'][:], d_['Ub'][:], start=False, stop=False)
                        P.mm(pq[6], d_['kT'][:], d_['Vb'][:], start=False, stop=True)
                        P.mm(pq[1], ST0[:], rt, start=True, stop=False)
                        P.mm(pq[1], d_['Ub'][:], d_['Arb'][:], start=False, stop=False)
                        P.mm(pq[1], d_['Vb'][:], d_['Ark'][:], start=False, stop=True)
                        yield
                        P.ts('dve', ST1[:], pq[6], d_['wend'][:, n:n + 1], ALU.mult)
                        P.copy('act', d_['y'][0:64, n * 64:(n + 1) * 64], pq[1][0:64, 0:64])
                        P.copy('act', d_['y'][64:128, n * 64:(n + 1) * 64], pq[1][64:128, 64:128])
                        yield

                    for n in range(int(os.environ.get('KNCH', NCH)) if 'c' in os.environ.get('KRW', 'abcd') else 0):
                        gens = [chunk_gen(n, c) for c in range(int(os.environ.get('KPAIRS', 3)))]
                        alive = True
                        ny = 0
                        while alive and ny < int(os.environ.get('KGEN', 1000)):
                            alive = False
                            ny += 1
                            for g_ in gens:
                                try:
                                    next(g_)
                                    alive = True
                                except StopIteration:
                                    pass
                    for c in range(3 if 'd' in os.environ.get('KRW', 'abcd') else 0):
                        d_ = pr[c]
                        P.mm(pbs[6][:], bd64, d_['y'][:])
                        P.ts('dve', tmp[:], pbs[6][:], 1.0 / 64, ALU.mult)
                        P.tt('dve', d_['y'][:], d_['y'][:], tmp[:], ALU.subtract)
                        P.act(tmp2[:], d_['y'][:], AF.Square)
                        P.mm(pbs[7][:], bd64, tmp2[:])
                        P.act(tmp2[:], pbs[7][:], AF.Sqrt, bias=epsgn[:, 0:1], scale=1.0 / 64)
                        P.recip(tmp2[:], tmp2[:])
                        P.tt('dve', d_['y'][:], d_['y'][:], tmp2[:], ALU.mult)
                        P.ts('dve', d_['y'][:], d_['y'][:], pcol(l, 49 + c), ALU.mult, pcol(l, 52 + c), ALU.add)
                        P.tt('dve', d_['y'][:], d_['y'][:], d_['bonus'][:], ALU.add)
                        P.tt('dve', d_['wc'][:], d_['y'][:], d_['g'][:], ALU.mult)
                        P.dma('pool', mixT[c, :, tok0:tok0 + SEG], d_['wc'][:])
            P.barrier()

        P.barrier()
        for l in range(L + 1):
            if 'pass' in phases:
                run_pass(l)
            if l < L:
                if 'conv' in phases:
                    run_conv(l)
                if 'attn' in phases:
                    run_attn(l)
                if 'rwkv' in phases:
                    run_rwkv(l)
        st = P.emit(final_ops)
    return nc, st


def _alibi_slopes(n):
    def pow2(m):
        start = 2.0 ** (-8.0 / m)
        return [start ** (i + 1) for i in range(m)]
    if math.log2(n).is_integer():
        return pow2(n)
    c = 2 ** int(math.floor(math.log2(n)))
    return pow2(c) + pow2(2 * c)[0::2][: n - c]


def _consts():
    cst = np.zeros((128, NCST), np.float32)
    cst[:, C_ID:C_ID + 128] = np.eye(128, dtype=np.float32)
    cst[:, C_ONES:C_ONES + 128] = 1.0
    i = np.arange(128)
    same = (i[:, None] // 64) == (i[None, :] // 64)
    cst[:, C_BD:C_BD + 128] = same
    loc = i % 64
    cst[:, C_MSU:C_MSU + 128] = same & (loc[:, None] < loc[None, :])
    cst[:, C_MSL:C_MSL + 128] = same & (loc[:, None] > loc[None, :])
    cst[:, C_MU:C_MU + 128] = same & (loc[:, None] <= loc[None, :])
    cst[:, C_RM:C_RM + 512] = (np.arange(512) % 64 != 0).astype(np.float32)[None, :]
    slopes = _alibi_slopes(6)
    ab = np.zeros((128, 18, 256), np.float32)
    kk = np.arange(128)[:, None]
    qq = np.arange(256)[None, :]
    dist = qq - kk
    for h in range(6):
        for pi, (w, d) in enumerate(PATTERNS):
            valid = (dist >= 0) & (dist <= w // d)
            ab[:, h * 3 + pi, :] = np.where(valid, -slopes[h] * (dist * d).astype(np.float32), -30000.0)
    return cst, ab.reshape(128, 18 * 256)


def _cols(v):
    v = np.asarray(v, np.float32).reshape(-1, 128)
    return v.T


def _prep(inp, L):
    f = lambda a: np.ascontiguousarray(np.asarray(a, np.float32))
    m = {}
    for i, nm in ((1, 'ffn1'), (2, 'ffn2')):
        g = f(inp[f'{nm}_w_gate'])[:L].reshape(L, 8, 128, NFC, 128).transpose(0, 3, 2, 1, 4)
        u = f(inp[f'{nm}_w_up'])[:L].reshape(L, 8, 128, NFC, 128).transpose(0, 3, 2, 1, 4)
        m[f'wgu{i}'] = np.ascontiguousarray(np.stack([g, u], axis=3)).reshape(L, NFC, 128, 2048)
        dn = f(inp[f'{nm}_w_down'])[:L].reshape(L, NFC, 128, 8, 128).transpose(0, 3, 2, 1, 4)
        m[f'wd{i}'] = np.ascontiguousarray(dn).reshape(L, 8, 128, NFC * 128)
    m['win'] = np.ascontiguousarray(f(inp['w_in'])[:L].reshape(L, 8, 128, NOC, 128).transpose(0, 3, 2, 1, 4)).reshape(L, NOC, 128, 1024)
    m['wout'] = np.ascontiguousarray(f(inp['w_out'])[:L].reshape(L, 8, 128, 8, 128).transpose(0, 3, 2, 1, 4)).reshape(L, 8, 128, 1024)
    m['lora'] = np.ascontiguousarray(np.concatenate([f(inp['rwkv_w2'])[:L], f(inp['rwkv_a2'])[:L], f(inp['rwkv_g2'])[:L]], axis=1))
    prm = np.zeros((128, L, NP), np.float32)
    for l in range(L):
        prm[:, l, 0:8] = _cols(inp['norm_ffn1'][l])
        prm[:, l, 8:16] = _cols(inp['norm_mix'][l])
        prm[:, l, 16:24] = _cols(inp['norm_ffn2'][l])
        prm[:, l, 24:34] = _cols(inp['shift_mu'][l])
        prm[:, l, 34:37] = _cols(inp['rwkv_w0'][l])
        prm[:, l, 37:40] = _cols(inp['rwkv_a0'][l])
        prm[:, l, 40:43] = _cols(inp['rwkv_k_k'][l])
        prm[:, l, 43:46] = _cols(inp['rwkv_k_a'][l])
        prm[:, l, 46:49] = _cols(np.asarray(inp['rwkv_r_k'][l]).reshape(-1))
        prm[:, l, 49:52] = _cols(inp['rwkv_ln_w'][l])
        prm[:, l, 52:55] = _cols(inp['rwkv_ln_b'][l])
        prm[:, l, 55] = np.tile(np.asarray(inp['attn_q_norm'][l], np.float32), 2)
        prm[:, l, 56] = np.tile(np.asarray(inp['attn_k_norm'][l], np.float32), 2)
        prm[:, l, 57:59] = _cols(inp['conv_dw_b'][l])
        prm[:, l, 59:61] = _cols(inp['conv_ln_w'][l])
        prm[:, l, 61:63] = _cols(inp['conv_ln_b'][l])
        cw = np.asarray(inp['conv_dw_w'][l], np.float32)
        for ch in range(2):
            prm[:, l, 63 + ch * 31: 63 + (ch + 1) * 31] = cw[:, ch * 128:(ch + 1) * 128].T
    m['prm'] = np.ascontiguousarray(prm.reshape(128, L * NP))
    cst, ab = _consts()
    m['cst'] = cst
    m['abias'] = ab
    return m


_CACHE = {}


def kernel(**inputs):
    L = 4
    x = np.ascontiguousarray(np.asarray(inputs['x'], np.float32))
    B = x.shape[0]
    if 'nc' not in _CACHE:
        _CACHE['nc'] = build(L)[0]
    nc = _CACHE['nc']
    shared = _prep(inputs, L)
    in_maps = []
    for b in range(B):
        d = dict(shared)
        d['x'] = x[b]
        in_maps.append(d)
    res = run_bass_kernel_spmd(nc, in_maps, core_ids=list(range(B)))
    return np.stack([np.asarray(r['out'], np.float32) for r in res.results], axis=0)
```

```python
import contextlib
import math
import os
import numpy as np
import concourse.bass as bass
import concourse.mybir as mybir
from concourse.bass_utils import run_bass_kernel_spmd

F32 = mybir.dt.float32
F32R = mybir.dt.float32r
FAST = os.environ.get('KFAST', '1') == '1'


def R(ap):
    return ap
ALU = mybir.AluOpType
AF = mybir.ActivationFunctionType

S = 4096
D = 1024
DFF = 2816
NFC = 22
NOC = 23
TT = 512
NT = S // TT
NP = 128
C_ID, C_ONES, C_BD, C_MSU, C_MSL, C_MU, C_RM = 0, 128, 256, 384, 512, 640, 768
NCST = 768 + 512
PATTERNS = ((128, 1), (512, 4), (2048, 16))
EXPM05 = math.exp(-0.5)

SEM_GEN = 8000
KF = os.environ.get('KF', 'ofi')
DMA_RING = 6


class Op:
    __slots__ = ('eng', 'fn', 'deps', 'is_dma', 'needs_inc', 'tok', 'ring_wait')

    def __init__(self, eng, fn, is_dma):
        self.eng = eng
        self.fn = fn
        self.deps = []
        self.is_dma = is_dma
        self.needs_inc = is_dma
        self.tok = None
        self.ring_wait = None


class Prog:
    def __init__(self, nc):
        self.nc = nc
        self.ops = {e: [] for e in ('pe', 'act', 'dve', 'pool', 'sp')}
        self.lastw = {}
        self.readers = {}
        self.fine = {}
        self.pending = None
        self.applied = set()

    def keys(self, ap):
        name = ap.tensor.name
        g = self.fine.get(name)
        if g is None:
            return [name]
        gran, row = g
        off = ap.offset % row
        span = 1
        for st, cnt in list(ap.ap)[1:]:
            span += (cnt - 1) * st
        return [(name, i) for i in range(off // gran, (off + span - 1) // gran + 1)]

    def _add(self, eng, fn, reads, writes, is_dma):
        o = Op(eng, fn, is_dma)
        lst = self.ops[eng]
        me = (eng, len(lst))
        lst.append(o)
        reads = list(dict.fromkeys(reads))
        writes = list(dict.fromkeys(writes))
        deps = set()
        for k in reads:
            w = self.lastw.get(k)
            if w is not None:
                if self.ops[w[0]][w[1]].is_dma or is_dma or w[0] != eng or eng != 'pe':
                    deps.add(w)
            if isinstance(k, str) and k.startswith('pb'):
                for r in self.readers.get(k, ()):
                    if r[0] != eng:
                        deps.add(r)
        for k in writes:
            w = self.lastw.get(k)
            if w is not None:
                if self.ops[w[0]][w[1]].is_dma or w[0] != eng or is_dma or eng != 'pe':
                    deps.add(w)
            for r in self.readers.get(k, ()):
                if self.ops[r[0]][r[1]].is_dma or r[0] != eng or is_dma or eng != 'pe':
                    deps.add(r)
        if self.pending is not None and eng not in self.applied:
            deps.update(self.pending)
            self.applied.add(eng)
        deps.discard(me)
        o.deps = list(deps)
        for d in o.deps:
            self.ops[d[0]][d[1]].needs_inc = True
        for k in reads:
            rl = self.readers.setdefault(k, [])
            if not is_dma:
                rl[:] = [r for r in rl if r[0] != eng or self.ops[r[0]][r[1]].is_dma]
            rl.append(me)
        for k in writes:
            self.lastw[k] = me
            self.readers[k] = []
        return o

    def barrier(self):
        deps = []
        for eng, lst in self.ops.items():
            for i in range(len(lst) - 1, -1, -1):
                if not lst[i].is_dma:
                    deps.append((eng, i))
                    break
            c = 0
            for i in range(len(lst) - 1, -1, -1):
                if lst[i].is_dma:
                    deps.append((eng, i))
                    c += 1
                    if c >= DMA_RING:
                        break
        self.pending = deps
        self.applied = set()
        self.lastw.clear()
        self.readers.clear()

    def _k(self, aps):
        ks = []
        for a in aps:
            if a is not None and not isinstance(a, (int, float)) and a.tensor.name in self.track:
                ks += self.keys(a)
        return ks

    def mm(self, out, lhsT, rhs, start=True, stop=True):
        return self._add('pe', lambda e: e.matmul(out, lhsT=lhsT, rhs=rhs, start=start, stop=stop),
                         self._k([lhsT, rhs]), self._k([out]), False)

    def tr(self, out, in_, ident):
        return self._add('pe', lambda e: e.transpose(out, in_, ident), self._k([in_, ident]), self._k([out]), False)

    def act(self, out, in_, func, bias=0.0, scale=1.0):
        return self._add('act', lambda e: e.activation(out=out, in_=in_, func=func, bias=bias, scale=scale),
                         self._k([in_, bias, scale]), self._k([out]), False)

    def tt(self, eng, out, in0, in1, op):
        return self._add(eng, lambda e: e.tensor_tensor(out=out, in0=in0, in1=in1, op=op),
                         self._k([in0, in1]), self._k([out]), False)

    def ts(self, eng, out, in0, s1, op0, s2=None, op1=None):
        if op1 is None:
            fn = lambda e: e.tensor_scalar(out=out, in0=in0, scalar1=s1, scalar2=None, op0=op0)
        else:
            fn = lambda e: e.tensor_scalar(out=out, in0=in0, scalar1=s1, scalar2=s2, op0=op0, op1=op1)
        return self._add(eng, fn, self._k([in0, s1, s2]), self._k([out]), False)

    def stt(self, eng, out, in0, scalar, in1, op0, op1):
        return self._add(eng, lambda e: e.scalar_tensor_tensor(out=out, in0=in0, scalar=scalar, in1=in1, op0=op0, op1=op1),
                         self._k([in0, scalar, in1]), self._k([out]), False)

    def scan(self, out, d0, d1, init, op0, op1):
        return self._add('dve', lambda e: e.tensor_tensor_scan(out=out, data0=d0, data1=d1, initial=init, op0=op0, op1=op1),
                         self._k([d0, d1]), self._k([out]), False)

    def copy(self, eng, out, in_):
        if eng == 'act':
            return self.act(out, in_, AF.Copy)
        return self._add(eng, lambda e: e.tensor_copy(out=out, in_=in_), self._k([in_]), self._k([out]), False)

    def recip(self, out, in_):
        return self._add('dve', lambda e: e.reciprocal(out=out, in_=in_), self._k([in_]), self._k([out]), False)

    def memset(self, eng, ap, val):
        return self._add(eng, lambda e: e.memset(ap, val), [], self._k([ap]), False)

    def dma(self, eng, out, in_):
        return self._add(eng, lambda e: e.dma_start(out=out, in_=in_), self._k([in_]), self._k([out]), True)

    def emit(self, final_ops):
        nc = self.nc
        with contextlib.ExitStack() as es:
            sems = {}
            tail = []
            for eng, lst in self.ops.items():
                for i in range(len(lst) - 1, -1, -1):
                    if not lst[i].is_dma:
                        lst[i].needs_inc = True
                        tail.append(lst[i])
                        break
                c_ = 0
                for i in range(len(lst) - 1, -1, -1):
                    if lst[i].is_dma:
                        tail.append(lst[i])
                        c_ += 1
                        if c_ >= DMA_RING:
                            break
            for eng, lst in self.ops.items():
                cnt = 0
                nd = 0
                for o in lst:
                    if o.is_dma:
                        slot, rnd = nd % DMA_RING, nd // DMA_RING
                        o.tok = (f"d_{eng}_{slot}", 16 * (rnd + 1))
                        if rnd > 0:
                            o.ring_wait = (f"d_{eng}_{slot}", 16 * rnd)
                        nd += 1
                    elif o.needs_inc:
                        g = cnt // SEM_GEN
                        o.tok = (f"c_{eng}_{g}", cnt - g * SEM_GEN + 1)
                        cnt += 1
            self.maxtok = {}
            for lst in self.ops.values():
                for o in lst:
                    if o.tok:
                        self.maxtok[o.tok[0]] = max(self.maxtok.get(o.tok[0], 0), o.tok[1])
            if os.environ.get('KSIM'):
                print('MAXTOK', self.maxtok)
            for lst in self.ops.values():
                for o in lst:
                    if o.tok and o.tok[0] not in sems:
                        sems[o.tok[0]] = es.enter_context(nc.semaphore(o.tok[0]))
            engmap = {'pe': 'tensor', 'act': 'scalar', 'dve': 'vector', 'pool': 'gpsimd', 'sp': 'sync'}
            plan = {}
            stats = {}
            for eng, lst in self.ops.items():
                known = {}
                nw = 0
                pl = []
                for o in lst:
                    need = {}
                    for d in o.deps:
                        t = self.ops[d[0]][d[1]].tok
                        if need.get(t[0], 0) < t[1]:
                            need[t[0]] = t[1]
                    if o.ring_wait:
                        t = o.ring_wait
                        if need.get(t[0], 0) < t[1]:
                            need[t[0]] = t[1]
                    ws = []
                    for s_, v in need.items():
                        if known.get(s_, 0) < v:
                            ws.append((s_, v))
                            known[s_] = v
                            nw += 1
                    pl.append(ws)
                plan[eng] = pl
                stats[eng] = (len(lst), nw)
            if os.environ.get('KSIM'):
                val = {k_: 0 for k_ in sems}
                pos = {e_: 0 for e_ in self.ops}
                prog = True
                while prog:
                    prog = False
                    for e_, lst in self.ops.items():
                        while pos[e_] < len(lst):
                            i_ = pos[e_]
                            if all(val[s_] >= v for s_, v in plan[e_][i_]):
                                o = lst[i_]
                                if o.tok is not None:
                                    val[o.tok[0]] += 16 if o.is_dma else 1
                                    assert val[o.tok[0]] == o.tok[1], (e_, i_, o.tok, val[o.tok[0]])
                                pos[e_] += 1
                                prog = True
                            else:
                                break
                for e_, lst in self.ops.items():
                    if pos[e_] < len(lst):
                        print("SIM DEADLOCK", e_, pos[e_], len(lst), plan[e_][pos[e_]], {s_: val[s_] for s_, v in plan[e_][pos[e_]]})
                print("SIM done", {e_: pos[e_] for e_ in pos})
            block = es.enter_context(nc.Block())
            for eng, lst in self.ops.items():
                def body(e, eng=eng, lst=lst):
                    for o, ws in zip(lst, plan[eng]):
                        for s_, v in ws:
                            e.wait_ge(sems[s_], v)
                        ins = o.fn(e)
                        if o.tok is not None:
                            ins.then_inc(sems[o.tok[0]], 16 if o.is_dma else 1)
                    for fo in list(final_ops) + tail:
                        e.wait_ge(sems[fo.tok[0]], fo.tok[1])
                getattr(block, engmap[eng])(body)
            self.stats = stats
        return stats


def build(L, dbg=False, phases=('pass', 'conv', 'attn', 'rwkv')):
    nc = bass.Bass("TRN2", target_bir_lowering=False)
    if FAST:
        nc.dge_precook = False

    def din(name, shape, dt=F32):
        return nc.dram_tensor(name, shape, dt, kind="ExternalInput").ap()

    x_in = din("x", [S, D])
    WDT = F32R if FAST else F32
    wgu = [din(f"wgu{i}", [L, NFC, 128, 2048], WDT) for i in (1, 2)]
    wd = [din(f"wd{i}", [L, 8, 128, NFC * 128], WDT) for i in (1, 2)]
    win = din("win", [L, NOC, 128, 1024], WDT)
    wout = din("wout", [L, 8, 128, 1024], WDT)
    lora_d = din("lora", [L, 128, 384])
    prm_d = din("prm", [128, L * NP])
    cst_d = din("cst", [128, NCST])
    abias_d = din("abias", [128, 18 * 256])
    out = nc.dram_tensor("out", [S, D], F32, kind="ExternalOutput").ap()
    skind = "ExternalOutput" if dbg else "Internal"
    xT = nc.dram_tensor("xT", [8, 128, S], F32, kind=skind).ap()
    projT = nc.dram_tensor("projT", [NOC, 128, S], F32, kind=skind).ap()
    mixT = nc.dram_tensor("mixT", [8, 128, S], F32R if FAST else F32, kind=skind).ap()

    P = Prog(nc)
    P.track = set()
    final_ops = []

    with contextlib.ExitStack() as gs:
        uid = [0]

        def sb(es, name, shape, fine=None, dt=F32):
            uid[0] += 1
            name = f"{name}_u{uid[0]}"
            t = es.enter_context(nc.sbuf_tensor(name, shape, dt))
            P.track.add(name)
            if fine:
                row = 1
                for s_ in shape[1:]:
                    row *= s_
                P.fine[name] = (fine, row)
            return t

        cst = sb(gs, "cst_s", [128, NCST], fine=128)
        prm = sb(gs, "prm_s", [128, L * NP])
        pbs = []
        for i in range(8):
            t = gs.enter_context(nc.psum_tensor(f"pb{i}", [128, 512], F32))
            P.track.add(f"pb{i}")
            pbs.append(t)
        P.dma('sp', cst[:], cst_d)
        P.dma('sp', prm[:], prm_d)
        ident = cst[:, C_ID:C_ID + 128]
        ones = cst[:, C_ONES:C_ONES + 128]
        bd64 = cst[:, C_BD:C_BD + 128]
        msu = cst[:, C_MSU:C_MSU + 128]
        msl = cst[:, C_MSL:C_MSL + 128]
        mu_ = cst[:, C_MU:C_MU + 128]
        rmask = cst[:, C_RM:C_RM + 512]

        def pcol(l, c, n=1):
            return prm[:, l * NP + c: l * NP + c + n]

        def run_pass(l):
            with contextlib.ExitStack() as es:
                xt = sb(es, "xt", [128, 8, TT], fine=TT)
                hT = sb(es, "hT", [128, 8, TT], dt=WDT)
                actT = sb(es, "actT", [128, NFC, TT], fine=TT, dt=WDT)
                sq = sb(es, "sq", [128, 8 * TT])
                rstd = sb(es, "rstd", [128, TT])
                sg = [sb(es, f"sg{i}", [128, TT]) for i in range(2)]
                wb = [sb(es, f"wb{i}", [128, 2048], dt=WDT) for i in range(3)]
                wdb = [sb(es, f"wdb{i}", [128, NFC * 128], dt=WDT) for i in range(2)]
                wb1 = [sb(es, f"wb1_{i}", [128, 1024], dt=WDT) for i in range(3)]
                pj = [sb(es, f"pj{i}", [128, TT]) for i in range(3)]
                xtok = [sb(es, f"xtok{i}", [128, D]) for i in range(2)]
                cnt = {'wb': 0, 'wd': 0, 'pj': 0, 'g': 0, 'o': 0}

                def norm_h(gcol0):
                    P.act(sq[:], xt[:].rearrange("p c t -> p (c t)"), AF.Square)
                    for dc in range(8):
                        P.mm(pbs[0][:], ones, sq[:, dc * TT:(dc + 1) * TT], start=(dc == 0), stop=(dc == 7))
                    P.act(rstd[:], pbs[0][:], AF.Sqrt, bias=eps6[:, 0:1], scale=1.0 / D)
                    P.recip(rstd[:], rstd[:])
                    for dc in range(8):
                        P.stt('dve', R(hT[:, dc, :]), xt[:, dc, :], pcol(l_cur[0], gcol0 + dc), rstd[:], ALU.mult, ALU.mult)

                def ffn(ll, which):
                    l_cur[0] = ll
                    norm_h(0 if which == 0 else 16)
                    for fc in range(NFC):
                        w = wb[cnt['wb'] % 3]
                        cnt['wb'] += 1
                        P.dma('sp', R(w[:]), wgu[which][ll, fc])
                        pg = pbs[1 + cnt['g'] % 2]
                        pu = pbs[3 + cnt['g'] % 2]
                        sgt = sg[cnt['g'] % 2]
                        cnt['g'] += 1
                        for dc in range(8):
                            P.mm(pg[:], R(w[:, dc * 128:(dc + 1) * 128]), R(hT[:, dc, :]), start=(dc == 0), stop=(dc == 7))
                        for dc in range(8):
                            P.mm(pu[:], R(w[:, 1024 + dc * 128:1024 + (dc + 1) * 128]), R(hT[:, dc, :]), start=(dc == 0), stop=(dc == 7))
                        P.act(sgt[:], pg[:], AF.Silu)
                        P.tt('dve', R(actT[:, fc, :]), sgt[:], pu[:], ALU.mult)
                    for dc in range(8):
                        w = wdb[cnt['wd'] % 2]
                        cnt['wd'] += 1
                        P.dma('sp', R(w[:]), wd[which][ll, dc])
                        po = pbs[5 + cnt['o'] % 2]
                        cnt['o'] += 1
                        for fc in range(NFC):
                            P.mm(po[:], R(w[:, fc * 128:(fc + 1) * 128]), R(actT[:, fc, :]), start=(fc == 0), stop=(fc == NFC - 1))
                        P.stt('dve', xt[:, dc, :], po[:], 0.5, xt[:, dc, :], ALU.mult, ALU.add)

                def inproj(ll, tok0):
                    l_cur[0] = ll
                    norm_h(8)
                    for oc in range(NOC):
                        w = wb1[cnt['wb'] % 3]
                        cnt['wb'] += 1
                        P.dma('sp', w[:], win[ll, oc])
                        po = pbs[5 + cnt['o'] % 2]
                        cnt['o'] += 1
                        for dc in range(8):
                            P.mm(po[:], R(w[:, dc * 128:(dc + 1) * 128]), R(hT[:, dc, :]), start=(dc == 0), stop=(dc == 7))
                        pt = pj[cnt['pj'] % 3]
                        cnt['pj'] += 1
                        P.copy('act' if oc % 2 else 'dve', pt[:], po[:])
                        P.dma('pool', projT[oc, :, tok0:tok0 + TT], pt[:])

                def outproj(ll, tok0):
                    mt = actT
                    P.dma('pool', R(mt[:, 0:8, :]), R(mixT[:, :, tok0:tok0 + TT]).rearrange("c p t -> p c t"))
                    for dc in range(8):
                        w = wb1[cnt['wb'] % 3]
                        cnt['wb'] += 1
                        P.dma('sp', w[:], wout[ll, dc])
                        po = pbs[5 + cnt['o'] % 2]
                        cnt['o'] += 1
                        for kc in range(8):
                            P.mm(po[:], R(w[:, kc * 128:(kc + 1) * 128]), R(mt[:, kc, :]), start=(kc == 0), stop=(kc == 7))
                        P.tt('dve', xt[:, dc, :], po[:], xt[:, dc, :], ALU.add)

                l_cur = [0]
                eps6 = sb(es, "eps6", [128, 1])
                P.memset('dve', eps6[:], 1e-6)
                for tt_ in range(int(os.environ.get('KNT', NT))):
                    tok0 = tt_ * TT
                    if l == 0:
                        for s4 in range(4):
                            xk = xtok[s4 % 2]
                            P.dma('pool', xk[:], x_in[tok0 + s4 * 128: tok0 + (s4 + 1) * 128, :])
                            for half in range(2):
                                pb = pbs[5 + half]
                                for q in range(4):
                                    dc = half * 4 + q
                                    P.tr(pb[:, q * 128:(q + 1) * 128], xk[:, dc * 128:(dc + 1) * 128], ident)
                                P.copy('act' if half else 'dve', xt[:, half * 4:half * 4 + 4, s4 * 128:(s4 + 1) * 128],
                                       pb[:].rearrange("p (c t) -> p c t", t=128))
                    else:
                        P.dma('pool', xt[:], xT[:, :, tok0:tok0 + TT].rearrange("c p t -> p c t"))
                    if l > 0:
                        if 'o' in KF:
                            outproj(l - 1, tok0)
                        if 'f' in KF:
                            ffn(l - 1, 1)
                    if l < L:
                        if 'f' in KF:
                            ffn(l, 0)
                        if 'i' in KF:
                            inproj(l, tok0)
                        P.dma('pool', xT[:, :, tok0:tok0 + TT].rearrange("c p t -> p c t"), xt[:])
                    else:
                        for s4 in range(4):
                            xk = xtok[s4 % 2]
                            for half in range(2):
                                pb = pbs[5 + half]
                                for q in range(4):
                                    dc = half * 4 + q
                                    P.tr(pb[:, q * 128:(q + 1) * 128], xt[:, dc, s4 * 128:(s4 + 1) * 128], ident)
                                P.copy('act' if half else 'dve', xk[:, half * 512:(half + 1) * 512], pb[:])
                            final_ops.append(P.dma('pool', out[tok0 + s4 * 128: tok0 + (s4 + 1) * 128, :], xk[:]))
            P.barrier()

        def run_conv(l):
            with contextlib.ExitStack() as es:
                a_t = sb(es, "cv_a", [128, S])
                b_t = sb(es, "cv_b", [128, S])
                zp = sb(es, "cv_zp", [128, 30 + S])
                zc = [sb(es, f"cv_zc{i}", [128, S]) for i in range(2)]
                sq = sb(es, "cv_sq", [128, TT])
                mean = sb(es, "cv_mean", [128, TT])
                var = sb(es, "cv_var", [128, TT])
                yt = [sb(es, f"cv_y{i}", [128, TT]) for i in range(2)]
                eps5 = sb(es, "eps5", [128, 1])
                P.memset('dve', eps5[:], 1e-5)
                P.memset('dve', zp[:, 0:30], 0.0)
                for ch in range(2):
                    P.dma('pool', a_t[:], projT[19 + ch, :, :])
                    P.dma('pool', b_t[:], projT[21 + ch, :, :])
                    for h in range(4):
                        sl = slice(h * 1024, (h + 1) * 1024)
                        P.act(b_t[:, sl], b_t[:, sl], AF.Sigmoid)
                    P.tt(os.environ.get('KCE', 'pool'), zp[:, 30:30 + S], a_t[:], b_t[:], ALU.mult)
                    if 'b' not in os.environ.get('KC', 'abc'):
                        continue
                    wc = 63 + ch * 31
                    acc = zc[ch]
                    P.ts('dve', acc[:], zp[:, 0:S], pcol(l, wc), ALU.mult, pcol(l, 57 + ch), ALU.add)
                    for j in range(1, 31):
                        P.stt('dve', acc[:], zp[:, j:j + S], pcol(l, wc + j), acc[:], ALU.mult, ALU.add)
                for tt_ in range(NT if 'c' in os.environ.get('KC', 'abc') else 0):
                    sl = slice(tt_ * TT, (tt_ + 1) * TT)
                    for ch in range(2):
                        P.mm(pbs[0][:], ones, zc[ch][:, sl], start=(ch == 0), stop=(ch == 1))
                    for ch in range(2):
                        P.act(sq[:], zc[ch][:, sl], AF.Square)
                        P.mm(pbs[1][:], ones, sq[:], start=(ch == 0), stop=(ch == 1))
                    P.ts('dve', mean[:], pbs[0][:], 1.0 / 256, ALU.mult)
                    P.tt('dve', var[:], mean[:], mean[:], ALU.mult)
                    P.stt('dve', var[:], pbs[1][:], 1.0 / 256, var[:], ALU.mult, ALU.subtract)
                    P.act(var[:], var[:], AF.Sqrt, bias=eps5[:, 0:1], scale=1.0)
                    P.recip(var[:], var[:])
                    for ch in range(2):
                        y = yt[ch]
                        P.tt('dve', y[:], zc[ch][:, sl], mean[:], ALU.subtract)
                        P.tt('dve', y[:], y[:], var[:], ALU.mult)
                        P.act(y[:], y[:], AF.Silu, bias=pcol(l, 61 + ch), scale=pcol(l, 59 + ch))
                        P.dma('pool', mixT[6 + ch, :, sl], y[:])
            P.barrier()

        def run_attn(l):
            with contextlib.ExitStack() as es:
                q = sb(es, "at_q", [128, S])
                k = sb(es, "at_k", [128, S])
                v = sb(es, "at_v", [128, S])
                acc = [sb(es, f"at_acc{i}", [128, S]) for i in range(2)]
                vaug = [sb(es, f"at_va{i}", [128, 32, 128], fine=128) for i in range(2)]
                ab = sb(es, "at_bias", [128, 18 * 256], fine=256)
                et = [sb(es, f"at_e{i}", [128, 256]) for i in range(3)]
                sq = sb(es, "at_sq", [128, TT])
                rs = sb(es, "at_rs", [128, TT])
                gq = sb(es, "at_gq", [128, 1])
                eps6 = sb(es, "at_eps", [128, 1])
                P.memset('dve', eps6[:], 1e-6)
                P.dma('sp', ab[:], abias_d)
                P.ts('dve', gq[:], pcol(l, 55), 0.125, ALU.mult)
                P.memset('pool', vaug[0][:, :, 64:128], 1.0)
                P.memset('pool', vaug[1][:, :, 0:64], 1.0)
                ne = 0
                for c in range(int(os.environ.get('KACH', 3))):
                    P.dma('pool', q[:], projT[10 + c, :, :])
                    P.dma('pool', k[:], projT[13 + c, :, :])
                    P.dma('pool', v[:], projT[16 + c, :, :])
                    for (t_, gcol) in ((q, gq[:, 0:1]), (k, pcol(l, 56))):
                        for tt_ in range(NT):
                            sl = slice(tt_ * TT, (tt_ + 1) * TT)
                            P.act(sq[:], t_[:, sl], AF.Square)
                            P.mm(pbs[0][:], bd64, sq[:])
                            P.act(rs[:], pbs[0][:], AF.Sqrt, bias=eps6[:, 0:1], scale=1.0 / 64)
                            P.recip(rs[:], rs[:])
                            P.stt('dve', t_[:, sl], t_[:, sl], gcol, rs[:], ALU.mult, ALU.mult)
                    P.memset('pool', acc[0][:], 0.0)
                    P.memset('pool', acc[1][:], 0.0)
                    for pi in [int(ch_) for ch_ in os.environ.get('KAP', '012')]:
                        win_, d = PATTERNS[pi]
                        nb = 32 // d
                        Lsub = S // d
                        if os.environ.get('KAB'):
                            P.barrier()
                        for cls in range(d):
                            for kb in range(nb):
                                b = cls * nb + kb
                                st = cls + d * kb * 128
                                pb = pbs[1 + b % 2]
                                P.tr(pb[:, 0:128], v[:, st: st + d * 127 + 1: d] if d > 1 else v[:, st:st + 128], ident)
                                ce = 'act' if b % 2 else 'dve'
                                P.copy(ce, vaug[0][:, b, 0:64], pb[:, 0:64])
                                P.copy(ce, vaug[1][:, b, 64:128], pb[:, 64:128])
                        for hh in range(2):
                            h = 2 * c + hh
                            hp = slice(hh * 64, hh * 64 + 64)
                            for cls in range(d):
                                for kb in range(nb):
                                    b = cls * nb + kb
                                    nq = min(256, Lsub - kb * 128)
                                    ks = cls + d * kb * 128
                                    kap = k[hp, ks: ks + d * 127 + 1: d] if d > 1 else k[hp, ks:ks + 128]
                                    qap = q[hp, ks: ks + d * (nq - 1) + 1: d] if d > 1 else q[hp, ks:ks + nq]
                                    ps_ = pbs[3 + ne % 2]
                                    po_ = pbs[5 + ne % 2]
                                    e_ = et[ne % 3]
                                    ne += 1
                                    P.mm(ps_[:, 0:nq], kap, qap, start=True, stop=False)
                                    bi = (h * 3 + pi) * 256
                                    P.mm(ps_[:, 0:nq], ident, ab[:, bi:bi + nq], start=False, stop=True)
                                    P.act(e_[:, 0:nq], ps_[:, 0:nq], AF.Exp)
                                    P.mm(po_[:, 0:nq], vaug[hh][:, b, :], e_[:, 0:nq])
                                    aap = acc[hh][:, ks: ks + d * (nq - 1) + 1: d] if d > 1 else acc[hh][:, ks:ks + nq]
                                    P.tt('dve', aap, aap, po_[:, 0:nq], ALU.add)
                    for h4 in range(4):
                        sl = slice(h4 * 1024, (h4 + 1) * 1024)
                        P.copy('dve', q[0:64, sl], acc[0][64:128, sl])
                        P.recip(q[0:64, sl], q[0:64, sl])
                        P.tt('dve', q[0:64, sl], acc[0][0:64, sl], q[0:64, sl], ALU.mult)
                        P.copy('act', q[64:128, sl], acc[1][0:64, sl])
                        P.recip(q[64:128, sl], q[64:128, sl])
                        P.tt('dve', q[64:128, sl], acc[1][64:128, sl], q[64:128, sl], ALU.mult)
                    P.dma('pool', mixT[3 + c, :, :], q[:])
            P.barrier()

        def run_rwkv(l):
            SEG = 512
            NSEG = S // SEG
            NCH = SEG // 64
            with contextlib.ExitStack() as es:
                lora = sb(es, "rw_lora", [128, 384])
                P.dma('sp', lora[:], lora_d[l])
                eps12 = sb(es, "rw_e12", [128, 1])
                P.memset('dve', eps12[:], 1e-12)
                epsgn = sb(es, "rw_egn", [128, 1])
                P.memset('dve', epsgn[:], 64e-5)
                praw = sb(es, "rw_praw", [128, 1 + SEG])
                x9 = sb(es, "rw_x9", [128, SEG])
                tmp = sb(es, "rw_tmp", [128, SEG])
                tmp2 = sb(es, "rw_tmp2", [128, SEG])
                pr = []
                for c in range(3):
                    d_ = {}
                    for nm in ('r', 'k', 'v', 'a', 'kk', 'lw', 'cum', 'g', 'bonus', 'y', 'wc'):
                        d_[nm] = sb(es, f"rw_{nm}{c}", [128, SEG])
                    for nm in ('at', 'rt', 'bt', 'kt', 'vb'):
                        d_[nm] = sb(es, f"rw_{nm}bd{c}", [128, NCH, 128], fine=128)
                        P.memset('pool', d_[nm][:], 0.0)
                    d_['ST'] = [sb(es, f"rw_ST{c}_{i}", [128, 128]) for i in range(2)]
                    P.memset('dve', d_['ST'][0][:], 0.0)
                    for nm in ('N', 'NT', 'N2', 'N2T', 'Pm', 'Pm2', 'Aak', 'Arb', 'Ark', 'bT', 'kT', 'Vb', 'XT', 'Ub'):
                        d_[nm] = sb(es, f"rw_{nm}{c}", [128, 128])
                    d_['wend'] = sb(es, f"rw_wend{c}", [128, NCH])
                    pr.append(d_)
                stp = [0, 0, 0]

                def shift_load(dst, ch, tok0, mucol):
                    if tok0 == 0:
                        P.memset('dve', praw[:, 0:1], 0.0)
                        P.dma('pool', praw[:, 1:1 + SEG], projT[ch, :, 0:SEG])
                    else:
                        P.dma('pool', praw[:], projT[ch, :, tok0 - 1:tok0 + SEG])
                    P.tt('dve', tmp[:], praw[:, 0:SEG], praw[:, 1:1 + SEG], ALU.subtract)
                    P.stt('dve', dst, tmp[:], mucol, praw[:, 1:1 + SEG], ALU.mult, ALU.add)

                for seg in range(int(os.environ.get('KSEG', NSEG))):
                    tok0 = seg * SEG
                    shift_load(x9[:], 9, tok0, pcol(l, 24 + 9))
                    P.act(x9[0:32, :], x9[0:32, :], AF.Tanh)
                    P.act(x9[64:128, :], x9[64:128, :], AF.Sigmoid)
                    for c in range(3 if 'b' in os.environ.get('KRW', 'abcd') else 0):
                        d_ = pr[c]
                        cs = slice(c * 128, (c + 1) * 128)
                        shift_load(d_['r'][:], c, tok0, pcol(l, 24 + c))
                        shift_load(d_['k'][:], 3 + c, tok0, pcol(l, 24 + 3 + c))
                        shift_load(d_['v'][:], 6 + c, tok0, pcol(l, 24 + 6 + c))
                        P.mm(pbs[0][:], lora[0:32, cs], x9[0:32, :])
                        P.act(d_['lw'][:], pbs[0][:], AF.Sigmoid, bias=pcol(l, 34 + c), scale=1.0)
                        P.mm(pbs[1][:], lora[32:64, cs], x9[32:64, :])
                        P.act(d_['a'][:], pbs[1][:], AF.Sigmoid, bias=pcol(l, 37 + c), scale=1.0)
                        P.mm(pbs[2][:], lora[64:128, cs], x9[64:128, :])
                        P.copy('act', d_['g'][:], pbs[2][:])
                        P.ts('dve', d_['kk'][:], d_['k'][:], pcol(l, 40 + c), ALU.mult)
                        P.act(tmp2[:], d_['kk'][:], AF.Square)
                        P.mm(pbs[0][:], bd64, tmp2[:])
                        P.act(tmp2[:], pbs[0][:], AF.Sqrt, bias=eps12[:, 0:1], scale=1.0)
                        P.recip(tmp2[:], tmp2[:])
                        P.tt('dve', d_['kk'][:], d_['kk'][:], tmp2[:], ALU.mult)
                        P.ts('dve', tmp2[:], d_['a'][:], -1.0, ALU.add, pcol(l, 43 + c), ALU.mult)
                        P.stt('dve', d_['k'][:], tmp2[:], 1.0, d_['k'][:], ALU.add, ALU.mult)
                        P.tt('pool', tmp2[:], d_['r'][:], d_['k'][:], ALU.mult)
                        P.ts('pool', tmp2[:], tmp2[:], pcol(l, 46 + c), ALU.mult)
                        P.mm(pbs[1][:], bd64, tmp2[:])
                        P.tt('dve', d_['bonus'][:], pbs[1][:], d_['v'][:], ALU.mult)
                        P.ts('dve', d_['lw'][:], d_['lw'][:], -EXPM05, ALU.mult)
                        P.scan(d_['cum'][:], rmask, d_['lw'][:], 0.0, ALU.mult, ALU.add)
                        P.act(d_['wc'][:], d_['cum'][:], AF.Exp)
                        P.copy('dve', d_['wend'][:], d_['wc'][:].rearrange("p (c t) -> p c t", t=64)[:, :, 63])
                        P.tt('dve', tmp2[:], d_['cum'][:], d_['lw'][:], ALU.subtract)
                        P.act(tmp2[:], tmp2[:], AF.Exp)
                        P.tt('dve', tmp2[:], tmp2[:], d_['kk'][:], ALU.mult)
                        P.act(tmp[:], d_['cum'][:], AF.Exp, scale=-1.0)
                        P.tt('pool', d_['y'][:], d_['kk'][:], d_['a'][:], ALU.mult)
                        for hh in range(2):
                            hp = slice(hh * 64, hh * 64 + 64)
                            cp = slice(hh * 64, hh * 64 + 64)

                            def v3(t):
                                return t[hp, :].rearrange("p (c t) -> p c t", t=64)
                            P.ts('dve', d_['at'][hp, :, cp], v3(tmp2), -1.0, ALU.mult)
                            P.tt('dve', d_['rt'][hp, :, cp], v3(d_['r']), v3(d_['wc']), ALU.mult)
                            P.tt('dve', d_['bt'][hp, :, cp], v3(d_['y']), v3(tmp), ALU.mult)
                            P.tt('dve', d_['kt'][hp, :, cp], v3(d_['k']), v3(tmp), ALU.mult)
                            P.copy('pool', d_['vb'][hp, :, cp], v3(d_['v']))
                    def chunk_gen(n, c):
                        d_ = pr[c]
                        at, rt, bt, kt, vb = (d_[x][:, n, :] for x in ('at', 'rt', 'bt', 'kt', 'vb'))
                        pq = [pbs[c * 2][:, 0:128], pbs[c * 2][:, 128:256], pbs[c * 2][:, 256:384], pbs[c * 2][:, 384:512],
                              pbs[c * 2 + 1][:, 0:128], pbs[c * 2 + 1][:, 128:256], pbs[c * 2 + 1][:, 256:384], pbs[c * 2 + 1][:, 384:512]]
                        P.mm(pq[0], bt, at)
                        P.mm(pq[1], at, bt)
                        P.mm(pq[2], kt, at)
                        P.mm(pq[3], bt, rt)
                        P.mm(pq[4], kt, rt)
                        P.tr(pq[5], bt, ident)
                        P.tr(pq[6], kt, ident)
                        P.tr(pq[7], vb, ident)
                        yield
                        P.tt('dve', d_['N'][:], pq[0], msu, ALU.mult)
                        P.tt('dve', d_['NT'][:], pq[1], msl, ALU.mult)
                        P.tt('dve', d_['Aak'][:], pq[2], msu, ALU.mult)
                        P.tt('dve', d_['Arb'][:], pq[3], mu_, ALU.mult)
                        P.tt('dve', d_['Ark'][:], pq[4], mu_, ALU.mult)
                        P.copy('act', d_['bT'][:], pq[5])
                        P.copy('act', d_['kT'][:], pq[6])
                        P.copy('act', d_['Vb'][:], pq[7])
                        P.tt('dve', d_['Pm'][:], d_['N'][:], ident, ALU.add)
                        yield
                        cur, curT, nxt, nxtT = d_['N'], d_['NT'], d_['N2'], d_['N2T']
                        Pc, Pn = d_['Pm'], d_['Pm2']
                        for lev in range(5):
                            P.mm(pq[0], cur[:], curT[:])
                            if lev < 4:
                                P.mm(pq[4], curT[:], cur[:])
                            yield
                            P.copy('act', nxtT[:], pq[0])
                            if lev < 4:
                                P.copy('dve', nxt[:], pq[4])
                            yield
                            P.mm(pq[5], nxtT[:], Pc[:])
                            yield
                            P.tt('dve', Pn[:], pq[5], Pc[:], ALU.add)
                            yield
                            cur, curT, nxt, nxtT = nxt, nxtT, cur, curT
                            Pc, Pn = Pn, Pc
                        Minv = Pc
                        ST0 = d_['ST'][stp[c] % 2]
                        ST1 = d_['ST'][(stp[c] + 1) % 2]
                        stp[c] += 1
                        P.mm(pq[3], at, ST0[:], start=True, stop=False)
                        P.mm(pq[3], d_['Aak'][:], d_['Vb'][:], start=False, stop=True)
                        yield
                        P.copy('act', d_['XT'][:], pq[3])
                        yield
                        P.mm(pq[4], Minv[:], d_['XT'][:])
                        yield
                        P.copy('dve', d_['Ub'][:], pq[4])
                        yield
                        P.mm(pq[6], ident, ST0[:], start=True, stop=False)
                        P.mm(pq[6], d_['bT'][:], d_['Ub'][:], start=False, stop=False)
                        P.mm(pq[6], d_['kT'][:], d_['Vb'][:], start=False, stop=True)
                        P.mm(pq[1], ST0[:], rt, start=True, stop=False)
                        P.mm(pq[1], d_['Ub'][:], d_['Arb'][:], start=False, stop=False)
                        P.mm(pq[1], d_['Vb'][:], d_['Ark'][:], start=False, stop=True)
                        yield
                        P.ts('dve', ST1[:], pq[6], d_['wend'][:, n:n + 1], ALU.mult)
                        P.copy('act', d_['y'][0:64, n * 64:(n + 1) * 64], pq[1][0:64, 0:64])
                        P.copy('act', d_['y'][64:128, n * 64:(n + 1) * 64], pq[1][64:128, 64:128])
                        yield

                    for n in range(int(os.environ.get('KNCH', NCH)) if 'c' in os.environ.get('KRW', 'abcd') else 0):
                        gens = [chunk_gen(n, c) for c in range(int(os.environ.get('KPAIRS', 3)))]
                        alive = True
                        ny = 0
                        while alive and ny < int(os.environ.get('KGEN', 1000)):
                            alive = False
                            ny += 1
                            for g_ in gens:
                                try:
                                    next(g_)
                                    alive = True
                                except StopIteration:
                                    pass
                    for c in range(3 if 'd' in os.environ.get('KRW', 'abcd') else 0):
                        d_ = pr[c]
                        P.mm(pbs[6][:], bd64, d_['y'][:])
                        P.ts('dve', tmp[:], pbs[6][:], 1.0 / 64, ALU.mult)
                        P.tt('dve', d_['y'][:], d_['y'][:], tmp[:], ALU.subtract)
                        P.act(tmp2[:], d_['y'][:], AF.Square)
                        P.mm(pbs[7][:], bd64, tmp2[:])
                        P.act(tmp2[:], pbs[7][:], AF.Sqrt, bias=epsgn[:, 0:1], scale=1.0 / 64)
                        P.recip(tmp2[:], tmp2[:])
                        P.tt('dve', d_['y'][:], d_['y'][:], tmp2[:], ALU.mult)
                        P.ts('dve', d_['y'][:], d_['y'][:], pcol(l, 49 + c), ALU.mult, pcol(l, 52 + c), ALU.add)
                        P.tt('dve', d_['y'][:], d_['y'][:], d_['bonus'][:], ALU.add)
                        P.tt('dve', d_['wc'][:], d_['y'][:], d_['g'][:], ALU.mult)
                        P.dma('pool', mixT[c, :, tok0:tok0 + SEG], d_['wc'][:])
            P.barrier()

        P.barrier()
        for l in range(L + 1):
            if 'pass' in phases:
                run_pass(l)
            if l < L:
                if 'conv' in phases:
                    run_conv(l)
                if 'attn' in phases:
                    run_attn(l)
                if 'rwkv' in phases:
                    run_rwkv(l)
        st = P.emit(final_ops)
    return nc, st


def _alibi_slopes(n):
    def pow2(m):
        start = 2.0 ** (-8.0 / m)
        return [start ** (i + 1) for i in range(m)]
    if math.log2(n).is_integer():
        return pow2(n)
    c = 2 ** int(math.floor(math.log2(n)))
    return pow2(c) + pow2(2 * c)[0::2][: n - c]


def _consts():
    cst = np.zeros((128, NCST), np.float32)
    cst[:, C_ID:C_ID + 128] = np.eye(128, dtype=np.float32)
    cst[:, C_ONES:C_ONES + 128] = 1.0
    i = np.arange(128)
    same = (i[:, None] // 64) == (i[None, :] // 64)
    cst[:, C_BD:C_BD + 128] = same
    loc = i % 64
    cst[:, C_MSU:C_MSU + 128] = same & (loc[:, None] < loc[None, :])
    cst[:, C_MSL:C_MSL + 128] = same & (loc[:, None] > loc[None, :])
    cst[:, C_MU:C_MU + 128] = same & (loc[:, None] <= loc[None, :])
    cst[:, C_RM:C_RM + 512] = (np.arange(512) % 64 != 0).astype(np.float32)[None, :]
    slopes = _alibi_slopes(6)
    ab = np.zeros((128, 18, 256), np.float32)
    kk = np.arange(128)[:, None]
    qq = np.arange(256)[None, :]
    dist = qq - kk
    for h in range(6):
        for pi, (w, d) in enumerate(PATTERNS):
            valid = (dist >= 0) & (dist <= w // d)
            ab[:, h * 3 + pi, :] = np.where(valid, -slopes[h] * (dist * d).astype(np.float32), -30000.0)
    return cst, ab.reshape(128, 18 * 256)


def _cols(v):
    v = np.asarray(v, np.float32).reshape(-1, 128)
    return v.T


def _prep(inp, L):
    f = lambda a: np.ascontiguousarray(np.asarray(a, np.float32))
    m = {}
    for i, nm in ((1, 'ffn1'), (2, 'ffn2')):
        g = f(inp[f'{nm}_w_gate'])[:L].reshape(L, 8, 128, NFC, 128).transpose(0, 3, 2, 1, 4)
        u = f(inp[f'{nm}_w_up'])[:L].reshape(L, 8, 128, NFC, 128).transpose(0, 3, 2, 1, 4)
        m[f'wgu{i}'] = np.ascontiguousarray(np.stack([g, u], axis=3)).reshape(L, NFC, 128, 2048)
        dn = f(inp[f'{nm}_w_down'])[:L].reshape(L, NFC, 128, 8, 128).transpose(0, 3, 2, 1, 4)
        m[f'wd{i}'] = np.ascontiguousarray(dn).reshape(L, 8, 128, NFC * 128)
    m['win'] = np.ascontiguousarray(f(inp['w_in'])[:L].reshape(L, 8, 128, NOC, 128).transpose(0, 3, 2, 1, 4)).reshape(L, NOC, 128, 1024)
    m['wout'] = np.ascontiguousarray(f(inp['w_out'])[:L].reshape(L, 8, 128, 8, 128).transpose(0, 3, 2, 1, 4)).reshape(L, 8, 128, 1024)
    m['lora'] = np.ascontiguousarray(np.concatenate([f(inp['rwkv_w2'])[:L], f(inp['rwkv_a2'])[:L], f(inp['rwkv_g2'])[:L]], axis=1))
    prm = np.zeros((128, L, NP), np.float32)
    for l in range(L):
        prm[:, l, 0:8] = _cols(inp['norm_ffn1'][l])
        prm[:, l, 8:16] = _cols(inp['norm_mix'][l])
        prm[:, l, 16:24] = _cols(inp['norm_ffn2'][l])
        prm[:, l, 24:34] = _cols(inp['shift_mu'][l])
        prm[:, l, 34:37] = _cols(inp['rwkv_w0'][l])
        prm[:, l, 37:40] = _cols(inp['rwkv_a0'][l])
        prm[:, l, 40:43] = _cols(inp['rwkv_k_k'][l])
        prm[:, l, 43:46] = _cols(inp['rwkv_k_a'][l])
        prm[:, l, 46:49] = _cols(np.asarray(inp['rwkv_r_k'][l]).reshape(-1))
        prm[:, l, 49:52] = _cols(inp['rwkv_ln_w'][l])
        prm[:, l, 52:55] = _cols(inp['rwkv_ln_b'][l])
        prm[:, l, 55] = np.tile(np.asarray(inp['attn_q_norm'][l], np.float32), 2)
        prm[:, l, 56] = np.tile(np.asarray(inp['attn_k_norm'][l], np.float32), 2)
        prm[:, l, 57:59] = _cols(inp['conv_dw_b'][l])
        prm[:, l, 59:61] = _cols(inp['conv_ln_w'][l])
        prm[:, l, 61:63] = _cols(inp['conv_ln_b'][l])
        cw = np.asarray(inp['conv_dw_w'][l], np.float32)
        for ch in range(2):
            prm[:, l, 63 + ch * 31: 63 + (ch + 1) * 31] = cw[:, ch * 128:(ch + 1) * 128].T
    m['prm'] = np.ascontiguousarray(prm.reshape(128, L * NP))
    cst, ab = _consts()
    m['cst'] = cst
    m['abias'] = ab
    return m


_CACHE = {}


def kernel(**inputs):
    L = 4
    x = np.ascontiguousarray(np.asarray(inputs['x'], np.float32))
    B = x.shape[0]
    if 'nc' not in _CACHE:
        _CACHE['nc'] = build(L)[0]
    nc = _CACHE['nc']
    shared = _prep(inputs, L)
    in_maps = []
    for b in range(B):
        d = dict(shared)
        d['x'] = x[b]
        in_maps.append(d)
    res = run_bass_kernel_spmd(nc, in_maps, core_ids=list(range(B)))
    return np.stack([np.asarray(r['out'], np.float32) for r in res.results], axis=0)
```

```python
import contextlib
import math
import os
import numpy as np
import concourse.bass as bass
import concourse.mybir as mybir
from concourse.bass_utils import run_bass_kernel_spmd

F32 = mybir.dt.float32
F32R = mybir.dt.float32r
FAST = os.environ.get('KFAST', '1') == '1'


def R(ap):
    return ap
ALU = mybir.AluOpType
AF = mybir.ActivationFunctionType

S = 4096
D = 1024
DFF = 2816
NFC = 22
NOC = 23
TT = 512
NT = S // TT
NP = 128
C_ID, C_ONES, C_BD, C_MSU, C_MSL, C_MU, C_RM = 0, 128, 256, 384, 512, 640, 768
NCST = 768 + 512
PATTERNS = ((128, 1), (512, 4), (2048, 16))
EXPM05 = math.exp(-0.5)

SEM_GEN = 8000
KF = os.environ.get('KF', 'ofi')
DMA_RING = 6


class Op:
    __slots__ = ('eng', 'fn', 'deps', 'is_dma', 'needs_inc', 'tok', 'ring_wait')

    def __init__(self, eng, fn, is_dma):
        self.eng = eng
        self.fn = fn
        self.deps = []
        self.is_dma = is_dma
        self.needs_inc = is_dma
        self.tok = None
        self.ring_wait = None


class Prog:
    def __init__(self, nc):
        self.nc = nc
        self.ops = {e: [] for e in ('pe', 'act', 'dve', 'pool', 'sp')}
        self.lastw = {}
        self.readers = {}
        self.fine = {}
        self.pending = None
        self.applied = set()

    def keys(self, ap):
        name = ap.tensor.name
        g = self.fine.get(name)
        if g is None:
            return [name]
        gran, row = g
        off = ap.offset % row
        span = 1
        for st, cnt in list(ap.ap)[1:]:
            span += (cnt - 1) * st
        return [(name, i) for i in range(off // gran, (off + span - 1) // gran + 1)]

    def _add(self, eng, fn, reads, writes, is_dma):
        o = Op(eng, fn, is_dma)
        lst = self.ops[eng]
        me = (eng, len(lst))
        lst.append(o)
        reads = list(dict.fromkeys(reads))
        writes = list(dict.fromkeys(writes))
        deps = set()
        for k in reads:
            w = self.lastw.get(k)
            if w is not None:
                if self.ops[w[0]][w[1]].is_dma or is_dma or w[0] != eng or eng != 'pe':
                    deps.add(w)
            if isinstance(k, str) and k.startswith('pb'):
                for r in self.readers.get(k, ()):
                    if r[0] != eng:
                        deps.add(r)
        for k in writes:
            w = self.lastw.get(k)
            if w is not None:
                if self.ops[w[0]][w[1]].is_dma or w[0] != eng or is_dma or eng != 'pe':
                    deps.add(w)
            for r in self.readers.get(k, ()):
                if self.ops[r[0]][r[1]].is_dma or r[0] != eng or is_dma or eng != 'pe':
                    deps.add(r)
        if self.pending is not None and eng not in self.applied:
            deps.update(self.pending)
            self.applied.add(eng)
        deps.discard(me)
        o.deps = list(deps)
        for d in o.deps:
            self.ops[d[0]][d[1]].needs_inc = True
        for k in reads:
            rl = self.readers.setdefault(k, [])
            if not is_dma:
                rl[:] = [r for r in rl if r[0] != eng or self.ops[r[0]][r[1]].is_dma]
            rl.append(me)
        for k in writes:
            self.lastw[k] = me
            self.readers[k] = []
        return o

    def barrier(self):
        deps = []
        for eng, lst in self.ops.items():
            for i in range(len(lst) - 1, -1, -1):
                if not lst[i].is_dma:
                    deps.append((eng, i))
                    break
            c = 0
            for i in range(len(lst) - 1, -1, -1):
                if lst[i].is_dma:
                    deps.append((eng, i))
                    c += 1
                    if c >= DMA_RING:
                        break
        self.pending = deps
        self.applied = set()
        self.lastw.clear()
        self.readers.clear()

    def _k(self, aps):
        ks = []
        for a in aps:
            if a is not None and not isinstance(a, (int, float)) and a.tensor.name in self.track:
                ks += self.keys(a)
        return ks

    def mm(self, out, lhsT, rhs, start=True, stop=True):
        return self._add('pe', lambda e: e.matmul(out, lhsT=lhsT, rhs=rhs, start=start, stop=stop),
                         self._k([lhsT, rhs]), self._k([out]), False)

    def tr(self, out, in_, ident):
        return self._add('pe', lambda e: e.transpose(out, in_, ident), self._k([in_, ident]), self._k([out]), False)

    def act(self, out, in_, func, bias=0.0, scale=1.0):
        return self._add('act', lambda e: e.activation(out=out, in_=in_, func=func, bias=bias, scale=scale),
                         self._k([in_, bias, scale]), self._k([out]), False)

    def tt(self, eng, out, in0, in1, op):
        return self._add(eng, lambda e: e.tensor_tensor(out=out, in0=in0, in1=in1, op=op),
                         self._k([in0, in1]), self._k([out]), False)

    def ts(self, eng, out, in0, s1, op0, s2=None, op1=None):
        if op1 is None:
            fn = lambda e: e.tensor_scalar(out=out, in0=in0, scalar1=s1, scalar2=None, op0=op0)
        else:
            fn = lambda e: e.tensor_scalar(out=out, in0=in0, scalar1=s1, scalar2=s2, op0=op0, op1=op1)
        return self._add(eng, fn, self._k([in0, s1, s2]), self._k([out]), False)

    def stt(self, eng, out, in0, scalar, in1, op0, op1):
        return self._add(eng, lambda e: e.scalar_tensor_tensor(out=out, in0=in0, scalar=scalar, in1=in1, op0=op0, op1=op1),
                         self._k([in0, scalar, in1]), self._k([out]), False)

    def scan(self, out, d0, d1, init, op0, op1):
        return self._add('dve', lambda e: e.tensor_tensor_scan(out=out, data0=d0, data1=d1, initial=init, op0=op0, op1=op1),
                         self._k([d0, d1]), self._k([out]), False)

    def copy(self, eng, out, in_):
        if eng == 'act':
            return self.act(out, in_, AF.Copy)
        return self._add(eng, lambda e: e.tensor_copy(out=out, in_=in_), self._k([in_]), self._k([out]), False)

    def recip(self, out, in_):
        return self._add('dve', lambda e: e.reciprocal(out=out, in_=in_), self._k([in_]), self._k([out]), False)

    def memset(self, eng, ap, val):
        return self._add(eng, lambda e: e.memset(ap, val), [], self._k([ap]), False)

    def dma(self, eng, out, in_):
        return self._add(eng, lambda e: e.dma_start(out=out, in_=in_), self._k([in_]), self._k([out]), True)

    def emit(self, final_ops):
        nc = self.nc
        with contextlib.ExitStack() as es:
            sems = {}
            tail = []
            for eng, lst in self.ops.items():
                for i in range(len(lst) - 1, -1, -1):
                    if not lst[i].is_dma:
                        lst[i].needs_inc = True
                        tail.append(lst[i])
                        break
                c_ = 0
                for i in range(len(lst) - 1, -1, -1):
                    if lst[i].is_dma:
                        tail.append(lst[i])
                        c_ += 1
                        if c_ >= DMA_RING:
                            break
            for eng, lst in self.ops.items():
                cnt = 0
                nd = 0
                for o in lst:
                    if o.is_dma:
                        slot, rnd = nd % DMA_RING, nd // DMA_RING
                        o.tok = (f"d_{eng}_{slot}", 16 * (rnd + 1))
                        if rnd > 0:
                            o.ring_wait = (f"d_{eng}_{slot}", 16 * rnd)
                        nd += 1
                    elif o.needs_inc:
                        g = cnt // SEM_GEN
                        o.tok = (f"c_{eng}_{g}", cnt - g * SEM_GEN + 1)
                        cnt += 1
            self.maxtok = {}
            for lst in self.ops.values():
                for o in lst:
                    if o.tok:
                        self.maxtok[o.tok[0]] = max(self.maxtok.get(o.tok[0], 0), o.tok[1])
            if os.environ.get('KSIM'):
                print('MAXTOK', self.maxtok)
            for lst in self.ops.values():
                for o in lst:
                    if o.tok and o.tok[0] not in sems:
                        sems[o.tok[0]] = es.enter_context(nc.semaphore(o.tok[0]))
            engmap = {'pe': 'tensor', 'act': 'scalar', 'dve': 'vector', 'pool': 'gpsimd', 'sp': 'sync'}
            plan = {}
            stats = {}
            for eng, lst in self.ops.items():
                known = {}
                nw = 0
                pl = []
                for o in lst:
                    need = {}
                    for d in o.deps:
                        t = self.ops[d[0]][d[1]].tok
                        if need.get(t[0], 0) < t[1]:
                            need[t[0]] = t[1]
                    if o.ring_wait:
                        t = o.ring_wait
                        if need.get(t[0], 0) < t[1]:
                            need[t[0]] = t[1]
                    ws = []
                    for s_, v in need.items():
                        if known.get(s_, 0) < v:
                            ws.append((s_, v))
                            known[s_] = v
                            nw += 1
                    pl.append(ws)
                plan[eng] = pl
                stats[eng] = (len(lst), nw)
            if os.environ.get('KSIM'):
                val = {k_: 0 for k_ in sems}
                pos = {e_: 0 for e_ in self.ops}
                prog = True
                while prog:
                    prog = False
                    for e_, lst in self.ops.items():
                        while pos[e_] < len(lst):
                            i_ = pos[e_]
                            if all(val[s_] >= v for s_, v in plan[e_][i_]):
                                o = lst[i_]
                                if o.tok is not None:
                                    val[o.tok[0]] += 16 if o.is_dma else 1
                                    assert val[o.tok[0]] == o.tok[1], (e_, i_, o.tok, val[o.tok[0]])
                                pos[e_] += 1
                                prog = True
                            else:
                                break
                for e_, lst in self.ops.items():
                    if pos[e_] < len(lst):
                        print("SIM DEADLOCK", e_, pos[e_], len(lst), plan[e_][pos[e_]], {s_: val[s_] for s_, v in plan[e_][pos[e_]]})
                print("SIM done", {e_: pos[e_] for e_ in pos})
            block = es.enter_context(nc.Block())
            for eng, lst in self.ops.items():
                def body(e, eng=eng, lst=lst):
                    for o, ws in zip(lst, plan[eng]):
                        for s_, v in ws:
                            e.wait_ge(sems[s_], v)
                        ins = o.fn(e)
                        if o.tok is not None:
                            ins.then_inc(sems[o.tok[0]], 16 if o.is_dma else 1)
                    for fo in list(final_ops) + tail:
                        e.wait_ge(sems[fo.tok[0]], fo.tok[1])
                getattr(block, engmap[eng])(body)
            self.stats = stats
        return stats


def build(L, dbg=False, phases=('pass', 'conv', 'attn', 'rwkv')):
    nc = bass.Bass("TRN2", target_bir_lowering=False)
    if FAST:
        nc.dge_precook = False

    def din(name, shape, dt=F32):
        return nc.dram_tensor(name, shape, dt, kind="ExternalInput").ap()

    x_in = din("x", [S, D])
    WDT = F32R if FAST else F32
    wgu = [din(f"wgu{i}", [L, NFC, 128, 2048], WDT) for i in (1, 2)]
    wd = [din(f"wd{i}", [L, 8, 128, NFC * 128], WDT) for i in (1, 2)]
    win = din("win", [L, NOC, 128, 1024], WDT)
    wout = din("wout", [L, 8, 128, 1024], WDT)
    lora_d = din("lora", [L, 128, 384])
    prm_d = din("prm", [128, L * NP])
    cst_d = din("cst", [128, NCST])
    abias_d = din("abias", [128, 18 * 256])
    out = nc.dram_tensor("out", [S, D], F32, kind="ExternalOutput").ap()
    skind = "ExternalOutput" if dbg else "Internal"
    xT = nc.dram_tensor("xT", [8, 128, S], F32, kind=skind).ap()
    projT = nc.dram_tensor("projT", [NOC, 128, S], F32, kind=skind).ap()
    mixT = nc.dram_tensor("mixT", [8, 128, S], F32R if FAST else F32, kind=skind).ap()

    P = Prog(nc)
    P.track = set()
    final_ops = []

    with contextlib.ExitStack() as gs:
        uid = [0]

        def sb(es, name, shape, fine=None, dt=F32):
            uid[0] += 1
            name = f"{name}_u{uid[0]}"
            t = es.enter_context(nc.sbuf_tensor(name, shape, dt))
            P.track.add(name)
            if fine:
                row = 1
                for s_ in shape[1:]:
                    row *= s_
                P.fine[name] = (fine, row)
            return t

        cst = sb(gs, "cst_s", [128, NCST], fine=128)
        prm = sb(gs, "prm_s", [128, L * NP])
        pbs = []
        for i in range(8):
            t = gs.enter_context(nc.psum_tensor(f"pb{i}", [128, 512], F32))
            P.track.add(f"pb{i}")
            pbs.append(t)
        P.dma('sp', cst[:], cst_d)
        P.dma('sp', prm[:], prm_d)
        ident = cst[:, C_ID:C_ID + 128]
        ones = cst[:, C_ONES:C_ONES + 128]
        bd64 = cst[:, C_BD:C_BD + 128]
        msu = cst[:, C_MSU:C_MSU + 128]
        msl = cst[:, C_MSL:C_MSL + 128]
        mu_ = cst[:, C_MU:C_MU + 128]
        rmask = cst[:, C_RM:C_RM + 512]

        def pcol(l, c, n=1):
            return prm[:, l * NP + c: l * NP + c + n]

        def run_pass(l):
            with contextlib.ExitStack() as es:
                xt = sb(es, "xt", [128, 8, TT], fine=TT)
                hT = sb(es, "hT", [128, 8, TT], dt=WDT)
                actT = sb(es, "actT", [128, NFC, TT], fine=TT, dt=WDT)
                sq = sb(es, "sq", [128, 8 * TT])
                rstd = sb(es, "rstd", [128, TT])
                sg = [sb(es, f"sg{i}", [128, TT]) for i in range(2)]
                wb = [sb(es, f"wb{i}", [128, 2048], dt=WDT) for i in range(3)]
                wdb = [sb(es, f"wdb{i}", [128, NFC * 128], dt=WDT) for i in range(2)]
                wb1 = [sb(es, f"wb1_{i}", [128, 1024], dt=WDT) for i in range(3)]
                pj = [sb(es, f"pj{i}", [128, TT]) for i in range(3)]
                xtok = [sb(es, f"xtok{i}", [128, D]) for i in range(2)]
                cnt = {'wb': 0, 'wd': 0, 'pj': 0, 'g': 0, 'o': 0}

                def norm_h(gcol0):
                    P.act(sq[:], xt[:].rearrange("p c t -> p (c t)"), AF.Square)
                    for dc in range(8):
                        P.mm(pbs[0][:], ones, sq[:, dc * TT:(dc + 1) * TT], start=(dc == 0), stop=(dc == 7))
                    P.act(rstd[:], pbs[0][:], AF.Sqrt, bias=eps6[:, 0:1], scale=1.0 / D)
                    P.recip(rstd[:], rstd[:])
                    for dc in range(8):
                        P.stt('dve', R(hT[:, dc, :]), xt[:, dc, :], pcol(l_cur[0], gcol0 + dc), rstd[:], ALU.mult, ALU.mult)

                def ffn(ll, which):
                    l_cur[0] = ll
                    norm_h(0 if which == 0 else 16)
                    for fc in range(NFC):
                        w = wb[cnt['wb'] % 3]
                        cnt['wb'] += 1
                        P.dma('sp', R(w[:]), wgu[which][ll, fc])
                        pg = pbs[1 + cnt['g'] % 2]
                        pu = pbs[3 + cnt['g'] % 2]
                        sgt = sg[cnt['g'] % 2]
                        cnt['g'] += 1
                        for dc in range(8):
                            P.mm(pg[:], R(w[:, dc * 128:(dc + 1) * 128]), R(hT[:, dc, :]), start=(dc == 0), stop=(dc == 7))
                        for dc in range(8):
                            P.mm(pu[:], R(w[:, 1024 + dc * 128:1024 + (dc + 1) * 128]), R(hT[:, dc, :]), start=(dc == 0), stop=(dc == 7))
                        P.act(sgt[:], pg[:], AF.Silu)
                        P.tt('dve', R(actT[:, fc, :]), sgt[:], pu[:], ALU.mult)
                    for dc in range(8):
                        w = wdb[cnt['wd'] % 2]
                        cnt['wd'] += 1
                        P.dma('sp', R(w[:]), wd[which][ll, dc])
                        po = pbs[5 + cnt['o'] % 2]
                        cnt['o'] += 1
                        for fc in range(NFC):
                            P.mm(po[:], R(w[:, fc * 128:(fc + 1) * 128]), R(actT[:, fc, :]), start=(fc == 0), stop=(fc == NFC - 1))
                        P.stt('dve', xt[:, dc, :], po[:], 0.5, xt[:, dc, :], ALU.mult, ALU.add)

                def inproj(ll, tok0):
                    l_cur[0] = ll
                    norm_h(8)
                    for oc in range(NOC):
                        w = wb1[cnt['wb'] % 3]
                        cnt['wb'] += 1
                        P.dma('sp', w[:], win[ll, oc])
                        po = pbs[5 + cnt['o'] % 2]
                        cnt['o'] += 1
                        for dc in range(8):
                            P.mm(po[:], R(w[:, dc * 128:(dc + 1) * 128]), R(hT[:, dc, :]), start=(dc == 0), stop=(dc == 7))
                        pt = pj[cnt['pj'] % 3]
                        cnt['pj'] += 1
                        P.copy('act' if oc % 2 else 'dve', pt[:], po[:])
                        P.dma('pool', projT[oc, :, tok0:tok0 + TT], pt[:])

                def outproj(ll, tok0):
                    mt = actT
                    P.dma('pool', R(mt[:, 0:8, :]), R(mixT[:, :, tok0:tok0 + TT]).rearrange("c p t -> p c t"))
                    for dc in range(8):
                        w = wb1[cnt['wb'] % 3]
                        cnt['wb'] += 1
                        P.dma('sp', w[:], wout[ll, dc])
                        po = pbs[5 + cnt['o'] % 2]
                        cnt['o'] += 1
                        for kc in range(8):
                            P.mm(po[:], R(w[:, kc * 128:(kc + 1) * 128]), R(mt[:, kc, :]), start=(kc == 0), stop=(kc == 7))
                        P.tt('dve', xt[:, dc, :], po[:], xt[:, dc, :], ALU.add)

                l_cur = [0]
                eps6 = sb(es, "eps6", [128, 1])
                P.memset('dve', eps6[:], 1e-6)
                for tt_ in range(int(os.environ.get('KNT', NT))):
                    tok0 = tt_ * TT
                    if l == 0:
                        for s4 in range(4):
                            xk = xtok[s4 % 2]
                            P.dma('pool', xk[:], x_in[tok0 + s4 * 128: tok0 + (s4 + 1) * 128, :])
                            for half in range(2):
                                pb = pbs[5 + half]
                                for q in range(4):
                                    dc = half * 4 + q
                                    P.tr(pb[:, q * 128:(q + 1) * 128], xk[:, dc * 128:(dc + 1) * 128], ident)
                                P.copy('act' if half else 'dve', xt[:, half * 4:half * 4 + 4, s4 * 128:(s4 + 1) * 128],
                                       pb[:].rearrange("p (c t) -> p c t", t=128))
                    else:
                        P.dma('pool', xt[:], xT[:, :, tok0:tok0 + TT].rearrange("c p t -> p c t"))
                    if l > 0:
                        if 'o' in KF:
                            outproj(l - 1, tok0)
                        if 'f' in KF:
                            ffn(l - 1, 1)
                    if l < L:
                        if 'f' in KF:
                            ffn(l, 0)
                        if 'i' in KF:
                            inproj(l, tok0)
                        P.dma('pool', xT[:, :, tok0:tok0 + TT].rearrange("c p t -> p c t"), xt[:])
                    else:
                        for s4 in range(4):
                            xk = xtok[s4 % 2]
                            for half in range(2):
                                pb = pbs[5 + half]
                                for q in range(4):
                                    dc = half * 4 + q
                                    P.tr(pb[:, q * 128:(q + 1) * 128], xt[:, dc, s4 * 128:(s4 + 1) * 128], ident)
                                P.copy('act' if half else 'dve', xk[:, half * 512:(half + 1) * 512], pb[:])
                            final_ops.append(P.dma('pool', out[tok0 + s4 * 128: tok0 + (s4 + 1) * 128, :], xk[:]))
            P.barrier()

        def run_conv(l):
            with contextlib.ExitStack() as es:
                a_t = sb(es, "cv_a", [128, S])
                b_t = sb(es, "cv_b", [128, S])
                zp = sb(es, "cv_zp", [128, 30 + S])
                zc = [sb(es, f"cv_zc{i}", [128, S]) for i in range(2)]
                sq = sb(es, "cv_sq", [128, TT])
                mean = sb(es, "cv_mean", [128, TT])
                var = sb(es, "cv_var", [128, TT])
                yt = [sb(es, f"cv_y{i}", [128, TT]) for i in range(2)]
                eps5 = sb(es, "eps5", [128, 1])
                P.memset('dve', eps5[:], 1e-5)
                P.memset('dve', zp[:, 0:30], 0.0)
                for ch in range(2):
                    P.dma('pool', a_t[:], projT[19 + ch, :, :])
                    P.dma('pool', b_t[:], projT[21 + ch, :, :])
                    for h in range(4):
                        sl = slice(h * 1024, (h + 1) * 1024)
                        P.act(b_t[:, sl], b_t[:, sl], AF.Sigmoid)
                    P.tt(os.environ.get('KCE', 'pool'), zp[:, 30:30 + S], a_t[:], b_t[:], ALU.mult)
                    if 'b' not in os.environ.get('KC', 'abc'):
                        continue
                    wc = 63 + ch * 31
                    acc = zc[ch]
                    P.ts('dve', acc[:], zp[:, 0:S], pcol(l, wc), ALU.mult, pcol(l, 57 + ch), ALU.add)
                    for j in range(1, 31):
                        P.stt('dve', acc[:], zp[:, j:j + S], pcol(l, wc + j), acc[:], ALU.mult, ALU.add)
                for tt_ in range(NT if 'c' in os.environ.get('KC', 'abc') else 0):
                    sl = slice(tt_ * TT, (tt_ + 1) * TT)
                    for ch in range(2):
                        P.mm(pbs[0][:], ones, zc[ch][:, sl], start=(ch == 0), stop=(ch == 1))
                    for ch in range(2):
                        P.act(sq[:], zc[ch][:, sl], AF.Square)
                        P.mm(pbs[1][:], ones, sq[:], start=(ch == 0), stop=(ch == 1))
                    P.ts('dve', mean[:], pbs[0][:], 1.0 / 256, ALU.mult)
                    P.tt('dve', var[:], mean[:], mean[:], ALU.mult)
                    P.stt('dve', var[:], pbs[1][:], 1.0 / 256, var[:], ALU.mult, ALU.subtract)
                    P.act(var[:], var[:], AF.Sqrt, bias=eps5[:, 0:1], scale=1.0)
                    P.recip(var[:], var[:])
                    for ch in range(2):
                        y = yt[ch]
                        P.tt('dve', y[:], zc[ch][:, sl], mean[:], ALU.subtract)
                        P.tt('dve', y[:], y[:], var[:], ALU.mult)
                        P.act(y[:], y[:], AF.Silu, bias=pcol(l, 61 + ch), scale=pcol(l, 59 + ch))
                        P.dma('pool', mixT[6 + ch, :, sl], y[:])
            P.barrier()

        def run_attn(l):
            with contextlib.ExitStack() as es:
                q = sb(es, "at_q", [128, S])
                k = sb(es, "at_k", [128, S])
                v = sb(es, "at_v", [128, S])
                qn = sb(es, "at_qn", [128, S], dt=WDT)
                kn = sb(es, "at_kn", [128, S], dt=WDT)
                acc = [sb(es, f"at_acc{i}", [128, S]) for i in range(2)]
                vaug = [sb(es, f"at_va{i}", [128, 32, 128], fine=128, dt=WDT) for i in range(2)]
                ab = sb(es, "at_bias", [128, 18 * 256], fine=256)
                et = [sb(es, f"at_e{i}", [128, 256], dt=WDT) for i in range(4)]
                e0 = [sb(es, f"at_e0{i}", [128, 256]) for i in range(4)]
                sq = sb(es, "at_sq", [128, TT])
                rs = sb(es, "at_rs", [128, TT])
                gq = sb(es, "at_gq", [128, 1])
                eps6 = sb(es, "at_eps", [128, 1])
                P.memset('dve', eps6[:], 1e-6)
                P.dma('sp', ab[:], abias_d)
                P.ts('dve', gq[:], pcol(l, 55), 0.125, ALU.mult)
                fin_ = ab[:, 0:2048].rearrange("p (b c) -> p b c", c=64)
                P.ts('dve', vaug[0][:, :, 64:128], fin_, 0.0, ALU.mult, 1.0, ALU.add)
                P.ts('dve', vaug[1][:, :, 0:64], fin_, 0.0, ALU.mult, 1.0, ALU.add)
                ne = 0
                for c in range(int(os.environ.get('KACH', 3))):
                    P.dma('pool', q[:], projT[10 + c, :, :])
                    P.dma('pool', k[:], projT[13 + c, :, :])
                    P.dma('pool', v[:], projT[16 + c, :, :])
                    for (t_, tn_, gcol) in ((q, qn, gq[:, 0:1]), (k, kn, pcol(l, 56))):
                        for tt_ in range(NT):
                            sl = slice(tt_ * TT, (tt_ + 1) * TT)
                            P.act(sq[:], t_[:, sl], AF.Square)
                            P.mm(pbs[0][:], bd64, sq[:])
                            P.act(rs[:], pbs[0][:], AF.Sqrt, bias=eps6[:, 0:1], scale=1.0 / 64)
                            P.recip(rs[:], rs[:])
                            P.stt('dve', tn_[:, sl], t_[:, sl], gcol, rs[:], ALU.mult, ALU.mult)
                    P.memset('pool', acc[0][:], 0.0)
                    P.memset('pool', acc[1][:], 0.0)
                    for pi in [int(ch_) for ch_ in os.environ.get('KAP', '012')]:
                        win_, d = PATTERNS[pi]
                        nb = 32 // d
                        Lsub = S // d
                        if os.environ.get('KAB'):
                            P.barrier()
                        for cls in range(d):
                            for kb in range(nb):
                                b = cls * nb + kb
                                st = cls + d * kb * 128
                                pb = pbs[1 + b % 2]
                                P.tr(pb[:, 0:128], v[:, st: st + d * 127 + 1: d] if d > 1 else v[:, st:st + 128], ident)
                                ce = 'act' if b % 2 else 'dve'
                                P.copy(ce, vaug[0][:, b, 0:64], pb[:, 0:64])
                                P.copy(ce, vaug[1][:, b, 64:128], pb[:, 64:128])
                        blocks = []
                        for hh in range(2):
                            for cls in range(d):
                                for kb in range(nb):
                                    blocks.append((hh, cls, kb))
                        pend = []

                        def emit_pv(blk):
                            hh, cls, kb, e_, nq, ks, i_ = blk
                            b = cls * nb + kb
                            po_ = pbs[5 + i_ % 2]
                            P.mm(po_[:, 0:nq], vaug[hh][:, b, :], e_[:, 0:nq])
                            aap = acc[hh][:, ks: ks + d * (nq - 1) + 1: d] if d > 1 else acc[hh][:, ks:ks + nq]
                            P.tt('dve', aap, aap, po_[:, 0:nq], ALU.add)

                        for (hh, cls, kb) in blocks:
                            h = 2 * c + hh
                            hp = slice(hh * 64, hh * 64 + 64)
                            nq = min(256, Lsub - kb * 128)
                            ks = cls + d * kb * 128
                            kap = kn[hp, ks: ks + d * 127 + 1: d] if d > 1 else kn[hp, ks:ks + 128]
                            qap = qn[hp, ks: ks + d * (nq - 1) + 1: d] if d > 1 else qn[hp, ks:ks + nq]
                            ps_ = pbs[(3, 4, 7)[ne % 3]]
                            e_ = et[ne % 4]
                            ee = e0[ne % 4]
                            P.mm(ps_[:, 0:nq], kap, qap, start=True, stop=True)
                            bi = (h * 3 + pi) * 256
                            P.act(ee[:, 0:nq], ps_[:, 0:nq], AF.Exp)
                            P.tt('pool', e_[:, 0:nq], ee[:, 0:nq], ab[:, bi:bi + nq], ALU.mult)
                            pend.append((hh, cls, kb, e_, nq, ks, ne))
                            ne += 1
                            if len(pend) > 2:
                                emit_pv(pend.pop(0))
                        while pend:
                            emit_pv(pend.pop(0))
                    for h4 in range(4):
                        sl = slice(h4 * 1024, (h4 + 1) * 1024)
                        P.copy('dve', q[0:64, sl], acc[0][64:128, sl])
                        P.recip(q[0:64, sl], q[0:64, sl])
                        P.tt('dve', q[0:64, sl], acc[0][0:64, sl], q[0:64, sl], ALU.mult)
                        P.copy('act', q[64:128, sl], acc[1][0:64, sl])
                        P.recip(q[64:128, sl], q[64:128, sl])
                        P.tt('dve', q[64:128, sl], acc[1][64:128, sl], q[64:128, sl], ALU.mult)
                    P.dma('pool', mixT[3 + c, :, :], q[:])
            P.barrier()

        def run_rwkv(l):
            SEG = 512
            NSEG = S // SEG
            NCH = SEG // 64
            with contextlib.ExitStack() as es:
                lora = sb(es, "rw_lora", [128, 384])
                P.dma('sp', lora[:], lora_d[l])
                eps12 = sb(es, "rw_e12", [128, 1])
                P.memset('dve', eps12[:], 1e-12)
                epsgn = sb(es, "rw_egn", [128, 1])
                P.memset('dve', epsgn[:], 64e-5)
                praw = sb(es, "rw_praw", [128, 1 + SEG])
                x9 = sb(es, "rw_x9", [128, SEG])
                tmp = sb(es, "rw_tmp", [128, SEG])
                tmp2 = sb(es, "rw_tmp2", [128, SEG])
                pr = []
                for c in range(3):
                    d_ = {}
                    for nm in ('r', 'k', 'v', 'a', 'kk', 'lw', 'cum', 'g', 'bonus', 'y', 'wc'):
                        d_[nm] = sb(es, f"rw_{nm}{c}", [128, SEG])
                    for nm in ('at', 'rt', 'bt', 'kt', 'vb'):
                        d_[nm] = sb(es, f"rw_{nm}bd{c}", [128, NCH, 128], fine=128)
                        P.memset('pool', d_[nm][:], 0.0)
                    d_['ST'] = [sb(es, f"rw_ST{c}_{i}", [128, 128]) for i in range(2)]
                    P.memset('dve', d_['ST'][0][:], 0.0)
                    for nm in ('N', 'NT', 'N2', 'N2T', 'Pm', 'Pm2', 'Aak', 'Arb', 'Ark', 'bT', 'kT', 'Vb', 'XT', 'Ub'):
                        d_[nm] = sb(es, f"rw_{nm}{c}", [128, 128])
                    d_['wend'] = sb(es, f"rw_wend{c}", [128, NCH])
                    pr.append(d_)
                stp = [0, 0, 0]

                def shift_load(dst, ch, tok0, mucol):
                    if tok0 == 0:
                        P.memset('dve', praw[:, 0:1], 0.0)
                        P.dma('pool', praw[:, 1:1 + SEG], projT[ch, :, 0:SEG])
                    else:
                        P.dma('pool', praw[:], projT[ch, :, tok0 - 1:tok0 + SEG])
                    P.tt('dve', tmp[:], praw[:, 0:SEG], praw[:, 1:1 + SEG], ALU.subtract)
                    P.stt('dve', dst, tmp[:], mucol, praw[:, 1:1 + SEG], ALU.mult, ALU.add)

                for seg in range(int(os.environ.get('KSEG', NSEG))):
                    tok0 = seg * SEG
                    shift_load(x9[:], 9, tok0, pcol(l, 24 + 9))
                    P.act(x9[0:32, :], x9[0:32, :], AF.Tanh)
                    P.act(x9[64:128, :], x9[64:128, :], AF.Sigmoid)
                    for c in range(3 if 'b' in os.environ.get('KRW', 'abcd') else 0):
                        d_ = pr[c]
                        cs = slice(c * 128, (c + 1) * 128)
                        shift_load(d_['r'][:], c, tok0, pcol(l, 24 + c))
                        shift_load(d_['k'][:], 3 + c, tok0, pcol(l, 24 + 3 + c))
                        shift_load(d_['v'][:], 6 + c, tok0, pcol(l, 24 + 6 + c))
                        P.mm(pbs[0][:], lora[0:32, cs], x9[0:32, :])
                        P.act(d_['lw'][:], pbs[0][:], AF.Sigmoid, bias=pcol(l, 34 + c), scale=1.0)
                        P.mm(pbs[1][:], lora[32:64, cs], x9[32:64, :])
                        P.act(d_['a'][:], pbs[1][:], AF.Sigmoid, bias=pcol(l, 37 + c), scale=1.0)
                        P.mm(pbs[2][:], lora[64:128, cs], x9[64:128, :])
                        P.copy('act', d_['g'][:], pbs[2][:])
                        P.ts('dve', d_['kk'][:], d_['k'][:], pcol(l, 40 + c), ALU.mult)
                        P.act(tmp2[:], d_['kk'][:], AF.Square)
                        P.mm(pbs[0][:], bd64, tmp2[:])
                        P.act(tmp2[:], pbs[0][:], AF.Sqrt, bias=eps12[:, 0:1], scale=1.0)
                        P.recip(tmp2[:], tmp2[:])
                        P.tt('dve', d_['kk'][:], d_['kk'][:], tmp2[:], ALU.mult)
                        P.ts('dve', tmp2[:], d_['a'][:], -1.0, ALU.add, pcol(l, 43 + c), ALU.mult)
                        P.stt('dve', d_['k'][:], tmp2[:], 1.0, d_['k'][:], ALU.add, ALU.mult)
                        P.tt('pool', tmp2[:], d_['r'][:], d_['k'][:], ALU.mult)
                        P.ts('pool', tmp2[:], tmp2[:], pcol(l, 46 + c), ALU.mult)
                        P.mm(pbs[1][:], bd64, tmp2[:])
                        P.tt('dve', d_['bonus'][:], pbs[1][:], d_['v'][:], ALU.mult)
                        P.ts('dve', d_['lw'][:], d_['lw'][:], -EXPM05, ALU.mult)
                        P.scan(d_['cum'][:], rmask, d_['lw'][:], 0.0, ALU.mult, ALU.add)
                        P.act(d_['wc'][:], d_['cum'][:], AF.Exp)
                        P.copy('dve', d_['wend'][:], d_['wc'][:].rearrange("p (c t) -> p c t", t=64)[:, :, 63])
                        P.tt('dve', tmp2[:], d_['cum'][:], d_['lw'][:], ALU.subtract)
                        P.act(tmp2[:], tmp2[:], AF.Exp)
                        P.tt('dve', tmp2[:], tmp2[:], d_['kk'][:], ALU.mult)
                        P.act(tmp[:], d_['cum'][:], AF.Exp, scale=-1.0)
                        P.tt('pool', d_['y'][:], d_['kk'][:], d_['a'][:], ALU.mult)
                        for hh in range(2):
                            hp = slice(hh * 64, hh * 64 + 64)
                            cp = slice(hh * 64, hh * 64 + 64)

                            def v3(t):
                                return t[hp, :].rearrange("p (c t) -> p c t", t=64)
                            P.ts('dve', d_['at'][hp, :, cp], v3(tmp2), -1.0, ALU.mult)
                            P.tt('dve', d_['rt'][hp, :, cp], v3(d_['r']), v3(d_['wc']), ALU.mult)
                            P.tt('dve', d_['bt'][hp, :, cp], v3(d_['y']), v3(tmp), ALU.mult)
                            P.tt('dve', d_['kt'][hp, :, cp], v3(d_['k']), v3(tmp), ALU.mult)
                            P.copy('pool', d_['vb'][hp, :, cp], v3(d_['v']))
                    def chunk_gen(n, c):
                        d_ = pr[c]
                        at, rt, bt, kt, vb = (d_[x][:, n, :] for x in ('at', 'rt', 'bt', 'kt', 'vb'))
                        pq = [pbs[c * 2][:, 0:128], pbs[c * 2][:, 128:256], pbs[c * 2][:, 256:384], pbs[c * 2][:, 384:512],
                              pbs[c * 2 + 1][:, 0:128], pbs[c * 2 + 1][:, 128:256], pbs[c * 2 + 1][:, 256:384], pbs[c * 2 + 1][:, 384:512]]
                        P.mm(pq[0], bt, at)
                        P.mm(pq[1], at, bt)
                        P.mm(pq[2], kt, at)
                        P.mm(pq[3], bt, rt)
                        P.mm(pq[4], kt, rt)
                        P.tr(pq[5], bt, ident)
                        P.tr(pq[6], kt, ident)
                        P.tr(pq[7], vb, ident)
                        yield
                        P.tt('dve', d_['N'][:], pq[0], msu, ALU.mult)
                        P.tt('dve', d_['NT'][:], pq[1], msl, ALU.mult)
                        P.tt('dve', d_['Aak'][:], pq[2], msu, ALU.mult)
                        P.tt('dve', d_['Arb'][:], pq[3], mu_, ALU.mult)
                        P.tt('dve', d_['Ark'][:], pq[4], mu_, ALU.mult)
                        P.copy('act', d_['bT'][:], pq[5])
                        P.copy('act', d_['kT'][:], pq[6])
                        P.copy('act', d_['Vb'][:], pq[7])
                        P.tt('dve', d_['Pm'][:], d_['N'][:], ident, ALU.add)
                        yield
                        cur, curT, nxt, nxtT = d_['N'], d_['NT'], d_['N2'], d_['N2T']
                        Pc, Pn = d_['Pm'], d_['Pm2']
                        for lev in range(5):
                            P.mm(pq[0], cur[:], curT[:])
                            if lev < 4:
                                P.mm(pq[4], curT[:], cur[:])
                            yield
                            P.copy('act', nxtT[:], pq[0])
                            if lev < 4:
                                P.copy('dve', nxt[:], pq[4])
                            yield
                            P.mm(pq[5], nxtT[:], Pc[:])
                            yield
                            P.tt('dve', Pn[:], pq[5], Pc[:], ALU.add)
                            yield
                            cur, curT, nxt, nxtT = nxt, nxtT, cur, curT
                            Pc, Pn = Pn, Pc
                        Minv = Pc
                        ST0 = d_['ST'][stp[c] % 2]
                        ST1 = d_['ST'][(stp[c] + 1) % 2]
                        stp[c] += 1
                        P.mm(pq[3], at, ST0[:], start=True, stop=False)
                        P.mm(pq[3], d_['Aak'][:], d_['Vb'][:], start=False, stop=True)
                        yield
                        P.copy('act', d_['XT'][:], pq[3])
                        yield
                        P.mm(pq[4], Minv[:], d_['XT'][:])
                        yield
                        P.copy('dve', d_['Ub'][:], pq[4])
                        yield
                        P.mm(pq[6], ident, ST0[:], start=True, stop=False)
                        P.mm(pq[6], d_['bT'][:], d_['Ub'][:], start=False, stop=False)
                        P.mm(pq[6], d_['kT'][:], d_['Vb'][:], start=False, stop=True)
                        P.mm(pq[1], ST0[:], rt, start=True, stop=False)
                        P.mm(pq[1], d_['Ub'][:], d_['Arb'][:], start=False, stop=False)
                        P.mm(pq[1], d_['Vb'][:], d_['Ark'][:], start=False, stop=True)
                        yield
                        P.ts('dve', ST1[:], pq[6], d_['wend'][:, n:n + 1], ALU.mult)
                        P.copy('act', d_['y'][0:64, n * 64:(n + 1) * 64], pq[1][0:64, 0:64])
                        P.copy('act', d_['y'][64:128, n * 64:(n + 1) * 64], pq[1][64:128, 64:128])
                        yield

                    for n in range(int(os.environ.get('KNCH', NCH)) if 'c' in os.environ.get('KRW', 'abcd') else 0):
                        gens = [chunk_gen(n, c) for c in range(int(os.environ.get('KPAIRS', 3)))]
                        alive = True
                        ny = 0
                        while alive and ny < int(os.environ.get('KGEN', 1000)):
                            alive = False
                            ny += 1
                            for g_ in gens:
                                try:
                                    next(g_)
                                    alive = True
                                except StopIteration:
                                    pass
                    for c in range(3 if 'd' in os.environ.get('KRW', 'abcd') else 0):
                        d_ = pr[c]
                        P.mm(pbs[6][:], bd64, d_['y'][:])
                        P.ts('dve', tmp[:], pbs[6][:], 1.0 / 64, ALU.mult)
                        P.tt('dve', d_['y'][:], d_['y'][:], tmp[:], ALU.subtract)
                        P.act(tmp2[:], d_['y'][:], AF.Square)
                        P.mm(pbs[7][:], bd64, tmp2[:])
                        P.act(tmp2[:], pbs[7][:], AF.Sqrt, bias=epsgn[:, 0:1], scale=1.0 / 64)
                        P.recip(tmp2[:], tmp2[:])
                        P.tt('dve', d_['y'][:], d_['y'][:], tmp2[:], ALU.mult)
                        P.ts('dve', d_['y'][:], d_['y'][:], pcol(l, 49 + c), ALU.mult, pcol(l, 52 + c), ALU.add)
                        P.tt('dve', d_['y'][:], d_['y'][:], d_['bonus'][:], ALU.add)
                        P.tt('dve', d_['wc'][:], d_['y'][:], d_['g'][:], ALU.mult)
                        P.dma('pool', mixT[c, :, tok0:tok0 + SEG], d_['wc'][:])
            P.barrier()

        P.barrier()
        for l in range(L + 1):
            if 'pass' in phases:
                run_pass(l)
            if l < L:
                if 'conv' in phases:
                    run_conv(l)
                if 'attn' in phases:
                    run_attn(l)
                if 'rwkv' in phases:
                    run_rwkv(l)
        st = P.emit(final_ops)
    return nc, st


def _alibi_slopes(n):
    def pow2(m):
        start = 2.0 ** (-8.0 / m)
        return [start ** (i + 1) for i in range(m)]
    if math.log2(n).is_integer():
        return pow2(n)
    c = 2 ** int(math.floor(math.log2(n)))
    return pow2(c) + pow2(2 * c)[0::2][: n - c]


def _consts():
    cst = np.zeros((128, NCST), np.float32)
    cst[:, C_ID:C_ID + 128] = np.eye(128, dtype=np.float32)
    cst[:, C_ONES:C_ONES + 128] = 1.0
    i = np.arange(128)
    same = (i[:, None] // 64) == (i[None, :] // 64)
    cst[:, C_BD:C_BD + 128] = same
    loc = i % 64
    cst[:, C_MSU:C_MSU + 128] = same & (loc[:, None] < loc[None, :])
    cst[:, C_MSL:C_MSL + 128] = same & (loc[:, None] > loc[None, :])
    cst[:, C_MU:C_MU + 128] = same & (loc[:, None] <= loc[None, :])
    cst[:, C_RM:C_RM + 512] = (np.arange(512) % 64 != 0).astype(np.float32)[None, :]
    slopes = _alibi_slopes(6)
    ab = np.zeros((128, 18, 256), np.float32)
    kk = np.arange(128)[:, None]
    qq = np.arange(256)[None, :]
    dist = qq - kk
    for h in range(6):
        for pi, (w, d) in enumerate(PATTERNS):
            valid = (dist >= 0) & (dist <= w // d)
            ab[:, h * 3 + pi, :] = np.where(valid, np.exp(-slopes[h] * (dist * d).astype(np.float64)), 0.0).astype(np.float32)
    return cst, ab.reshape(128, 18 * 256)


def _cols(v):
    v = np.asarray(v, np.float32).reshape(-1, 128)
    return v.T


def _prep(inp, L):
    f = lambda a: np.ascontiguousarray(np.asarray(a, np.float32))
    m = {}
    for i, nm in ((1, 'ffn1'), (2, 'ffn2')):
        g = f(inp[f'{nm}_w_gate'])[:L].reshape(L, 8, 128, NFC, 128).transpose(0, 3, 2, 1, 4)
        u = f(inp[f'{nm}_w_up'])[:L].reshape(L, 8, 128, NFC, 128).transpose(0, 3, 2, 1, 4)
        m[f'wgu{i}'] = np.ascontiguousarray(np.stack([g, u], axis=3)).reshape(L, NFC, 128, 2048)
        dn = f(inp[f'{nm}_w_down'])[:L].reshape(L, NFC, 128, 8, 128).transpose(0, 3, 2, 1, 4)
        m[f'wd{i}'] = np.ascontiguousarray(dn).reshape(L, 8, 128, NFC * 128)
    m['win'] = np.ascontiguousarray(f(inp['w_in'])[:L].reshape(L, 8, 128, NOC, 128).transpose(0, 3, 2, 1, 4)).reshape(L, NOC, 128, 1024)
    m['wout'] = np.ascontiguousarray(f(inp['w_out'])[:L].reshape(L, 8, 128, 8, 128).transpose(0, 3, 2, 1, 4)).reshape(L, 8, 128, 1024)
    m['lora'] = np.ascontiguousarray(np.concatenate([f(inp['rwkv_w2'])[:L], f(inp['rwkv_a2'])[:L], f(inp['rwkv_g2'])[:L]], axis=1))
    prm = np.zeros((128, L, NP), np.float32)
    for l in range(L):
        prm[:, l, 0:8] = _cols(inp['norm_ffn1'][l])
        prm[:, l, 8:16] = _cols(inp['norm_mix'][l])
        prm[:, l, 16:24] = _cols(inp['norm_ffn2'][l])
        prm[:, l, 24:34] = _cols(inp['shift_mu'][l])
        prm[:, l, 34:37] = _cols(inp['rwkv_w0'][l])
        prm[:, l, 37:40] = _cols(inp['rwkv_a0'][l])
        prm[:, l, 40:43] = _cols(inp['rwkv_k_k'][l])
        prm[:, l, 43:46] = _cols(inp['rwkv_k_a'][l])
        prm[:, l, 46:49] = _cols(np.asarray(inp['rwkv_r_k'][l]).reshape(-1))
        prm[:, l, 49:52] = _cols(inp['rwkv_ln_w'][l])
        prm[:, l, 52:55] = _cols(inp['rwkv_ln_b'][l])
        prm[:, l, 55] = np.tile(np.asarray(inp['attn_q_norm'][l], np.float32), 2)
        prm[:, l, 56] = np.tile(np.asarray(inp['attn_k_norm'][l], np.float32), 2)
        prm[:, l, 57:59] = _cols(inp['conv_dw_b'][l])
        prm[:, l, 59:61] = _cols(inp['conv_ln_w'][l])
        prm[:, l, 61:63] = _cols(inp['conv_ln_b'][l])
        cw = np.asarray(inp['conv_dw_w'][l], np.float32)
        for ch in range(2):
            prm[:, l, 63 + ch * 31: 63 + (ch + 1) * 31] = cw[:, ch * 128:(ch + 1) * 128].T
    m['prm'] = np.ascontiguousarray(prm.reshape(128, L * NP))
    cst, ab = _consts()
    m['cst'] = cst
    m['abias'] = ab
    return m


_CACHE = {}


def kernel(**inputs):
    L = 4
    x = np.ascontiguousarray(np.asarray(inputs['x'], np.float32))
    B = x.shape[0]
    if 'nc' not in _CACHE:
        _CACHE['nc'] = build(L)[0]
    nc = _CACHE['nc']
    shared = _prep(inputs, L)
    in_maps = []
    for b in range(B):
        d = dict(shared)
        d['x'] = x[b]
        in_maps.append(d)
    res = run_bass_kernel_spmd(nc, in_maps, core_ids=list(range(B)))
    return np.stack([np.asarray(r['out'], np.float32) for r in res.results], axis=0)
```

```python
import contextlib
import math
import os
import numpy as np
import concourse.bass as bass
import concourse.mybir as mybir
from concourse.bass_utils import run_bass_kernel_spmd

F32 = mybir.dt.float32
F32R = mybir.dt.float32r
FAST = os.environ.get('KFAST', '1') == '1'


def R(ap):
    return ap
ALU = mybir.AluOpType
AF = mybir.ActivationFunctionType

S = 4096
D = 1024
DFF = 2816
NFC = 22
NOC = 23
TT = 512
NT = S // TT
NP = 128
C_ID, C_ONES, C_BD, C_MSU, C_MSL, C_MU, C_RM = 0, 128, 256, 384, 512, 640, 768
NCST = 768 + 512
PATTERNS = ((128, 1), (512, 4), (2048, 16))
EXPM05 = math.exp(-0.5)

SEM_GEN = 8000
KF = os.environ.get('KF', 'ofi')
DMA_RING = 6


class Op:
    __slots__ = ('eng', 'fn', 'deps', 'is_dma', 'needs_inc', 'tok', 'ring_wait')

    def __init__(self, eng, fn, is_dma):
        self.eng = eng
        self.fn = fn
        self.deps = []
        self.is_dma = is_dma
        self.needs_inc = is_dma
        self.tok = None
        self.ring_wait = None


class Prog:
    def __init__(self, nc):
        self.nc = nc
        self.ops = {e: [] for e in ('pe', 'act', 'dve', 'pool', 'sp')}
        self.lastw = {}
        self.readers = {}
        self.fine = {}
        self.pending = None
        self.applied = set()

    def keys(self, ap):
        name = ap.tensor.name
        g = self.fine.get(name)
        if g is None:
            return [name]
        gran, row = g
        off = ap.offset % row
        span = 1
        for st, cnt in list(ap.ap)[1:]:
            span += (cnt - 1) * st
        return [(name, i) for i in range(off // gran, (off + span - 1) // gran + 1)]

    def _add(self, eng, fn, reads, writes, is_dma):
        o = Op(eng, fn, is_dma)
        lst = self.ops[eng]
        me = (eng, len(lst))
        lst.append(o)
        reads = list(dict.fromkeys(reads))
        writes = list(dict.fromkeys(writes))
        deps = set()
        for k in reads:
            w = self.lastw.get(k)
            if w is not None:
                if self.ops[w[0]][w[1]].is_dma or is_dma or w[0] != eng or eng != 'pe':
                    deps.add(w)
            if isinstance(k, str) and k.startswith('pb'):
                for r in self.readers.get(k, ()):
                    if r[0] != eng:
                        deps.add(r)
        for k in writes:
            w = self.lastw.get(k)
            if w is not None:
                if self.ops[w[0]][w[1]].is_dma or w[0] != eng or is_dma or eng != 'pe':
                    deps.add(w)
            for r in self.readers.get(k, ()):
                if self.ops[r[0]][r[1]].is_dma or r[0] != eng or is_dma or eng != 'pe':
                    deps.add(r)
        if self.pending is not None and eng not in self.applied:
            deps.update(self.pending)
            self.applied.add(eng)
        deps.discard(me)
        o.deps = list(deps)
        for d in o.deps:
            self.ops[d[0]][d[1]].needs_inc = True
        for k in reads:
            rl = self.readers.setdefault(k, [])
            if not is_dma:
                rl[:] = [r for r in rl if r[0] != eng or self.ops[r[0]][r[1]].is_dma]
            rl.append(me)
        for k in writes:
            self.lastw[k] = me
            self.readers[k] = []
        return o

    def barrier(self):
        deps = []
        for eng, lst in self.ops.items():
            for i in range(len(lst) - 1, -1, -1):
                if not lst[i].is_dma:
                    deps.append((eng, i))
                    break
            c = 0
            for i in range(len(lst) - 1, -1, -1):
                if lst[i].is_dma:
                    deps.append((eng, i))
                    c += 1
                    if c >= DMA_RING:
                        break
        self.pending = deps
        self.applied = set()
        self.lastw.clear()
        self.readers.clear()

    def _k(self, aps):
        ks = []
        for a in aps:
            if a is not None and not isinstance(a, (int, float)) and a.tensor.name in self.track:
                ks += self.keys(a)
        return ks

    def mm(self, out, lhsT, rhs, start=True, stop=True):
        return self._add('pe', lambda e: e.matmul(out, lhsT=lhsT, rhs=rhs, start=start, stop=stop),
                         self._k([lhsT, rhs]), self._k([out]), False)

    def tr(self, out, in_, ident):
        return self._add('pe', lambda e: e.transpose(out, in_, ident), self._k([in_, ident]), self._k([out]), False)

    def act(self, out, in_, func, bias=0.0, scale=1.0):
        return self._add('act', lambda e: e.activation(out=out, in_=in_, func=func, bias=bias, scale=scale),
                         self._k([in_, bias, scale]), self._k([out]), False)

    def tt(self, eng, out, in0, in1, op):
        return self._add(eng, lambda e: e.tensor_tensor(out=out, in0=in0, in1=in1, op=op),
                         self._k([in0, in1]), self._k([out]), False)

    def ts(self, eng, out, in0, s1, op0, s2=None, op1=None):
        if op1 is None:
            fn = lambda e: e.tensor_scalar(out=out, in0=in0, scalar1=s1, scalar2=None, op0=op0)
        else:
            fn = lambda e: e.tensor_scalar(out=out, in0=in0, scalar1=s1, scalar2=s2, op0=op0, op1=op1)
        return self._add(eng, fn, self._k([in0, s1, s2]), self._k([out]), False)

    def stt(self, eng, out, in0, scalar, in1, op0, op1):
        return self._add(eng, lambda e: e.scalar_tensor_tensor(out=out, in0=in0, scalar=scalar, in1=in1, op0=op0, op1=op1),
                         self._k([in0, scalar, in1]), self._k([out]), False)

    def scan(self, out, d0, d1, init, op0, op1):
        return self._add('dve', lambda e: e.tensor_tensor_scan(out=out, data0=d0, data1=d1, initial=init, op0=op0, op1=op1),
                         self._k([d0, d1]), self._k([out]), False)

    def copy(self, eng, out, in_):
        if eng == 'act':
            return self.act(out, in_, AF.Copy)
        return self._add(eng, lambda e: e.tensor_copy(out=out, in_=in_), self._k([in_]), self._k([out]), False)

    def recip(self, out, in_):
        return self._add('dve', lambda e: e.reciprocal(out=out, in_=in_), self._k([in_]), self._k([out]), False)

    def memset(self, eng, ap, val):
        return self._add(eng, lambda e: e.memset(ap, val), [], self._k([ap]), False)

    def dma(self, eng, out, in_):
        return self._add(eng, lambda e: e.dma_start(out=out, in_=in_), self._k([in_]), self._k([out]), True)

    def emit(self, final_ops):
        nc = self.nc
        with contextlib.ExitStack() as es:
            sems = {}
            tail = []
            for eng, lst in self.ops.items():
                for i in range(len(lst) - 1, -1, -1):
                    if not lst[i].is_dma:
                        lst[i].needs_inc = True
                        tail.append(lst[i])
                        break
                c_ = 0
                for i in range(len(lst) - 1, -1, -1):
                    if lst[i].is_dma:
                        tail.append(lst[i])
                        c_ += 1
                        if c_ >= DMA_RING:
                            break
            for eng, lst in self.ops.items():
                cnt = 0
                nd = 0
                for o in lst:
                    if o.is_dma:
                        slot, rnd = nd % DMA_RING, nd // DMA_RING
                        o.tok = (f"d_{eng}_{slot}", 16 * (rnd + 1))
                        if rnd > 0:
                            o.ring_wait = (f"d_{eng}_{slot}", 16 * rnd)
                        nd += 1
                    elif o.needs_inc:
                        g = cnt // SEM_GEN
                        o.tok = (f"c_{eng}_{g}", cnt - g * SEM_GEN + 1)
                        cnt += 1
            self.maxtok = {}
            for lst in self.ops.values():
                for o in lst:
                    if o.tok:
                        self.maxtok[o.tok[0]] = max(self.maxtok.get(o.tok[0], 0), o.tok[1])
            if os.environ.get('KSIM'):
                print('MAXTOK', self.maxtok)
            for lst in self.ops.values():
                for o in lst:
                    if o.tok and o.tok[0] not in sems:
                        sems[o.tok[0]] = es.enter_context(nc.semaphore(o.tok[0]))
            engmap = {'pe': 'tensor', 'act': 'scalar', 'dve': 'vector', 'pool': 'gpsimd', 'sp': 'sync'}
            plan = {}
            stats = {}
            for eng, lst in self.ops.items():
                known = {}
                nw = 0
                pl = []
                for o in lst:
                    need = {}
                    for d in o.deps:
                        t = self.ops[d[0]][d[1]].tok
                        if need.get(t[0], 0) < t[1]:
                            need[t[0]] = t[1]
                    if o.ring_wait:
                        t = o.ring_wait
                        if need.get(t[0], 0) < t[1]:
                            need[t[0]] = t[1]
                    ws = []
                    for s_, v in need.items():
                        if known.get(s_, 0) < v:
                            ws.append((s_, v))
                            known[s_] = v
                            nw += 1
                    pl.append(ws)
                plan[eng] = pl
                stats[eng] = (len(lst), nw)
            if os.environ.get('KSIM'):
                val = {k_: 0 for k_ in sems}
                pos = {e_: 0 for e_ in self.ops}
                prog = True
                while prog:
                    prog = False
                    for e_, lst in self.ops.items():
                        while pos[e_] < len(lst):
                            i_ = pos[e_]
                            if all(val[s_] >= v for s_, v in plan[e_][i_]):
                                o = lst[i_]
                                if o.tok is not None:
                                    val[o.tok[0]] += 16 if o.is_dma else 1
                                    assert val[o.tok[0]] == o.tok[1], (e_, i_, o.tok, val[o.tok[0]])
                                pos[e_] += 1
                                prog = True
                            else:
                                break
                for e_, lst in self.ops.items():
                    if pos[e_] < len(lst):
                        print("SIM DEADLOCK", e_, pos[e_], len(lst), plan[e_][pos[e_]], {s_: val[s_] for s_, v in plan[e_][pos[e_]]})
                print("SIM done", {e_: pos[e_] for e_ in pos})
            block = es.enter_context(nc.Block())
            for eng, lst in self.ops.items():
                def body(e, eng=eng, lst=lst):
                    for o, ws in zip(lst, plan[eng]):
                        for s_, v in ws:
                            e.wait_ge(sems[s_], v)
                        ins = o.fn(e)
                        if o.tok is not None:
                            ins.then_inc(sems[o.tok[0]], 16 if o.is_dma else 1)
                    for fo in list(final_ops) + tail:
                        e.wait_ge(sems[fo.tok[0]], fo.tok[1])
                getattr(block, engmap[eng])(body)
            self.stats = stats
        return stats


def build(L, dbg=False, phases=('pass', 'conv', 'attn', 'rwkv')):
    nc = bass.Bass("TRN2", target_bir_lowering=False)
    if FAST:
        nc.dge_precook = False

    def din(name, shape, dt=F32):
        return nc.dram_tensor(name, shape, dt, kind="ExternalInput").ap()

    x_in = din("x", [S, D])
    WDT = F32R if FAST else F32
    wgu = [din(f"wgu{i}", [L, NFC, 128, 2048], WDT) for i in (1, 2)]
    wd = [din(f"wd{i}", [L, 8, 128, NFC * 128], WDT) for i in (1, 2)]
    win = din("win", [L, NOC, 128, 1024], WDT)
    wout = din("wout", [L, 8, 128, 1024], WDT)
    lora_d = din("lora", [L, 128, 384])
    prm_d = din("prm", [128, L * NP])
    cst_d = din("cst", [128, NCST])
    abias_d = din("abias", [128, 18 * 256])
    out = nc.dram_tensor("out", [S, D], F32, kind="ExternalOutput").ap()
    skind = "ExternalOutput" if dbg else "Internal"
    xT = nc.dram_tensor("xT", [8, 128, S], F32, kind=skind).ap()
    projT = nc.dram_tensor("projT", [NOC, 128, S], F32, kind=skind).ap()
    mixT = nc.dram_tensor("mixT", [8, 128, S], F32R if FAST else F32, kind=skind).ap()

    P = Prog(nc)
    P.track = set()
    final_ops = []

    with contextlib.ExitStack() as gs:
        uid = [0]

        def sb(es, name, shape, fine=None, dt=F32):
            uid[0] += 1
            name = f"{name}_u{uid[0]}"
            t = es.enter_context(nc.sbuf_tensor(name, shape, dt))
            P.track.add(name)
            if fine:
                row = 1
                for s_ in shape[1:]:
                    row *= s_
                P.fine[name] = (fine, row)
            return t

        cst = sb(gs, "cst_s", [128, NCST], fine=128)
        prm = sb(gs, "prm_s", [128, L * NP])
        pbs = []
        for i in range(8):
            t = gs.enter_context(nc.psum_tensor(f"pb{i}", [128, 512], F32))
            P.track.add(f"pb{i}")
            pbs.append(t)
        P.dma('sp', cst[:], cst_d)
        P.dma('sp', prm[:], prm_d)
        ident = cst[:, C_ID:C_ID + 128]
        ones = cst[:, C_ONES:C_ONES + 128]
        bd64 = cst[:, C_BD:C_BD + 128]
        msu = cst[:, C_MSU:C_MSU + 128]
        msl = cst[:, C_MSL:C_MSL + 128]
        mu_ = cst[:, C_MU:C_MU + 128]
        rmask = cst[:, C_RM:C_RM + 512]

        def pcol(l, c, n=1):
            return prm[:, l * NP + c: l * NP + c + n]

        def run_pass(l):
            with contextlib.ExitStack() as es:
                xts = [sb(es, f"xt{i}", [128, 8, TT], fine=TT) for i in range(2)]
                tg = [sb(es, f"tg{i}", [128, TT]) for i in range(2)]
                cur = {}
                hT = sb(es, "hT", [128, 8, TT], dt=WDT)
                actT = sb(es, "actT", [128, NFC, TT], fine=TT, dt=WDT)
                sq = sb(es, "sq", [128, 8 * TT])
                rstd = sb(es, "rstd", [128, TT])
                sg = [sb(es, f"sg{i}", [128, TT]) for i in range(2)]
                wb = [sb(es, f"wb{i}", [128, 2048], dt=WDT) for i in range(3)]
                wdb = [sb(es, f"wdb{i}", [128, NFC * 128], dt=WDT) for i in range(2)]
                wb1 = [sb(es, f"wb1_{i}", [128, 1024], dt=WDT) for i in range(3)]
                pj = [sb(es, f"pj{i}", [128, TT]) for i in range(3)]
                xtok = [sb(es, f"xtok{i}", [128, D]) for i in range(2)]
                cnt = {'wb': 0, 'wd': 0, 'pj': 0, 'g': 0, 'o': 0}

                def norm_a(gcol0):
                    xt = cur['xt']
                    P.act(sq[:], xt[:].rearrange("p c t -> p (c t)"), AF.Square)
                    for dc in range(8):
                        P.ts('dve', hT[:, dc, :], xt[:, dc, :], pcol(l_cur[0], gcol0 + dc), ALU.mult)

                def norm_b():
                    for dc in range(8):
                        P.mm(pbs[0][:], ones, sq[:, dc * TT:(dc + 1) * TT], start=(dc == 0), stop=(dc == 7))
                    P.act(rstd[:], pbs[0][:], AF.Sqrt, bias=eps6[:, 0:1], scale=1.0 / D)
                    P.recip(rstd[:], rstd[:])

                def ffn(ll, which):
                    l_cur[0] = ll
                    xt = cur['xt']
                    norm_a(0 if which == 0 else 16)
                    for fc in range(NFC):
                        w = wb[cnt['wb'] % 3]
                        cnt['wb'] += 1
                        P.dma('sp', R(w[:]), wgu[which][ll, fc])
                        pg = pbs[1 + cnt['g'] % 2]
                        pu = pbs[3 + cnt['g'] % 2]
                        sgt = sg[cnt['g'] % 2]
                        cnt['g'] += 1
                        for dc in range(8):
                            P.mm(pg[:], R(w[:, dc * 128:(dc + 1) * 128]), R(hT[:, dc, :]), start=(dc == 0), stop=(dc == 7))
                        for dc in range(8):
                            P.mm(pu[:], R(w[:, 1024 + dc * 128:1024 + (dc + 1) * 128]), R(hT[:, dc, :]), start=(dc == 0), stop=(dc == 7))
                        if fc == 0:
                            norm_b()
                        t1 = tg[cnt['g'] % 2]
                        P.tt('dve', t1[:], pg[:], rstd[:], ALU.mult)
                        P.act(sgt[:], t1[:], AF.Silu)
                        P.tt('dve', t1[:], pu[:], rstd[:], ALU.mult)
                        P.tt('pool', R(actT[:, fc, :]), sgt[:], t1[:], ALU.mult)
                    for dc in range(8):
                        w = wdb[cnt['wd'] % 2]
                        cnt['wd'] += 1
                        P.dma('sp', R(w[:]), wd[which][ll, dc])
                        po = pbs[5 + cnt['o'] % 2]
                        cnt['o'] += 1
                        for fc in range(NFC):
                            P.mm(po[:], R(w[:, fc * 128:(fc + 1) * 128]), R(actT[:, fc, :]), start=(fc == 0), stop=(fc == NFC - 1))
                        P.stt('dve', xt[:, dc, :], po[:], 0.5, xt[:, dc, :], ALU.mult, ALU.add)

                def inproj(ll, tok0):
                    l_cur[0] = ll
                    xt = cur['xt']
                    norm_a(8)
                    for oc in range(NOC):
                        w = wb1[cnt['wb'] % 3]
                        cnt['wb'] += 1
                        P.dma('sp', w[:], win[ll, oc])
                        po = pbs[5 + cnt['o'] % 2]
                        cnt['o'] += 1
                        for dc in range(8):
                            P.mm(po[:], R(w[:, dc * 128:(dc + 1) * 128]), R(hT[:, dc, :]), start=(dc == 0), stop=(dc == 7))
                        pt = pj[cnt['pj'] % 3]
                        cnt['pj'] += 1
                        if oc == 0:
                            norm_b()
                        P.tt('dve', pt[:], po[:], rstd[:], ALU.mult)
                        P.dma('pool', projT[oc, :, tok0:tok0 + TT], pt[:])

                def outproj(ll, tok0):
                    xt = cur['xt']
                    mt = actT
                    P.dma('pool', R(mt[:, 0:8, :]), R(mixT[:, :, tok0:tok0 + TT]).rearrange("c p t -> p c t"))
                    for dc in range(8):
                        w = wb1[cnt['wb'] % 3]
                        cnt['wb'] += 1
                        P.dma('sp', w[:], wout[ll, dc])
                        po = pbs[5 + cnt['o'] % 2]
                        cnt['o'] += 1
                        for kc in range(8):
                            P.mm(po[:], R(w[:, kc * 128:(kc + 1) * 128]), R(mt[:, kc, :]), start=(kc == 0), stop=(kc == 7))
                        P.tt('dve', xt[:, dc, :], po[:], xt[:, dc, :], ALU.add)

                l_cur = [0]
                eps6 = sb(es, "eps6", [128, 1])
                P.memset('dve', eps6[:], 1e-6)
                def load_x(tt_):
                    xt = xts[tt_ % 2]
                    tok0 = tt_ * TT
                    if l == 0:
                        for s4 in range(4):
                            xk = xtok[s4 % 2]
                            P.dma('pool', xk[:], x_in[tok0 + s4 * 128: tok0 + (s4 + 1) * 128, :])
                            for half in range(2):
                                pb = pbs[5 + half]
                                for q in range(4):
                                    dc = half * 4 + q
                                    P.tr(pb[:, q * 128:(q + 1) * 128], xk[:, dc * 128:(dc + 1) * 128], ident)
                                P.copy('act' if half else 'dve', xt[:, half * 4:half * 4 + 4, s4 * 128:(s4 + 1) * 128],
                                       pb[:].rearrange("p (c t) -> p c t", t=128))
                    else:
                        P.dma('pool', xt[:], xT[:, :, tok0:tok0 + TT].rearrange("c p t -> p c t"))

                NTL = int(os.environ.get('KNT', NT))
                if l > 0:
                    load_x(0)
                for tt_ in range(NTL):
                    tok0 = tt_ * TT
                    xt = xts[tt_ % 2]
                    cur['xt'] = xt
                    if l == 0:
                        load_x(tt_)
                    elif tt_ + 1 < NTL:
                        load_x(tt_ + 1)
                    if l > 0:
                        if 'o' in KF:
                            outproj(l - 1, tok0)
                        if 'f' in KF:
                            ffn(l - 1, 1)
                    if l < L:
                        if 'f' in KF:
                            ffn(l, 0)
                        if 'i' in KF:
                            inproj(l, tok0)
                        P.dma('pool', xT[:, :, tok0:tok0 + TT].rearrange("c p t -> p c t"), xt[:])
                    else:
                        for s4 in range(4):
                            xk = xtok[s4 % 2]
                            for half in range(2):
                                pb = pbs[5 + half]
                                for q in range(4):
                                    dc = half * 4 + q
                                    P.tr(pb[:, q * 128:(q + 1) * 128], xt[:, dc, s4 * 128:(s4 + 1) * 128], ident)
                                P.copy('act' if half else 'dve', xk[:, half * 512:(half + 1) * 512], pb[:])
                            final_ops.append(P.dma('pool', out[tok0 + s4 * 128: tok0 + (s4 + 1) * 128, :], xk[:]))
            P.barrier()

        def run_conv(l):
            with contextlib.ExitStack() as es:
                a_t = sb(es, "cv_a", [128, S])
                b_t = sb(es, "cv_b", [128, S])
                zp = sb(es, "cv_zp", [128, 30 + S])
                zc = [sb(es, f"cv_zc{i}", [128, S]) for i in range(2)]
                sq = sb(es, "cv_sq", [128, TT])
                mean = sb(es, "cv_mean", [128, TT])
                var = sb(es, "cv_var", [128, TT])
                yt = [sb(es, f"cv_y{i}", [128, TT]) for i in range(2)]
                eps5 = sb(es, "eps5", [128, 1])
                P.memset('dve', eps5[:], 1e-5)
                P.memset('dve', zp[:, 0:30], 0.0)
                for ch in range(2):
                    P.dma('pool', a_t[:], projT[19 + ch, :, :])
                    P.dma('pool', b_t[:], projT[21 + ch, :, :])
                    for h in range(4):
                        sl = slice(h * 1024, (h + 1) * 1024)
                        P.act(b_t[:, sl], b_t[:, sl], AF.Sigmoid)
                    P.tt(os.environ.get('KCE', 'pool'), zp[:, 30:30 + S], a_t[:], b_t[:], ALU.mult)
                    if 'b' not in os.environ.get('KC', 'abc'):
                        continue
                    wc = 63 + ch * 31
                    acc = zc[ch]
                    P.ts('dve', acc[:], zp[:, 0:S], pcol(l, wc), ALU.mult, pcol(l, 57 + ch), ALU.add)
                    for j in range(1, 31):
                        P.stt('dve', acc[:], zp[:, j:j + S], pcol(l, wc + j), acc[:], ALU.mult, ALU.add)
                for tt_ in range(NT if 'c' in os.environ.get('KC', 'abc') else 0):
                    sl = slice(tt_ * TT, (tt_ + 1) * TT)
                    for ch in range(2):
                        P.mm(pbs[0][:], ones, zc[ch][:, sl], start=(ch == 0), stop=(ch == 1))
                    for ch in range(2):
                        P.act(sq[:], zc[ch][:, sl], AF.Square)
                        P.mm(pbs[1][:], ones, sq[:], start=(ch == 0), stop=(ch == 1))
                    P.ts('dve', mean[:], pbs[0][:], 1.0 / 256, ALU.mult)
                    P.tt('dve', var[:], mean[:], mean[:], ALU.mult)
                    P.stt('dve', var[:], pbs[1][:], 1.0 / 256, var[:], ALU.mult, ALU.subtract)
                    P.act(var[:], var[:], AF.Sqrt, bias=eps5[:, 0:1], scale=1.0)
                    P.recip(var[:], var[:])
                    for ch in range(2):
                        y = yt[ch]
                        P.tt('dve', y[:], zc[ch][:, sl], mean[:], ALU.subtract)
                        P.tt('dve', y[:], y[:], var[:], ALU.mult)
                        P.act(y[:], y[:], AF.Silu, bias=pcol(l, 61 + ch), scale=pcol(l, 59 + ch))
                        P.dma('pool', mixT[6 + ch, :, sl], y[:])
            P.barrier()

        def run_attn(l):
            with contextlib.ExitStack() as es:
                q = sb(es, "at_q", [128, S])
                k = sb(es, "at_k", [128, S])
                v = sb(es, "at_v", [128, S])
                qn = sb(es, "at_qn", [128, S], dt=WDT)
                kn = sb(es, "at_kn", [128, S], dt=WDT)
                acc = [sb(es, f"at_acc{i}", [128, S]) for i in range(2)]
                vaug = [sb(es, f"at_va{i}", [128, 32, 128], fine=128, dt=WDT) for i in range(2)]
                ab = sb(es, "at_bias", [128, 18 * 256], fine=256)
                et = [sb(es, f"at_e{i}", [128, 256], dt=WDT) for i in range(4)]
                e0 = [sb(es, f"at_e0{i}", [128, 256]) for i in range(4)]
                sq = sb(es, "at_sq", [128, TT])
                rs = sb(es, "at_rs", [128, TT])
                gq = sb(es, "at_gq", [128, 1])
                eps6 = sb(es, "at_eps", [128, 1])
                P.memset('dve', eps6[:], 1e-6)
                P.dma('sp', ab[:], abias_d)
                P.ts('dve', gq[:], pcol(l, 55), 0.125, ALU.mult)
                fin_ = ab[:, 0:2048].rearrange("p (b c) -> p b c", c=64)
                P.ts('dve', vaug[0][:, :, 64:128], fin_, 0.0, ALU.mult, 1.0, ALU.add)
                P.ts('dve', vaug[1][:, :, 0:64], fin_, 0.0, ALU.mult, 1.0, ALU.add)
                ne = 0
                for c in range(int(os.environ.get('KACH', 3))):
                    P.dma('pool', q[:], projT[10 + c, :, :])
                    P.dma('pool', k[:], projT[13 + c, :, :])
                    P.dma('pool', v[:], projT[16 + c, :, :])
                    for (t_, tn_, gcol) in ((q, qn, gq[:, 0:1]), (k, kn, pcol(l, 56))):
                        for tt_ in range(NT):
                            sl = slice(tt_ * TT, (tt_ + 1) * TT)
                            P.act(sq[:], t_[:, sl], AF.Square)
                            P.mm(pbs[0][:], bd64, sq[:])
                            P.act(rs[:], pbs[0][:], AF.Sqrt, bias=eps6[:, 0:1], scale=1.0 / 64)
                            P.recip(rs[:], rs[:])
                            P.stt('dve', tn_[:, sl], t_[:, sl], gcol, rs[:], ALU.mult, ALU.mult)
                    P.memset('pool', acc[0][:], 0.0)
                    P.memset('pool', acc[1][:], 0.0)
                    for pi in [int(ch_) for ch_ in os.environ.get('KAP', '012')]:
                        win_, d = PATTERNS[pi]
                        nb = 32 // d
                        Lsub = S // d
                        if os.environ.get('KAB'):
                            P.barrier()
                        for cls in range(d):
                            for kb in range(nb):
                                b = cls * nb + kb
                                st = cls + d * kb * 128
                                pb = pbs[1 + b % 2]
                                P.tr(pb[:, 0:128], v[:, st: st + d * 127 + 1: d] if d > 1 else v[:, st:st + 128], ident)
                                ce = 'act' if b % 2 else 'dve'
                                P.copy(ce, vaug[0][:, b, 0:64], pb[:, 0:64])
                                P.copy(ce, vaug[1][:, b, 64:128], pb[:, 64:128])
                        blocks = []
                        for hh in range(2):
                            for cls in range(d):
                                for kb in range(nb):
                                    blocks.append((hh, cls, kb))
                        pend = []

                        def emit_pv(blk):
                            hh, cls, kb, e_, nq, ks, i_ = blk
                            b = cls * nb + kb
                            po_ = pbs[5 + i_ % 2]
                            P.mm(po_[:, 0:nq], vaug[hh][:, b, :], e_[:, 0:nq])
                            aap = acc[hh][:, ks: ks + d * (nq - 1) + 1: d] if d > 1 else acc[hh][:, ks:ks + nq]
                            P.tt('dve', aap, aap, po_[:, 0:nq], ALU.add)

                        for (hh, cls, kb) in blocks:
                            h = 2 * c + hh
                            hp = slice(hh * 64, hh * 64 + 64)
                            nq = min(256, Lsub - kb * 128)
                            ks = cls + d * kb * 128
                            kap = kn[hp, ks: ks + d * 127 + 1: d] if d > 1 else kn[hp, ks:ks + 128]
                            qap = qn[hp, ks: ks + d * (nq - 1) + 1: d] if d > 1 else qn[hp, ks:ks + nq]
                            ps_ = pbs[(3, 4, 7)[ne % 3]]
                            e_ = et[ne % 4]
                            ee = e0[ne % 4]
                            P.mm(ps_[:, 0:nq], kap, qap, start=True, stop=True)
                            bi = (h * 3 + pi) * 256
                            P.act(ee[:, 0:nq], ps_[:, 0:nq], AF.Exp)
                            P.tt('pool', e_[:, 0:nq], ee[:, 0:nq], ab[:, bi:bi + nq], ALU.mult)
                            pend.append((hh, cls, kb, e_, nq, ks, ne))
                            ne += 1
                            if len(pend) > 2:
                                emit_pv(pend.pop(0))
                        while pend:
                            emit_pv(pend.pop(0))
                    for h4 in range(4):
                        sl = slice(h4 * 1024, (h4 + 1) * 1024)
                        P.copy('dve', q[0:64, sl], acc[0][64:128, sl])
                        P.recip(q[0:64, sl], q[0:64, sl])
                        P.tt('dve', q[0:64, sl], acc[0][0:64, sl], q[0:64, sl], ALU.mult)
                        P.copy('act', q[64:128, sl], acc[1][0:64, sl])
                        P.recip(q[64:128, sl], q[64:128, sl])
                        P.tt('dve', q[64:128, sl], acc[1][64:128, sl], q[64:128, sl], ALU.mult)
                    P.dma('pool', mixT[3 + c, :, :], q[:])
            P.barrier()

        def run_rwkv(l):
            SEG = 512
            NSEG = S // SEG
            PCM = ('N', 'NT', 'N2', 'N2T', 'Pm', 'Pm2', 'Aak', 'Arb', 'Ark', 'bT', 'kT', 'Vb')
            NCH = SEG // 64
            with contextlib.ExitStack() as es:
                lora = sb(es, "rw_lora", [128, 384])
                P.dma('sp', lora[:], lora_d[l])
                eps12 = sb(es, "rw_e12", [128, 1])
                P.memset('dve', eps12[:], 1e-12)
                epsgn = sb(es, "rw_egn", [128, 1])
                P.memset('dve', epsgn[:], 64e-5)
                praw = sb(es, "rw_praw", [128, 1 + SEG])
                x9 = sb(es, "rw_x9", [128, SEG])
                tmp = sb(es, "rw_tmp", [128, SEG])
                tmp2 = sb(es, "rw_tmp2", [128, SEG])
                pr = []
                for c in range(3):
                    d_ = {}
                    for nm in ('r', 'k', 'v', 'a', 'kk', 'lw', 'cum', 'g', 'bonus', 'y', 'wc'):
                        d_[nm] = sb(es, f"rw_{nm}{c}", [128, SEG])
                    for nm in ('at', 'rt', 'bt', 'kt', 'vb'):
                        d_[nm] = sb(es, f"rw_{nm}bd{c}", [128, NCH, 128], fine=128)
                        P.memset('pool', d_[nm][:], 0.0)
                    d_['ST'] = [sb(es, f"rw_ST{c}_{i}", [128, 128]) for i in range(2)]
                    P.memset('dve', d_['ST'][0][:], 0.0)
                    for nm in PCM:
                        d_[nm] = [sb(es, f"rw_{nm}{c}_{i}", [128, 128]) for i in range(2)]
                    for nm in ('XT', 'Ub'):
                        d_[nm] = sb(es, f"rw_{nm}{c}", [128, 128])
                    d_['wend'] = sb(es, f"rw_wend{c}", [128, NCH])
                    pr.append(d_)
                stp = [0, 0, 0]

                def shift_load(dst, ch, tok0, mucol):
                    if tok0 == 0:
                        P.memset('dve', praw[:, 0:1], 0.0)
                        P.dma('pool', praw[:, 1:1 + SEG], projT[ch, :, 0:SEG])
                    else:
                        P.dma('pool', praw[:], projT[ch, :, tok0 - 1:tok0 + SEG])
                    P.tt('dve', tmp[:], praw[:, 0:SEG], praw[:, 1:1 + SEG], ALU.subtract)
                    P.stt('dve', dst, tmp[:], mucol, praw[:, 1:1 + SEG], ALU.mult, ALU.add)

                for seg in range(int(os.environ.get('KSEG', NSEG))):
                    tok0 = seg * SEG
                    shift_load(x9[:], 9, tok0, pcol(l, 24 + 9))
                    P.act(x9[0:32, :], x9[0:32, :], AF.Tanh)
                    P.act(x9[64:128, :], x9[64:128, :], AF.Sigmoid)
                    for c in range(3 if 'b' in os.environ.get('KRW', 'abcd') else 0):
                        d_ = pr[c]
                        cs = slice(c * 128, (c + 1) * 128)
                        shift_load(d_['r'][:], c, tok0, pcol(l, 24 + c))
                        shift_load(d_['k'][:], 3 + c, tok0, pcol(l, 24 + 3 + c))
                        shift_load(d_['v'][:], 6 + c, tok0, pcol(l, 24 + 6 + c))
                        P.mm(pbs[0][:], lora[0:32, cs], x9[0:32, :])
                        P.act(d_['lw'][:], pbs[0][:], AF.Sigmoid, bias=pcol(l, 34 + c), scale=1.0)
                        P.mm(pbs[1][:], lora[32:64, cs], x9[32:64, :])
                        P.act(d_['a'][:], pbs[1][:], AF.Sigmoid, bias=pcol(l, 37 + c), scale=1.0)
                        P.mm(pbs[2][:], lora[64:128, cs], x9[64:128, :])
                        P.copy('act', d_['g'][:], pbs[2][:])
                        P.ts('dve', d_['kk'][:], d_['k'][:], pcol(l, 40 + c), ALU.mult)
                        P.act(tmp2[:], d_['kk'][:], AF.Square)
                        P.mm(pbs[0][:], bd64, tmp2[:])
                        P.act(tmp2[:], pbs[0][:], AF.Sqrt, bias=eps12[:, 0:1], scale=1.0)
                        P.recip(tmp2[:], tmp2[:])
                        P.tt('dve', d_['kk'][:], d_['kk'][:], tmp2[:], ALU.mult)
                        P.ts('dve', tmp2[:], d_['a'][:], -1.0, ALU.add, pcol(l, 43 + c), ALU.mult)
                        P.stt('dve', d_['k'][:], tmp2[:], 1.0, d_['k'][:], ALU.add, ALU.mult)
                        P.tt('pool', tmp2[:], d_['r'][:], d_['k'][:], ALU.mult)
                        P.ts('pool', tmp2[:], tmp2[:], pcol(l, 46 + c), ALU.mult)
                        P.mm(pbs[1][:], bd64, tmp2[:])
                        P.tt('dve', d_['bonus'][:], pbs[1][:], d_['v'][:], ALU.mult)
                        P.ts('dve', d_['lw'][:], d_['lw'][:], -EXPM05, ALU.mult)
                        P.scan(d_['cum'][:], rmask, d_['lw'][:], 0.0, ALU.mult, ALU.add)
                        P.act(d_['wc'][:], d_['cum'][:], AF.Exp)
                        P.copy('dve', d_['wend'][:], d_['wc'][:].rearrange("p (c t) -> p c t", t=64)[:, :, 63])
                        P.tt('dve', tmp2[:], d_['cum'][:], d_['lw'][:], ALU.subtract)
                        P.act(tmp2[:], tmp2[:], AF.Exp)
                        P.tt('dve', tmp2[:], tmp2[:], d_['kk'][:], ALU.mult)
                        P.act(tmp[:], d_['cum'][:], AF.Exp, scale=-1.0)
                        P.tt('pool', d_['y'][:], d_['kk'][:], d_['a'][:], ALU.mult)
                        for hh in range(2):
                            hp = slice(hh * 64, hh * 64 + 64)
                            cp = slice(hh * 64, hh * 64 + 64)

                            def v3(t):
                                return t[hp, :].rearrange("p (c t) -> p c t", t=64)
                            P.ts('dve', d_['at'][hp, :, cp], v3(tmp2), -1.0, ALU.mult)
                            P.tt('dve', d_['rt'][hp, :, cp], v3(d_['r']), v3(d_['wc']), ALU.mult)
                            P.tt('dve', d_['bt'][hp, :, cp], v3(d_['y']), v3(tmp), ALU.mult)
                            P.tt('dve', d_['kt'][hp, :, cp], v3(d_['k']), v3(tmp), ALU.mult)
                            P.copy('pool', d_['vb'][hp, :, cp], v3(d_['v']))
                    def pre_gen(n, c):
                        d_ = pr[c]
                        par = n % 2
                        M = {nm: d_[nm][par] for nm in PCM}
                        at, rt, bt, kt, vb = (d_[x][:, n, :] for x in ('at', 'rt', 'bt', 'kt', 'vb'))
                        pq = [pbs[c * 2][:, 0:128], pbs[c * 2][:, 128:256], pbs[c * 2][:, 256:384], pbs[c * 2][:, 384:512],
                              pbs[c * 2 + 1][:, 0:128], pbs[c * 2 + 1][:, 128:256], pbs[c * 2 + 1][:, 256:384], pbs[c * 2 + 1][:, 384:512]]
                        P.mm(pq[0], bt, at)
                        P.mm(pq[1], at, bt)
                        P.mm(pq[2], kt, at)
                        P.mm(pq[3], bt, rt)
                        P.mm(pq[4], kt, rt)
                        P.tr(pq[5], bt, ident)
                        P.tr(pq[6], kt, ident)
                        P.tr(pq[7], vb, ident)
                        yield
                        P.tt('dve', M['N'][:], pq[0], msu, ALU.mult)
                        P.tt('dve', M['NT'][:], pq[1], msl, ALU.mult)
                        P.tt('dve', M['Aak'][:], pq[2], msu, ALU.mult)
                        P.tt('dve', M['Arb'][:], pq[3], mu_, ALU.mult)
                        P.tt('dve', M['Ark'][:], pq[4], mu_, ALU.mult)
                        P.copy('dve', M['bT'][:], pq[5])
                        P.copy('dve', M['kT'][:], pq[6])
                        P.copy('dve', M['Vb'][:], pq[7])
                        P.tt('dve', M['Pm'][:], M['N'][:], ident, ALU.add)
                        yield
                        cur, curT, nxt, nxtT = M['N'], M['NT'], M['N2'], M['N2T']
                        Pc, Pn = M['Pm'], M['Pm2']
                        for lev in range(5):
                            P.mm(pq[0], cur[:], curT[:])
                            if lev < 4:
                                P.mm(pq[4], curT[:], cur[:])
                            if lev > 0:
                                P.mm(pq[5], curT[:], Pc[:])
                            yield
                            P.copy('act', nxtT[:], pq[0])
                            if lev < 4:
                                P.copy('dve', nxt[:], pq[4])
                            if lev > 0:
                                P.tt('dve', Pn[:], pq[5], Pc[:], ALU.add)
                                Pc, Pn = Pn, Pc
                            yield
                            cur, curT, nxt, nxtT = nxt, nxtT, cur, curT
                        P.mm(pq[5], curT[:], Pc[:])
                        yield
                        P.tt('dve', Pn[:], pq[5], Pc[:], ALU.add)
                        minv[(c, n)] = Pn
                        yield

                    def seq_gen(n, c):
                        d_ = pr[c]
                        par = n % 2
                        M = {nm: d_[nm][par] for nm in PCM}
                        at, rt = d_['at'][:, n, :], d_['rt'][:, n, :]
                        Minv = minv[(c, n)]
                        pa = pbs[6][:, c * 128:(c + 1) * 128]
                        pd = pbs[7][:, c * 128:(c + 1) * 128]
                        ST0 = d_['ST'][stp[c] % 2]
                        ST1 = d_['ST'][(stp[c] + 1) % 2]
                        stp[c] += 1
                        P.mm(pa, at, ST0[:], start=True, stop=False)
                        P.mm(pa, M['Aak'][:], M['Vb'][:], start=False, stop=True)
                        yield
                        P.copy('act', d_['XT'][:], pa)
                        yield
                        P.mm(pd, Minv[:], d_['XT'][:])
                        yield
                        P.copy('dve', d_['Ub'][:], pd)
                        yield
                        P.mm(pd, ident, ST0[:], start=True, stop=False)
                        P.mm(pd, M['bT'][:], d_['Ub'][:], start=False, stop=False)
                        P.mm(pd, M['kT'][:], M['Vb'][:], start=False, stop=True)
                        P.mm(pa, ST0[:], rt, start=True, stop=False)
                        P.mm(pa, d_['Ub'][:], M['Arb'][:], start=False, stop=False)
                        P.mm(pa, M['Vb'][:], M['Ark'][:], start=False, stop=True)
                        yield
                        P.ts('dve', ST1[:], pd, d_['wend'][:, n:n + 1], ALU.mult)
                        P.copy('act', d_['y'][0:64, n * 64:(n + 1) * 64], pa[0:64, 0:64])
                        P.copy('act', d_['y'][64:128, n * 64:(n + 1) * 64], pa[64:128, 64:128])
                        yield

                    minv = {}
                    for n in range(NCH + 1):
                        gens = []
                        if n < NCH:
                            gens += [pre_gen(n, c) for c in range(3)]
                        if n >= 1:
                            gens += [seq_gen(n - 1, c) for c in range(3)]
                        offs = [int(x_) for x_ in os.environ.get('KOFF', '0,1,2,1,2,3').split(',')]
                        done_ = [False] * len(gens)
                        tick = 0
                        while not all(done_):
                            for gi, g_ in enumerate(gens):
                                if done_[gi] or tick < offs[gi % len(offs)]:
                                    continue
                                try:
                                    next(g_)
                                except StopIteration:
                                    done_[gi] = True
                            tick += 1
                    for c in range(3 if 'd' in os.environ.get('KRW', 'abcd') else 0):
                        d_ = pr[c]
                        P.mm(pbs[6][:], bd64, d_['y'][:])
                        P.ts('dve', tmp[:], pbs[6][:], 1.0 / 64, ALU.mult)
                        P.tt('dve', d_['y'][:], d_['y'][:], tmp[:], ALU.subtract)
                        P.act(tmp2[:], d_['y'][:], AF.Square)
                        P.mm(pbs[7][:], bd64, tmp2[:])
                        P.act(tmp2[:], pbs[7][:], AF.Sqrt, bias=epsgn[:, 0:1], scale=1.0 / 64)
                        P.recip(tmp2[:], tmp2[:])
                        P.tt('dve', d_['y'][:], d_['y'][:], tmp2[:], ALU.mult)
                        P.ts('dve', d_['y'][:], d_['y'][:], pcol(l, 49 + c), ALU.mult, pcol(l, 52 + c), ALU.add)
                        P.tt('dve', d_['y'][:], d_['y'][:], d_['bonus'][:], ALU.add)
                        P.tt('dve', d_['wc'][:], d_['y'][:], d_['g'][:], ALU.mult)
                        P.dma('pool', mixT[c, :, tok0:tok0 + SEG], d_['wc'][:])
            P.barrier()

        P.barrier()
        for l in range(L + 1):
            if 'pass' in phases:
                run_pass(l)
            if l < L:
                if 'conv' in phases:
                    run_conv(l)
                if 'attn' in phases:
                    run_attn(l)
                if 'rwkv' in phases:
                    run_rwkv(l)
        st = P.emit(final_ops)
    return nc, st


def _alibi_slopes(n):
    def pow2(m):
        start = 2.0 ** (-8.0 / m)
        return [start ** (i + 1) for i in range(m)]
    if math.log2(n).is_integer():
        return pow2(n)
    c = 2 ** int(math.floor(math.log2(n)))
    return pow2(c) + pow2(2 * c)[0::2][: n - c]


def _consts():
    cst = np.zeros((128, NCST), np.float32)
    cst[:, C_ID:C_ID + 128] = np.eye(128, dtype=np.float32)
    cst[:, C_ONES:C_ONES + 128] = 1.0
    i = np.arange(128)
    same = (i[:, None] // 64) == (i[None, :] // 64)
    cst[:, C_BD:C_BD + 128] = same
    loc = i % 64
    cst[:, C_MSU:C_MSU + 128] = same & (loc[:, None] < loc[None, :])
    cst[:, C_MSL:C_MSL + 128] = same & (loc[:, None] > loc[None, :])
    cst[:, C_MU:C_MU + 128] = same & (loc[:, None] <= loc[None, :])
    cst[:, C_RM:C_RM + 512] = (np.arange(512) % 64 != 0).astype(np.float32)[None, :]
    slopes = _alibi_slopes(6)
    ab = np.zeros((128, 18, 256), np.float32)
    kk = np.arange(128)[:, None]
    qq = np.arange(256)[None, :]
    dist = qq - kk
    for h in range(6):
        for pi, (w, d) in enumerate(PATTERNS):
            valid = (dist >= 0) & (dist <= w // d)
            ab[:, h * 3 + pi, :] = np.where(valid, np.exp(-slopes[h] * (dist * d).astype(np.float64)), 0.0).astype(np.float32)
    return cst, ab.reshape(128, 18 * 256)


def _cols(v):
    v = np.asarray(v, np.float32).reshape(-1, 128)
    return v.T


def _prep(inp, L):
    f = lambda a: np.ascontiguousarray(np.asarray(a, np.float32))
    m = {}
    for i, nm in ((1, 'ffn1'), (2, 'ffn2')):
        g = f(inp[f'{nm}_w_gate'])[:L].reshape(L, 8, 128, NFC, 128).transpose(0, 3, 2, 1, 4)
        u = f(inp[f'{nm}_w_up'])[:L].reshape(L, 8, 128, NFC, 128).transpose(0, 3, 2, 1, 4)
        m[f'wgu{i}'] = np.ascontiguousarray(np.stack([g, u], axis=3)).reshape(L, NFC, 128, 2048)
        dn = f(inp[f'{nm}_w_down'])[:L].reshape(L, NFC, 128, 8, 128).transpose(0, 3, 2, 1, 4)
        m[f'wd{i}'] = np.ascontiguousarray(dn).reshape(L, 8, 128, NFC * 128)
    m['win'] = np.ascontiguousarray(f(inp['w_in'])[:L].reshape(L, 8, 128, NOC, 128).transpose(0, 3, 2, 1, 4)).reshape(L, NOC, 128, 1024)
    m['wout'] = np.ascontiguousarray(f(inp['w_out'])[:L].reshape(L, 8, 128, 8, 128).transpose(0, 3, 2, 1, 4)).reshape(L, 8, 128, 1024)
    m['lora'] = np.ascontiguousarray(np.concatenate([f(inp['rwkv_w2'])[:L], f(inp['rwkv_a2'])[:L], f(inp['rwkv_g2'])[:L]], axis=1))
    prm = np.zeros((128, L, NP), np.float32)
    for l in range(L):
        prm[:, l, 0:8] = _cols(inp['norm_ffn1'][l])
        prm[:, l, 8:16] = _cols(inp['norm_mix'][l])
        prm[:, l, 16:24] = _cols(inp['norm_ffn2'][l])
        prm[:, l, 24:34] = _cols(inp['shift_mu'][l])
        prm[:, l, 34:37] = _cols(inp['rwkv_w0'][l])
        prm[:, l, 37:40] = _cols(inp['rwkv_a0'][l])
        prm[:, l, 40:43] = _cols(inp['rwkv_k_k'][l])
        prm[:, l, 43:46] = _cols(inp['rwkv_k_a'][l])
        prm[:, l, 46:49] = _cols(np.asarray(inp['rwkv_r_k'][l]).reshape(-1))
        prm[:, l, 49:52] = _cols(inp['rwkv_ln_w'][l])
        prm[:, l, 52:55] = _cols(inp['rwkv_ln_b'][l])
        prm[:, l, 55] = np.tile(np.asarray(inp['attn_q_norm'][l], np.float32), 2)
        prm[:, l, 56] = np.tile(np.asarray(inp['attn_k_norm'][l], np.float32), 2)
        prm[:, l, 57:59] = _cols(inp['conv_dw_b'][l])
        prm[:, l, 59:61] = _cols(inp['conv_ln_w'][l])
        prm[:, l, 61:63] = _cols(inp['conv_ln_b'][l])
        cw = np.asarray(inp['conv_dw_w'][l], np.float32)
        for ch in range(2):
            prm[:, l, 63 + ch * 31: 63 + (ch + 1) * 31] = cw[:, ch * 128:(ch + 1) * 128].T
    m['prm'] = np.ascontiguousarray(prm.reshape(128, L * NP))
    cst, ab = _consts()
    m['cst'] = cst
    m['abias'] = ab
    return m


_CACHE = {}


def kernel(**inputs):
    L = 4
    x = np.ascontiguousarray(np.asarray(inputs['x'], np.float32))
    B = x.shape[0]
    if 'nc' not in _CACHE:
        _CACHE['nc'] = build(L)[0]
    nc = _CACHE['nc']
    shared = _prep(inputs, L)
    in_maps = []
    for b in range(B):
        d = dict(shared)
        d['x'] = x[b]
        in_maps.append(d)
    res = run_bass_kernel_spmd(nc, in_maps, core_ids=list(range(B)))
    return np.stack([np.asarray(r['out'], np.float32) for r in res.results], axis=0)
```

```python
import contextlib
import math
import os
import numpy as np
import concourse.bass as bass
import concourse.mybir as mybir
from concourse.bass_utils import run_bass_kernel_spmd

F32 = mybir.dt.float32
F32R = mybir.dt.float32r
FAST = os.environ.get('KFAST', '1') == '1'


def R(ap):
    return ap
ALU = mybir.AluOpType
AF = mybir.ActivationFunctionType

S = 4096
D = 1024
DFF = 2816
NFC = 22
NOC = 23
TT = 512
NT = S // TT
NP = 128
C_ID, C_ONES, C_BD, C_MSU, C_MSL, C_MU, C_RM = 0, 128, 256, 384, 512, 640, 768
NCST = 768 + 512
PATTERNS = ((128, 1), (512, 4), (2048, 16))
EXPM05 = math.exp(-0.5)

SEM_GEN = 8000
KF = os.environ.get('KF', 'ofi')
DMA_RING = 6


class Op:
    __slots__ = ('eng', 'fn', 'deps', 'is_dma', 'needs_inc', 'tok', 'ring_wait')

    def __init__(self, eng, fn, is_dma):
        self.eng = eng
        self.fn = fn
        self.deps = []
        self.is_dma = is_dma
        self.needs_inc = is_dma
        self.tok = None
        self.ring_wait = None


class Prog:
    def __init__(self, nc):
        self.nc = nc
        self.ops = {e: [] for e in ('pe', 'act', 'dve', 'pool', 'sp')}
        self.lastw = {}
        self.readers = {}
        self.fine = {}
        self.pending = None
        self.applied = set()

    def keys(self, ap):
        name = ap.tensor.name
        g = self.fine.get(name)
        if g is None:
            return [name]
        gran, row = g
        off = ap.offset % row
        span = 1
        for st, cnt in list(ap.ap)[1:]:
            span += (cnt - 1) * st
        return [(name, i) for i in range(off // gran, (off + span - 1) // gran + 1)]

    def _add(self, eng, fn, reads, writes, is_dma):
        o = Op(eng, fn, is_dma)
        lst = self.ops[eng]
        me = (eng, len(lst))
        lst.append(o)
        reads = list(dict.fromkeys(reads))
        writes = list(dict.fromkeys(writes))
        deps = set()
        for k in reads:
            w = self.lastw.get(k)
            if w is not None:
                if self.ops[w[0]][w[1]].is_dma or is_dma or w[0] != eng or eng != 'pe':
                    deps.add(w)
            if isinstance(k, str) and k.startswith('pb'):
                for r in self.readers.get(k, ()):
                    if r[0] != eng:
                        deps.add(r)
        for k in writes:
            w = self.lastw.get(k)
            if w is not None:
                if self.ops[w[0]][w[1]].is_dma or w[0] != eng or is_dma or eng != 'pe':
                    deps.add(w)
            for r in self.readers.get(k, ()):
                if self.ops[r[0]][r[1]].is_dma or r[0] != eng or is_dma or eng != 'pe':
                    deps.add(r)
        if self.pending is not None and eng not in self.applied:
            deps.update(self.pending)
            self.applied.add(eng)
        deps.discard(me)
        o.deps = list(deps)
        for d in o.deps:
            self.ops[d[0]][d[1]].needs_inc = True
        for k in reads:
            rl = self.readers.setdefault(k, [])
            if not is_dma:
                rl[:] = [r for r in rl if r[0] != eng or self.ops[r[0]][r[1]].is_dma]
            rl.append(me)
        for k in writes:
            self.lastw[k] = me
            self.readers[k] = []
        return o

    def barrier(self):
        deps = []
        for eng, lst in self.ops.items():
            for i in range(len(lst) - 1, -1, -1):
                if not lst[i].is_dma:
                    deps.append((eng, i))
                    break
            c = 0
            for i in range(len(lst) - 1, -1, -1):
                if lst[i].is_dma:
                    deps.append((eng, i))
                    c += 1
                    if c >= DMA_RING:
                        break
        self.pending = deps
        self.applied = set()
        self.lastw.clear()
        self.readers.clear()

    def _k(self, aps):
        ks = []
        for a in aps:
            if a is not None and not isinstance(a, (int, float)) and a.tensor.name in self.track:
                ks += self.keys(a)
        return ks

    def mm(self, out, lhsT, rhs, start=True, stop=True):
        return self._add('pe', lambda e: e.matmul(out, lhsT=lhsT, rhs=rhs, start=start, stop=stop),
                         self._k([lhsT, rhs]), self._k([out]), False)

    def tr(self, out, in_, ident):
        return self._add('pe', lambda e: e.transpose(out, in_, ident), self._k([in_, ident]), self._k([out]), False)

    def act(self, out, in_, func, bias=0.0, scale=1.0):
        return self._add('act', lambda e: e.activation(out=out, in_=in_, func=func, bias=bias, scale=scale),
                         self._k([in_, bias, scale]), self._k([out]), False)

    def tt(self, eng, out, in0, in1, op):
        return self._add(eng, lambda e: e.tensor_tensor(out=out, in0=in0, in1=in1, op=op),
                         self._k([in0, in1]), self._k([out]), False)

    def ts(self, eng, out, in0, s1, op0, s2=None, op1=None):
        if op1 is None:
            fn = lambda e: e.tensor_scalar(out=out, in0=in0, scalar1=s1, scalar2=None, op0=op0)
        else:
            fn = lambda e: e.tensor_scalar(out=out, in0=in0, scalar1=s1, scalar2=s2, op0=op0, op1=op1)
        return self._add(eng, fn, self._k([in0, s1, s2]), self._k([out]), False)

    def stt(self, eng, out, in0, scalar, in1, op0, op1):
        return self._add(eng, lambda e: e.scalar_tensor_tensor(out=out, in0=in0, scalar=scalar, in1=in1, op0=op0, op1=op1),
                         self._k([in0, scalar, in1]), self._k([out]), False)

    def scan(self, out, d0, d1, init, op0, op1):
        return self._add('dve', lambda e: e.tensor_tensor_scan(out=out, data0=d0, data1=d1, initial=init, op0=op0, op1=op1),
                         self._k([d0, d1]), self._k([out]), False)

    def copy(self, eng, out, in_):
        if eng == 'act':
            return self.act(out, in_, AF.Copy)
        return self._add(eng, lambda e: e.tensor_copy(out=out, in_=in_), self._k([in_]), self._k([out]), False)

    def recip(self, out, in_):
        return self._add('dve', lambda e: e.reciprocal(out=out, in_=in_), self._k([in_]), self._k([out]), False)

    def memset(self, eng, ap, val):
        return self._add(eng, lambda e: e.memset(ap, val), [], self._k([ap]), False)

    def dma(self, eng, out, in_):
        return self._add(eng, lambda e: e.dma_start(out=out, in_=in_), self._k([in_]), self._k([out]), True)

    def emit(self, final_ops):
        nc = self.nc
        with contextlib.ExitStack() as es:
            sems = {}
            tail = []
            for eng, lst in self.ops.items():
                for i in range(len(lst) - 1, -1, -1):
                    if not lst[i].is_dma:
                        lst[i].needs_inc = True
                        tail.append(lst[i])
                        break
                c_ = 0
                for i in range(len(lst) - 1, -1, -1):
                    if lst[i].is_dma:
                        tail.append(lst[i])
                        c_ += 1
                        if c_ >= DMA_RING:
                            break
            for eng, lst in self.ops.items():
                cnt = 0
                nd = 0
                for o in lst:
                    if o.is_dma:
                        slot, rnd = nd % DMA_RING, nd // DMA_RING
                        o.tok = (f"d_{eng}_{slot}", 16 * (rnd + 1))
                        if rnd > 0:
                            o.ring_wait = (f"d_{eng}_{slot}", 16 * rnd)
                        nd += 1
                    elif o.needs_inc:
                        g = cnt // SEM_GEN
                        o.tok = (f"c_{eng}_{g}", cnt - g * SEM_GEN + 1)
                        cnt += 1
            self.maxtok = {}
            for lst in self.ops.values():
                for o in lst:
                    if o.tok:
                        self.maxtok[o.tok[0]] = max(self.maxtok.get(o.tok[0], 0), o.tok[1])
            if os.environ.get('KSIM'):
                print('MAXTOK', self.maxtok)
            for lst in self.ops.values():
                for o in lst:
                    if o.tok and o.tok[0] not in sems:
                        sems[o.tok[0]] = es.enter_context(nc.semaphore(o.tok[0]))
            engmap = {'pe': 'tensor', 'act': 'scalar', 'dve': 'vector', 'pool': 'gpsimd', 'sp': 'sync'}
            plan = {}
            stats = {}
            for eng, lst in self.ops.items():
                known = {}
                nw = 0
                pl = []
                for o in lst:
                    need = {}
                    for d in o.deps:
                        t = self.ops[d[0]][d[1]].tok
                        if need.get(t[0], 0) < t[1]:
                            need[t[0]] = t[1]
                    if o.ring_wait:
                        t = o.ring_wait
                        if need.get(t[0], 0) < t[1]:
                            need[t[0]] = t[1]
                    ws = []
                    for s_, v in need.items():
                        if known.get(s_, 0) < v:
                            ws.append((s_, v))
                            known[s_] = v
                            nw += 1
                    pl.append(ws)
                plan[eng] = pl
                stats[eng] = (len(lst), nw)
            if os.environ.get('KSIM'):
                val = {k_: 0 for k_ in sems}
                pos = {e_: 0 for e_ in self.ops}
                prog = True
                while prog:
                    prog = False
                    for e_, lst in self.ops.items():
                        while pos[e_] < len(lst):
                            i_ = pos[e_]
                            if all(val[s_] >= v for s_, v in plan[e_][i_]):
                                o = lst[i_]
                                if o.tok is not None:
                                    val[o.tok[0]] += 16 if o.is_dma else 1
                                    assert val[o.tok[0]] == o.tok[1], (e_, i_, o.tok, val[o.tok[0]])
                                pos[e_] += 1
                                prog = True
                            else:
                                break
                for e_, lst in self.ops.items():
                    if pos[e_] < len(lst):
                        print("SIM DEADLOCK", e_, pos[e_], len(lst), plan[e_][pos[e_]], {s_: val[s_] for s_, v in plan[e_][pos[e_]]})
                print("SIM done", {e_: pos[e_] for e_ in pos})
            block = es.enter_context(nc.Block())
            for eng, lst in self.ops.items():
                def body(e, eng=eng, lst=lst):
                    for o, ws in zip(lst, plan[eng]):
                        for s_, v in ws:
                            e.wait_ge(sems[s_], v)
                        ins = o.fn(e)
                        if o.tok is not None:
                            ins.then_inc(sems[o.tok[0]], 16 if o.is_dma else 1)
                    for fo in list(final_ops) + tail:
                        e.wait_ge(sems[fo.tok[0]], fo.tok[1])
                getattr(block, engmap[eng])(body)
            self.stats = stats
        return stats


def build(L, dbg=False, phases=('pass', 'conv', 'attn', 'rwkv')):
    nc = bass.Bass("TRN2", target_bir_lowering=False)
    if FAST:
        nc.dge_precook = False

    def din(name, shape, dt=F32):
        return nc.dram_tensor(name, shape, dt, kind="ExternalInput").ap()

    x_in = din("x", [S, D])
    WDT = F32R if FAST else F32
    wgu = [din(f"wgu{i}", [L, NFC, 128, 2048], WDT) for i in (1, 2)]
    wd = [din(f"wd{i}", [L, 8, 128, NFC * 128], WDT) for i in (1, 2)]
    win = din("win", [L, NOC, 128, 1024], WDT)
    wout = din("wout", [L, 8, 128, 1024], WDT)
    lora_d = din("lora", [L, 128, 384])
    prm_d = din("prm", [128, L * NP])
    cst_d = din("cst", [128, NCST])
    abias_d = din("abias", [128, 18 * 256])
    out = nc.dram_tensor("out", [S, D], F32, kind="ExternalOutput").ap()
    skind = "ExternalOutput" if dbg else "Internal"
    xT = nc.dram_tensor("xT", [8, 128, S], F32, kind=skind).ap()
    projT = nc.dram_tensor("projT", [NOC, 128, S], F32, kind=skind).ap()
    mixT = nc.dram_tensor("mixT", [8, 128, S], F32R if FAST else F32, kind=skind).ap()

    P = Prog(nc)
    P.track = set()
    final_ops = []

    with contextlib.ExitStack() as gs:
        uid = [0]

        def sb(es, name, shape, fine=None, dt=F32):
            uid[0] += 1
            name = f"{name}_u{uid[0]}"
            t = es.enter_context(nc.sbuf_tensor(name, shape, dt))
            P.track.add(name)
            if fine:
                row = 1
                for s_ in shape[1:]:
                    row *= s_
                P.fine[name] = (fine, row)
            return t

        cst = sb(gs, "cst_s", [128, NCST], fine=128)
        prm = sb(gs, "prm_s", [128, L * NP])
        pbs = []
        for i in range(8):
            t = gs.enter_context(nc.psum_tensor(f"pb{i}", [128, 512], F32))
            P.track.add(f"pb{i}")
            pbs.append(t)
        P.dma('sp', cst[:], cst_d)
        P.dma('sp', prm[:], prm_d)
        ident = cst[:, C_ID:C_ID + 128]
        ones = cst[:, C_ONES:C_ONES + 128]
        bd64 = cst[:, C_BD:C_BD + 128]
        msu = cst[:, C_MSU:C_MSU + 128]
        msl = cst[:, C_MSL:C_MSL + 128]
        mu_ = cst[:, C_MU:C_MU + 128]
        rmask = cst[:, C_RM:C_RM + 512]

        def pcol(l, c, n=1):
            return prm[:, l * NP + c: l * NP + c + n]

        def run_pass(l):
            with contextlib.ExitStack() as es:
                xts = [sb(es, f"xt{i}", [128, 8, TT], fine=TT) for i in range(2)]
                tg = [sb(es, f"tg{i}", [128, TT]) for i in range(2)]
                cur = {}
                hT = sb(es, "hT", [128, 8, TT], dt=WDT)
                actT = sb(es, "actT", [128, NFC, TT], fine=TT, dt=WDT)
                sq = sb(es, "sq", [128, 8 * TT])
                rstd = sb(es, "rstd", [128, TT])
                sg = [sb(es, f"sg{i}", [128, TT]) for i in range(2)]
                wb = [sb(es, f"wb{i}", [128, 2048], dt=WDT) for i in range(3)]
                wdb = [sb(es, f"wdb{i}", [128, NFC * 128], dt=WDT) for i in range(2)]
                wb1 = [sb(es, f"wb1_{i}", [128, 1024], dt=WDT) for i in range(3)]
                pj = [sb(es, f"pj{i}", [128, TT]) for i in range(3)]
                xtok = [sb(es, f"xtok{i}", [128, D]) for i in range(2)]
                cnt = {'wb': 0, 'wd': 0, 'pj': 0, 'g': 0, 'o': 0}

                def norm_a(gcol0):
                    xt = cur['xt']
                    P.act(sq[:], xt[:].rearrange("p c t -> p (c t)"), AF.Square)
                    for dc in range(8):
                        P.ts('dve', hT[:, dc, :], xt[:, dc, :], pcol(l_cur[0], gcol0 + dc), ALU.mult)

                def norm_b():
                    for dc in range(8):
                        P.mm(pbs[0][:], ones, sq[:, dc * TT:(dc + 1) * TT], start=(dc == 0), stop=(dc == 7))
                    P.act(rstd[:], pbs[0][:], AF.Sqrt, bias=eps6[:, 0:1], scale=1.0 / D)
                    P.recip(rstd[:], rstd[:])

                def ffn(ll, which):
                    l_cur[0] = ll
                    xt = cur['xt']
                    norm_a(0 if which == 0 else 16)
                    for fc in range(NFC):
                        w = wb[cnt['wb'] % 3]
                        cnt['wb'] += 1
                        P.dma('sp', R(w[:]), wgu[which][ll, fc])
                        pg = pbs[1 + cnt['g'] % 2]
                        pu = pbs[3 + cnt['g'] % 2]
                        sgt = sg[cnt['g'] % 2]
                        cnt['g'] += 1
                        for dc in range(8):
                            P.mm(pg[:], R(w[:, dc * 128:(dc + 1) * 128]), R(hT[:, dc, :]), start=(dc == 0), stop=(dc == 7))
                        for dc in range(8):
                            P.mm(pu[:], R(w[:, 1024 + dc * 128:1024 + (dc + 1) * 128]), R(hT[:, dc, :]), start=(dc == 0), stop=(dc == 7))
                        if fc == 0:
                            norm_b()
                        t1 = tg[cnt['g'] % 2]
                        P.tt('dve', t1[:], pg[:], rstd[:], ALU.mult)
                        P.act(sgt[:], t1[:], AF.Silu)
                        P.tt('dve', t1[:], pu[:], rstd[:], ALU.mult)
                        P.tt('pool', R(actT[:, fc, :]), sgt[:], t1[:], ALU.mult)
                    for dc in range(8):
                        w = wdb[cnt['wd'] % 2]
                        cnt['wd'] += 1
                        P.dma('sp', R(w[:]), wd[which][ll, dc])
                        po = pbs[5 + cnt['o'] % 2]
                        cnt['o'] += 1
                        for fc in range(NFC):
                            P.mm(po[:], R(w[:, fc * 128:(fc + 1) * 128]), R(actT[:, fc, :]), start=(fc == 0), stop=(fc == NFC - 1))
                        P.stt('dve', xt[:, dc, :], po[:], 0.5, xt[:, dc, :], ALU.mult, ALU.add)

                def inproj(ll, tok0):
                    l_cur[0] = ll
                    xt = cur['xt']
                    norm_a(8)
                    for oc in range(NOC):
                        w = wb1[cnt['wb'] % 3]
                        cnt['wb'] += 1
                        P.dma('sp', w[:], win[ll, oc])
                        po = pbs[5 + cnt['o'] % 2]
                        cnt['o'] += 1
                        for dc in range(8):
                            P.mm(po[:], R(w[:, dc * 128:(dc + 1) * 128]), R(hT[:, dc, :]), start=(dc == 0), stop=(dc == 7))
                        pt = pj[cnt['pj'] % 3]
                        cnt['pj'] += 1
                        if oc == 0:
                            norm_b()
                        P.tt('dve', pt[:], po[:], rstd[:], ALU.mult)
                        P.dma('pool', projT[oc, :, tok0:tok0 + TT], pt[:])

                def outproj(ll, tok0):
                    xt = cur['xt']
                    mt = actT
                    P.dma('pool', R(mt[:, 0:8, :]), R(mixT[:, :, tok0:tok0 + TT]).rearrange("c p t -> p c t"))
                    for dc in range(8):
                        w = wb1[cnt['wb'] % 3]
                        cnt['wb'] += 1
                        P.dma('sp', w[:], wout[ll, dc])
                        po = pbs[5 + cnt['o'] % 2]
                        cnt['o'] += 1
                        for kc in range(8):
                            P.mm(po[:], R(w[:, kc * 128:(kc + 1) * 128]), R(mt[:, kc, :]), start=(kc == 0), stop=(kc == 7))
                        P.tt('dve', xt[:, dc, :], po[:], xt[:, dc, :], ALU.add)

                l_cur = [0]
                eps6 = sb(es, "eps6", [128, 1])
                P.memset('dve', eps6[:], 1e-6)
                def load_x(tt_):
                    xt = xts[tt_ % 2]
                    tok0 = tt_ * TT
                    if l == 0:
                        for s4 in range(4):
                            xk = xtok[s4 % 2]
                            P.dma('pool', xk[:], x_in[tok0 + s4 * 128: tok0 + (s4 + 1) * 128, :])
                            for half in range(2):
                                pb = pbs[5 + half]
                                for q in range(4):
                                    dc = half * 4 + q
                                    P.tr(pb[:, q * 128:(q + 1) * 128], xk[:, dc * 128:(dc + 1) * 128], ident)
                                P.copy('act' if half else 'dve', xt[:, half * 4:half * 4 + 4, s4 * 128:(s4 + 1) * 128],
                                       pb[:].rearrange("p (c t) -> p c t", t=128))
                    else:
                        P.dma('pool', xt[:], xT[:, :, tok0:tok0 + TT].rearrange("c p t -> p c t"))

                NTL = int(os.environ.get('KNT', NT))
                if l > 0:
                    load_x(0)
                for tt_ in range(NTL):
                    tok0 = tt_ * TT
                    xt = xts[tt_ % 2]
                    cur['xt'] = xt
                    if l == 0:
                        load_x(tt_)
                    elif tt_ + 1 < NTL:
                        load_x(tt_ + 1)
                    if l > 0:
                        if 'o' in KF:
                            outproj(l - 1, tok0)
                        if 'f' in KF:
                            ffn(l - 1, 1)
                    if l < L:
                        if 'f' in KF:
                            ffn(l, 0)
                        if 'i' in KF:
                            inproj(l, tok0)
                        P.dma('pool', xT[:, :, tok0:tok0 + TT].rearrange("c p t -> p c t"), xt[:])
                    else:
                        for s4 in range(4):
                            xk = xtok[s4 % 2]
                            for half in range(2):
                                pb = pbs[5 + half]
                                for q in range(4):
                                    dc = half * 4 + q
                                    P.tr(pb[:, q * 128:(q + 1) * 128], xt[:, dc, s4 * 128:(s4 + 1) * 128], ident)
                                P.copy('act' if half else 'dve', xk[:, half * 512:(half + 1) * 512], pb[:])
                            final_ops.append(P.dma('pool', out[tok0 + s4 * 128: tok0 + (s4 + 1) * 128, :], xk[:]))
            P.barrier()

        def run_conv(l):
            with contextlib.ExitStack() as es:
                a_t = sb(es, "cv_a", [128, S])
                b_t = sb(es, "cv_b", [128, S])
                zp = sb(es, "cv_zp", [128, 30 + S])
                zc = [sb(es, f"cv_zc{i}", [128, S]) for i in range(2)]
                sqs = [sb(es, f"cv_sq{i}", [128, TT]) for i in range(2)]
                means = [sb(es, f"cv_mean{i}", [128, TT]) for i in range(2)]
                vars_ = [sb(es, f"cv_var{i}", [128, TT]) for i in range(2)]
                yts = [sb(es, f"cv_y{i}", [128, TT]) for i in range(4)]
                eps5 = sb(es, "eps5", [128, 1])
                P.memset('dve', eps5[:], 1e-5)
                P.memset('dve', zp[:, 0:30], 0.0)
                for ch in range(2):
                    P.dma('pool', a_t[:], projT[19 + ch, :, :])
                    P.dma('pool', b_t[:], projT[21 + ch, :, :])
                    for h in range(4):
                        sl = slice(h * 1024, (h + 1) * 1024)
                        P.act(b_t[:, sl], b_t[:, sl], AF.Sigmoid)
                    P.tt(os.environ.get('KCE', 'pool'), zp[:, 30:30 + S], a_t[:], b_t[:], ALU.mult)
                    if 'b' not in os.environ.get('KC', 'abc'):
                        continue
                    wc = 63 + ch * 31
                    acc = zc[ch]
                    P.ts('dve', acc[:], zp[:, 0:S], pcol(l, wc), ALU.mult, pcol(l, 57 + ch), ALU.add)
                    for j in range(1, 31):
                        P.stt('dve', acc[:], zp[:, j:j + S], pcol(l, wc + j), acc[:], ALU.mult, ALU.add)
                for tt_ in range(NT if 'c' in os.environ.get('KC', 'abc') else 0):
                    sl = slice(tt_ * TT, (tt_ + 1) * TT)
                    pr_ = tt_ % 2
                    mean, var = means[pr_], vars_[pr_]
                    pm0, pm1 = pbs[2 * pr_], pbs[2 * pr_ + 1]
                    yt = yts[2 * pr_: 2 * pr_ + 2]
                    for ch in range(2):
                        P.mm(pm0[:], ones, zc[ch][:, sl], start=(ch == 0), stop=(ch == 1))
                    sq = sqs[pr_]
                    P.act(sq[:], zc[0][:, sl], AF.Square)
                    P.mm(pm1[:], ones, sq[:], start=True, stop=False)
                    sq2 = yt[1]
                    P.act(sq2[:], zc[1][:, sl], AF.Square)
                    P.mm(pm1[:], ones, sq2[:], start=False, stop=True)
                    P.ts('dve', mean[:], pm0[:], 1.0 / 256, ALU.mult)
                    P.tt('dve', var[:], mean[:], mean[:], ALU.mult)
                    P.stt('dve', var[:], pm1[:], 1.0 / 256, var[:], ALU.mult, ALU.subtract)
                    P.act(var[:], var[:], AF.Sqrt, bias=eps5[:, 0:1], scale=1.0)
                    P.recip(var[:], var[:])
                    for ch in range(2):
                        y = yt[ch]
                        P.tt('dve', y[:], zc[ch][:, sl], mean[:], ALU.subtract)
                        P.tt('dve', y[:], y[:], var[:], ALU.mult)
                        P.act(y[:], y[:], AF.Silu, bias=pcol(l, 61 + ch), scale=pcol(l, 59 + ch))
                        P.dma('pool', mixT[6 + ch, :, sl], y[:])
            P.barrier()

        def run_attn(l):
            with contextlib.ExitStack() as es:
                q = sb(es, "at_q", [128, S])
                k = sb(es, "at_k", [128, S])
                v = sb(es, "at_v", [128, S])
                qn = sb(es, "at_qn", [128, S], dt=WDT)
                kn = sb(es, "at_kn", [128, S], dt=WDT)
                acc = [sb(es, f"at_acc{i}", [128, S]) for i in range(2)]
                vaug = [sb(es, f"at_va{i}", [128, 32, 128], fine=128, dt=WDT) for i in range(2)]
                ab = sb(es, "at_bias", [128, 18 * 256], fine=256)
                et = [sb(es, f"at_e{i}", [128, 256], dt=WDT) for i in range(4)]
                e0 = [sb(es, f"at_e0{i}", [128, 256]) for i in range(4)]
                sqs = [sb(es, f"at_sq{i}", [128, TT]) for i in range(3)]
                rss = [sb(es, f"at_rs{i}", [128, TT]) for i in range(3)]
                gq = sb(es, "at_gq", [128, 1])
                eps6 = sb(es, "at_eps", [128, 1])
                P.memset('dve', eps6[:], 1e-6)
                P.dma('sp', ab[:], abias_d)
                P.ts('dve', gq[:], pcol(l, 55), 0.125, ALU.mult)
                fin_ = ab[:, 0:2048].rearrange("p (b c) -> p b c", c=64)
                P.ts('dve', vaug[0][:, :, 64:128], fin_, 0.0, ALU.mult, 1.0, ALU.add)
                P.ts('dve', vaug[1][:, :, 0:64], fin_, 0.0, ALU.mult, 1.0, ALU.add)
                ne = 0
                for c in range(int(os.environ.get('KACH', 3))):
                    P.dma('pool', q[:], projT[10 + c, :, :])
                    P.dma('pool', k[:], projT[13 + c, :, :])
                    P.dma('pool', v[:], projT[16 + c, :, :])
                    for (t_, tn_, gcol) in ((q, qn, gq[:, 0:1]), (k, kn, pcol(l, 56))):
                        for tt_ in range(NT):
                            sl = slice(tt_ * TT, (tt_ + 1) * TT)
                            sq, rs, pn = sqs[tt_ % 3], rss[tt_ % 3], pbs[tt_ % 3]
                            P.act(sq[:], t_[:, sl], AF.Square)
                            P.mm(pn[:], bd64, sq[:])
                            P.act(rs[:], pn[:], AF.Ln, bias=eps6[:, 0:1], scale=1.0 / 64)
                            P.act(rs[:], rs[:], AF.Exp, scale=-0.5)
                            P.stt('dve', tn_[:, sl], t_[:, sl], gcol, rs[:], ALU.mult, ALU.mult)
                    P.memset('pool', acc[0][:], 0.0)
                    P.memset('pool', acc[1][:], 0.0)
                    for pi in [int(ch_) for ch_ in os.environ.get('KAP', '012')]:
                        win_, d = PATTERNS[pi]
                        nb = 32 // d
                        Lsub = S // d
                        if os.environ.get('KAB'):
                            P.barrier()
                        for cls in range(d):
                            for kb in range(nb):
                                b = cls * nb + kb
                                st = cls + d * kb * 128
                                pb = pbs[1 + b % 2]
                                P.tr(pb[:, 0:128], v[:, st: st + d * 127 + 1: d] if d > 1 else v[:, st:st + 128], ident)
                                ce = 'act' if b % 2 else 'dve'
                                P.copy(ce, vaug[0][:, b, 0:64], pb[:, 0:64])
                                P.copy(ce, vaug[1][:, b, 64:128], pb[:, 64:128])
                        blocks = []
                        for hh in range(2):
                            for cls in range(d):
                                for kb in range(nb):
                                    blocks.append((hh, cls, kb))
                        pend = []

                        def emit_pv(blk):
                            hh, cls, kb, e_, nq, ks, i_ = blk
                            b = cls * nb + kb
                            po_ = pbs[5 + i_ % 2]
                            P.mm(po_[:, 0:nq], vaug[hh][:, b, :], e_[:, 0:nq])
                            aap = acc[hh][:, ks: ks + d * (nq - 1) + 1: d] if d > 1 else acc[hh][:, ks:ks + nq]
                            P.tt('dve', aap, aap, po_[:, 0:nq], ALU.add)

                        for (hh, cls, kb) in blocks:
                            h = 2 * c + hh
                            hp = slice(hh * 64, hh * 64 + 64)
                            nq = min(256, Lsub - kb * 128)
                            ks = cls + d * kb * 128
                            kap = kn[hp, ks: ks + d * 127 + 1: d] if d > 1 else kn[hp, ks:ks + 128]
                            qap = qn[hp, ks: ks + d * (nq - 1) + 1: d] if d > 1 else qn[hp, ks:ks + nq]
                            ps_ = pbs[(3, 4, 7)[ne % 3]]
                            e_ = et[ne % 4]
                            ee = e0[ne % 4]
                            P.mm(ps_[:, 0:nq], kap, qap, start=True, stop=True)
                            bi = (h * 3 + pi) * 256
                            P.act(ee[:, 0:nq], ps_[:, 0:nq], AF.Exp)
                            P.tt('pool', e_[:, 0:nq], ee[:, 0:nq], ab[:, bi:bi + nq], ALU.mult)
                            pend.append((hh, cls, kb, e_, nq, ks, ne))
                            ne += 1
                            if len(pend) > 2:
                                emit_pv(pend.pop(0))
                        while pend:
                            emit_pv(pend.pop(0))
                    for h4 in range(4):
                        sl = slice(h4 * 1024, (h4 + 1) * 1024)
                        P.copy('dve', q[0:64, sl], acc[0][64:128, sl])
                        P.act(q[0:64, sl], q[0:64, sl], AF.Ln)
                        P.act(q[0:64, sl], q[0:64, sl], AF.Exp, scale=-1.0)
                        P.tt('dve', q[0:64, sl], acc[0][0:64, sl], q[0:64, sl], ALU.mult)
                        P.copy('dve', q[64:128, sl], acc[1][0:64, sl])
                        P.act(q[64:128, sl], q[64:128, sl], AF.Ln)
                        P.act(q[64:128, sl], q[64:128, sl], AF.Exp, scale=-1.0)
                        P.tt('dve', q[64:128, sl], acc[1][64:128, sl], q[64:128, sl], ALU.mult)
                    P.dma('pool', mixT[3 + c, :, :], q[:])
            P.barrier()

        def run_rwkv(l):
            SEG = 512
            NSEG = S // SEG
            PCM = ('N', 'NT', 'N2', 'N2T', 'Pm', 'Pm2', 'Aak', 'Arb', 'Ark', 'bT', 'kT', 'Vb')
            NCH = SEG // 64
            with contextlib.ExitStack() as es:
                lora = sb(es, "rw_lora", [128, 384])
                P.dma('sp', lora[:], lora_d[l])
                eps12 = sb(es, "rw_e12", [128, 1])
                P.memset('dve', eps12[:], 1e-12)
                epsgn = sb(es, "rw_egn", [128, 1])
                P.memset('dve', epsgn[:], 64e-5)
                praw = sb(es, "rw_praw", [128, 1 + SEG])
                x9 = sb(es, "rw_x9", [128, SEG])
                tmp = sb(es, "rw_tmp", [128, SEG])
                tmp2 = sb(es, "rw_tmp2", [128, SEG])
                pr = []
                for c in range(3):
                    d_ = {}
                    for nm in ('r', 'k', 'v', 'a', 'kk', 'lw', 'cum', 'g', 'bonus', 'y', 'wc'):
                        d_[nm] = sb(es, f"rw_{nm}{c}", [128, SEG])
                    for nm in ('at', 'rt', 'bt', 'kt', 'vb'):
                        d_[nm] = sb(es, f"rw_{nm}bd{c}", [128, NCH, 128], fine=128)
                        P.memset('pool', d_[nm][:], 0.0)
                    d_['ST'] = [sb(es, f"rw_ST{c}_{i}", [128, 128]) for i in range(2)]
                    P.memset('dve', d_['ST'][0][:], 0.0)
                    for nm in PCM:
                        d_[nm] = [sb(es, f"rw_{nm}{c}_{i}", [128, 128]) for i in range(2)]
                    for nm in ('XT', 'Ub'):
                        d_[nm] = sb(es, f"rw_{nm}{c}", [128, 128])
                    d_['wend'] = sb(es, f"rw_wend{c}", [128, NCH])
                    pr.append(d_)
                stp = [0, 0, 0]

                def shift_load(dst, ch, tok0, mucol):
                    if tok0 == 0:
                        P.memset('dve', praw[:, 0:1], 0.0)
                        P.dma('pool', praw[:, 1:1 + SEG], projT[ch, :, 0:SEG])
                    else:
                        P.dma('pool', praw[:], projT[ch, :, tok0 - 1:tok0 + SEG])
                    P.tt('dve', tmp[:], praw[:, 0:SEG], praw[:, 1:1 + SEG], ALU.subtract)
                    P.stt('dve', dst, tmp[:], mucol, praw[:, 1:1 + SEG], ALU.mult, ALU.add)

                for seg in range(int(os.environ.get('KSEG', NSEG))):
                    tok0 = seg * SEG
                    shift_load(x9[:], 9, tok0, pcol(l, 24 + 9))
                    P.act(x9[0:32, :], x9[0:32, :], AF.Tanh)
                    P.act(x9[64:128, :], x9[64:128, :], AF.Sigmoid)
                    for c in range(3 if 'b' in os.environ.get('KRW', 'abcd') else 0):
                        d_ = pr[c]
                        cs = slice(c * 128, (c + 1) * 128)
                        shift_load(d_['r'][:], c, tok0, pcol(l, 24 + c))
                        shift_load(d_['k'][:], 3 + c, tok0, pcol(l, 24 + 3 + c))
                        shift_load(d_['v'][:], 6 + c, tok0, pcol(l, 24 + 6 + c))
                        P.mm(pbs[0][:], lora[0:32, cs], x9[0:32, :])
                        P.act(d_['lw'][:], pbs[0][:], AF.Sigmoid, bias=pcol(l, 34 + c), scale=1.0)
                        P.mm(pbs[1][:], lora[32:64, cs], x9[32:64, :])
                        P.act(d_['a'][:], pbs[1][:], AF.Sigmoid, bias=pcol(l, 37 + c), scale=1.0)
                        P.mm(pbs[2][:], lora[64:128, cs], x9[64:128, :])
                        P.copy('act', d_['g'][:], pbs[2][:])
                        P.ts('dve', d_['kk'][:], d_['k'][:], pcol(l, 40 + c), ALU.mult)
                        P.act(tmp2[:], d_['kk'][:], AF.Square)
                        P.mm(pbs[0][:], bd64, tmp2[:])
                        P.act(tmp2[:], pbs[0][:], AF.Ln, bias=eps12[:, 0:1], scale=1.0)
                        P.act(tmp2[:], tmp2[:], AF.Exp, scale=-0.5)
                        P.tt('dve', d_['kk'][:], d_['kk'][:], tmp2[:], ALU.mult)
                        P.ts('dve', tmp2[:], d_['a'][:], -1.0, ALU.add, pcol(l, 43 + c), ALU.mult)
                        P.stt('dve', d_['k'][:], tmp2[:], 1.0, d_['k'][:], ALU.add, ALU.mult)
                        P.stt('dve', tmp2[:], d_['r'][:], pcol(l, 46 + c), d_['k'][:], ALU.mult, ALU.mult)
                        P.mm(pbs[1][:], bd64, tmp2[:])
                        P.tt('dve', d_['bonus'][:], pbs[1][:], d_['v'][:], ALU.mult)
                        P.ts('dve', d_['lw'][:], d_['lw'][:], -EXPM05, ALU.mult)
                        P.scan(d_['cum'][:], rmask, d_['lw'][:], 0.0, ALU.mult, ALU.add)
                        P.act(d_['wc'][:], d_['cum'][:], AF.Exp)
                        P.copy('dve', d_['wend'][:], d_['wc'][:].rearrange("p (c t) -> p c t", t=64)[:, :, 63])
                        P.tt('dve', tmp2[:], d_['cum'][:], d_['lw'][:], ALU.subtract)
                        P.act(tmp2[:], tmp2[:], AF.Exp)
                        P.tt('dve', tmp2[:], tmp2[:], d_['kk'][:], ALU.mult)
                        P.act(tmp[:], d_['cum'][:], AF.Exp, scale=-1.0)
                        P.tt('pool', d_['y'][:], d_['kk'][:], d_['a'][:], ALU.mult)
                        for hh in range(2):
                            hp = slice(hh * 64, hh * 64 + 64)
                            cp = slice(hh * 64, hh * 64 + 64)

                            def v3(t):
                                return t[hp, :].rearrange("p (c t) -> p c t", t=64)
                            P.ts('dve', d_['at'][hp, :, cp], v3(tmp2), -1.0, ALU.mult)
                            P.tt('dve', d_['rt'][hp, :, cp], v3(d_['r']), v3(d_['wc']), ALU.mult)
                            P.tt('dve', d_['bt'][hp, :, cp], v3(d_['y']), v3(tmp), ALU.mult)
                            P.tt('dve', d_['kt'][hp, :, cp], v3(d_['k']), v3(tmp), ALU.mult)
                            P.copy('pool', d_['vb'][hp, :, cp], v3(d_['v']))
                    def pre_gen(n, c):
                        d_ = pr[c]
                        par = n % 2
                        M = {nm: d_[nm][par] for nm in PCM}
                        at, rt, bt, kt, vb = (d_[x][:, n, :] for x in ('at', 'rt', 'bt', 'kt', 'vb'))
                        pq = [pbs[c * 2][:, 0:128], pbs[c * 2][:, 128:256], pbs[c * 2][:, 256:384], pbs[c * 2][:, 384:512],
                              pbs[c * 2 + 1][:, 0:128], pbs[c * 2 + 1][:, 128:256], pbs[c * 2 + 1][:, 256:384], pbs[c * 2 + 1][:, 384:512]]
                        P.mm(pq[0], bt, at)
                        P.mm(pq[1], at, bt)
                        P.mm(pq[2], kt, at)
                        P.mm(pq[3], bt, rt)
                        P.mm(pq[4], kt, rt)
                        P.tr(pq[5], bt, ident)
                        P.tr(pq[6], kt, ident)
                        P.tr(pq[7], vb, ident)
                        yield
                        P.tt('dve', M['N'][:], pq[0], msu, ALU.mult)
                        P.tt('dve', M['NT'][:], pq[1], msl, ALU.mult)
                        P.tt('dve', M['Aak'][:], pq[2], msu, ALU.mult)
                        P.tt('dve', M['Arb'][:], pq[3], mu_, ALU.mult)
                        P.tt('dve', M['Ark'][:], pq[4], mu_, ALU.mult)
                        P.copy('dve', M['bT'][:], pq[5])
                        P.copy('dve', M['kT'][:], pq[6])
                        P.copy('dve', M['Vb'][:], pq[7])
                        P.tt('dve', M['Pm'][:], M['N'][:], ident, ALU.add)
                        yield
                        cur, curT, nxt, nxtT = M['N'], M['NT'], M['N2'], M['N2T']
                        Pc, Pn = M['Pm'], M['Pm2']
                        for lev in range(5):
                            P.mm(pq[0], cur[:], curT[:])
                            if lev < 4:
                                P.mm(pq[4], curT[:], cur[:])
                            if lev > 0:
                                P.mm(pq[5], curT[:], Pc[:])
                            yield
                            P.copy('act', nxtT[:], pq[0])
                            if lev < 4:
                                P.copy('dve', nxt[:], pq[4])
                            if lev > 0:
                                P.tt('dve', Pn[:], pq[5], Pc[:], ALU.add)
                                Pc, Pn = Pn, Pc
                            yield
                            cur, curT, nxt, nxtT = nxt, nxtT, cur, curT
                        P.mm(pq[5], curT[:], Pc[:])
                        yield
                        P.tt('dve', Pn[:], pq[5], Pc[:], ALU.add)
                        minv[(c, n)] = Pn
                        yield

                    def seq_gen(n, c):
                        d_ = pr[c]
                        par = n % 2
                        M = {nm: d_[nm][par] for nm in PCM}
                        at, rt = d_['at'][:, n, :], d_['rt'][:, n, :]
                        Minv = minv[(c, n)]
                        pa = pbs[6][:, c * 128:(c + 1) * 128]
                        pd = pbs[7][:, c * 128:(c + 1) * 128]
                        ST0 = d_['ST'][stp[c] % 2]
                        ST1 = d_['ST'][(stp[c] + 1) % 2]
                        stp[c] += 1
                        P.mm(pa, at, ST0[:], start=True, stop=False)
                        P.mm(pa, M['Aak'][:], M['Vb'][:], start=False, stop=True)
                        yield
                        P.copy('act', d_['XT'][:], pa)
                        yield
                        P.mm(pd, Minv[:], d_['XT'][:])
                        yield
                        P.copy('dve', d_['Ub'][:], pd)
                        yield
                        P.mm(pd, ident, ST0[:], start=True, stop=False)
                        P.mm(pd, M['bT'][:], d_['Ub'][:], start=False, stop=False)
                        P.mm(pd, M['kT'][:], M['Vb'][:], start=False, stop=True)
                        P.mm(pa, ST0[:], rt, start=True, stop=False)
                        P.mm(pa, d_['Ub'][:], M['Arb'][:], start=False, stop=False)
                        P.mm(pa, M['Vb'][:], M['Ark'][:], start=False, stop=True)
                        yield
                        P.ts('dve', ST1[:], pd, d_['wend'][:, n:n + 1], ALU.mult)
                        P.copy('act', d_['y'][0:64, n * 64:(n + 1) * 64], pa[0:64, 0:64])
                        P.copy('act', d_['y'][64:128, n * 64:(n + 1) * 64], pa[64:128, 64:128])
                        yield

                    minv = {}
                    for n in range(NCH + 1):
                        gens = []
                        if n < NCH:
                            gens += [pre_gen(n, c) for c in range(3)]
                        if n >= 1:
                            gens += [seq_gen(n - 1, c) for c in range(3)]
                        offs = [int(x_) for x_ in os.environ.get('KOFF', '0,1,2,1,2,3').split(',')]
                        done_ = [False] * len(gens)
                        tick = 0
                        while not all(done_):
                            for gi, g_ in enumerate(gens):
                                if done_[gi] or tick < offs[gi % len(offs)]:
                                    continue
                                try:
                                    next(g_)
                                except StopIteration:
                                    done_[gi] = True
                            tick += 1
                    for c in range(3 if 'd' in os.environ.get('KRW', 'abcd') else 0):
                        d_ = pr[c]
                        P.mm(pbs[6][:], bd64, d_['y'][:])
                        P.ts('dve', tmp[:], pbs[6][:], 1.0 / 64, ALU.mult)
                        P.tt('dve', d_['y'][:], d_['y'][:], tmp[:], ALU.subtract)
                        P.act(tmp2[:], d_['y'][:], AF.Square)
                        P.mm(pbs[7][:], bd64, tmp2[:])
                        P.act(tmp2[:], pbs[7][:], AF.Ln, bias=epsgn[:, 0:1], scale=1.0 / 64)
                        P.act(tmp2[:], tmp2[:], AF.Exp, scale=-0.5)
                        P.tt('dve', d_['y'][:], d_['y'][:], tmp2[:], ALU.mult)
                        P.ts('dve', d_['y'][:], d_['y'][:], pcol(l, 49 + c), ALU.mult, pcol(l, 52 + c), ALU.add)
                        P.tt('dve', d_['y'][:], d_['y'][:], d_['bonus'][:], ALU.add)
                        P.tt('dve', d_['wc'][:], d_['y'][:], d_['g'][:], ALU.mult)
                        P.dma('pool', mixT[c, :, tok0:tok0 + SEG], d_['wc'][:])
            P.barrier()

        P.barrier()
        for l in range(L + 1):
            if 'pass' in phases:
                run_pass(l)
            if l < L:
                if 'conv' in phases:
                    run_conv(l)
                if 'attn' in phases:
                    run_attn(l)
                if 'rwkv' in phases:
                    run_rwkv(l)
        st = P.emit(final_ops)
    return nc, st


def _alibi_slopes(n):
    def pow2(m):
        start = 2.0 ** (-8.0 / m)
        return [start ** (i + 1) for i in range(m)]
    if math.log2(n).is_integer():
        return pow2(n)
    c = 2 ** int(math.floor(math.log2(n)))
    return pow2(c) + pow2(2 * c)[0::2][: n - c]


def _consts():
    cst = np.zeros((128, NCST), np.float32)
    cst[:, C_ID:C_ID + 128] = np.eye(128, dtype=np.float32)
    cst[:, C_ONES:C_ONES + 128] = 1.0
    i = np.arange(128)
    same = (i[:, None] // 64) == (i[None, :] // 64)
    cst[:, C_BD:C_BD + 128] = same
    loc = i % 64
    cst[:, C_MSU:C_MSU + 128] = same & (loc[:, None] < loc[None, :])
    cst[:, C_MSL:C_MSL + 128] = same & (loc[:, None] > loc[None, :])
    cst[:, C_MU:C_MU + 128] = same & (loc[:, None] <= loc[None, :])
    cst[:, C_RM:C_RM + 512] = (np.arange(512) % 64 != 0).astype(np.float32)[None, :]
    slopes = _alibi_slopes(6)
    ab = np.zeros((128, 18, 256), np.float32)
    kk = np.arange(128)[:, None]
    qq = np.arange(256)[None, :]
    dist = qq - kk
    for h in range(6):
        for pi, (w, d) in enumerate(PATTERNS):
            valid = (dist >= 0) & (dist <= w // d)
            ab[:, h * 3 + pi, :] = np.where(valid, np.exp(-slopes[h] * (dist * d).astype(np.float64)), 0.0).astype(np.float32)
    return cst, ab.reshape(128, 18 * 256)


def _cols(v):
    v = np.asarray(v, np.float32).reshape(-1, 128)
    return v.T


def _prep(inp, L):
    f = lambda a: np.ascontiguousarray(np.asarray(a, np.float32))
    m = {}
    for i, nm in ((1, 'ffn1'), (2, 'ffn2')):
        g = f(inp[f'{nm}_w_gate'])[:L].reshape(L, 8, 128, NFC, 128).transpose(0, 3, 2, 1, 4)
        u = f(inp[f'{nm}_w_up'])[:L].reshape(L, 8, 128, NFC, 128).transpose(0, 3, 2, 1, 4)
        m[f'wgu{i}'] = np.ascontiguousarray(np.stack([g, u], axis=3)).reshape(L, NFC, 128, 2048)
        dn = f(inp[f'{nm}_w_down'])[:L].reshape(L, NFC, 128, 8, 128).transpose(0, 3, 2, 1, 4)
        m[f'wd{i}'] = np.ascontiguousarray(dn).reshape(L, 8, 128, NFC * 128)
    m['win'] = np.ascontiguousarray(f(inp['w_in'])[:L].reshape(L, 8, 128, NOC, 128).transpose(0, 3, 2, 1, 4)).reshape(L, NOC, 128, 1024)
    m['wout'] = np.ascontiguousarray(f(inp['w_out'])[:L].reshape(L, 8, 128, 8, 128).transpose(0, 3, 2, 1, 4)).reshape(L, 8, 128, 1024)
    m['lora'] = np.ascontiguousarray(np.concatenate([f(inp['rwkv_w2'])[:L], f(inp['rwkv_a2'])[:L], f(inp['rwkv_g2'])[:L]], axis=1))
    prm = np.zeros((128, L, NP), np.float32)
    for l in range(L):
        prm[:, l, 0:8] = _cols(inp['norm_ffn1'][l])
        prm[:, l, 8:16] = _cols(inp['norm_mix'][l])
        prm[:, l, 16:24] = _cols(inp['norm_ffn2'][l])
        prm[:, l, 24:34] = _cols(inp['shift_mu'][l])
        prm[:, l, 34:37] = _cols(inp['rwkv_w0'][l])
        prm[:, l, 37:40] = _cols(inp['rwkv_a0'][l])
        prm[:, l, 40:43] = _cols(inp['rwkv_k_k'][l])
        prm[:, l, 43:46] = _cols(inp['rwkv_k_a'][l])
        prm[:, l, 46:49] = _cols(np.asarray(inp['rwkv_r_k'][l]).reshape(-1))
        prm[:, l, 49:52] = _cols(inp['rwkv_ln_w'][l])
        prm[:, l, 52:55] = _cols(inp['rwkv_ln_b'][l])
        prm[:, l, 55] = np.tile(np.asarray(inp['attn_q_norm'][l], np.float32), 2)
        prm[:, l, 56] = np.tile(np.asarray(inp['attn_k_norm'][l], np.float32), 2)
        prm[:, l, 57:59] = _cols(inp['conv_dw_b'][l])
        prm[:, l, 59:61] = _cols(inp['conv_ln_w'][l])
        prm[:, l, 61:63] = _cols(inp['conv_ln_b'][l])
        cw = np.asarray(inp['conv_dw_w'][l], np.float32)
        for ch in range(2):
            prm[:, l, 63 + ch * 31: 63 + (ch + 1) * 31] = cw[:, ch * 128:(ch + 1) * 128].T
    m['prm'] = np.ascontiguousarray(prm.reshape(128, L * NP))
    cst, ab = _consts()
    m['cst'] = cst
    m['abias'] = ab
    return m


_CACHE = {}


def kernel(**inputs):
    L = 4
    x = np.ascontiguousarray(np.asarray(inputs['x'], np.float32))
    B = x.shape[0]
    if 'nc' not in _CACHE:
        _CACHE['nc'] = build(L)[0]
    nc = _CACHE['nc']
    shared = _prep(inputs, L)
    in_maps = []
    for b in range(B):
        d = dict(shared)
        d['x'] = x[b]
        in_maps.append(d)
    res = run_bass_kernel_spmd(nc, in_maps, core_ids=list(range(B)))
    return np.stack([np.asarray(r['out'], np.float32) for r in res.results], axis=0)
```
